# Optimizing a Trainium2 kernel written in Bass

```python
import math
import jax, jax.numpy as jnp
from jax import lax
import numpy as np

D_MODEL = 1024
BATCH = 2
SEQ = 8192
DEPTH = 1

CHUNK = 64
Q_BLOCK = 128
N_HEADS_A = 4
HEAD_DIM = 64
D_ATTN = N_HEADS_A * 2 * HEAD_DIM
POOL_WINDOWS = (2, 4, 8, 16)
N_POOL_GROUPS = 4
POOL_GROUP_DIM = 128
D_POOL = N_POOL_GROUPS * POOL_GROUP_DIM
D_IN = 3 * D_ATTN + D_POOL + 2 * D_MODEL
N_BUCKETS = 32
MAX_DISTANCE = 128
N_EXPERT_GROUPS = 4
EXPERTS_PER_GROUP = 4
N_EXPERTS = N_EXPERT_GROUPS * EXPERTS_PER_GROUP
TOP_K_INNER = 2
D_EXPERT = 512
RMS_EPS = 1e-6

kernel_name = "hybrid_diffattn_pool_hmoe_block"


def rms_norm(x, g):
    xf = x.astype(jnp.float32)
    y = xf * lax.rsqrt(jnp.mean(xf * xf, axis=-1, keepdims=True) + RMS_EPS)
    return (y * g.astype(jnp.float32)).astype(x.dtype)


def t5_bucket(rel):
    nb = N_BUCKETS // 2
    max_exact = nb // 2
    bucket = jnp.where(rel > 0, nb, 0)
    n = jnp.abs(rel)
    n_f = jnp.maximum(n, max_exact).astype(jnp.float32)
    large = max_exact + (jnp.log(n_f / max_exact) / math.log(MAX_DISTANCE / max_exact)
                         * (nb - max_exact)).astype(jnp.int32)
    large = jnp.minimum(large, nb - 1)
    return bucket + jnp.where(n < max_exact, n, large)


def diff_attention(q, k, v, rel_bias, lam):
    s = q.shape[1]
    scale = HEAD_DIM ** -0.5
    outs = []
    for i in range(s // Q_BLOCK):
        q0 = i * Q_BLOCK
        kend = q0 + Q_BLOCK
        qb = q[:, q0:kend]
        kb = k[:, :kend]
        vb = v[:, :kend]
        qpos = jnp.arange(q0, kend)
        kpos = jnp.arange(kend)
        rel = kpos[None, :] - qpos[:, None]
        bias = jnp.transpose(rel_bias[t5_bucket(rel)], (2, 0, 1)).astype(jnp.float32)
        mask = (kpos[None, :] // CHUNK) <= (qpos[:, None] // CHUNK)
        logits = jnp.einsum('bqhmd,bkhmd->bhmqk', qb, kb,
                            preferred_element_type=jnp.float32) * scale + bias[None, :, None]
        logits = jnp.where(mask, logits, -jnp.inf)
        p = jax.nn.softmax(logits, axis=-1)
        a = p[:, :, 0] - lam * p[:, :, 1]
        outs.append(jnp.einsum('bhqk,bkhe->bqhe', a.astype(v.dtype), vb))
    return jnp.concatenate(outs, axis=1)


def multiscale_pool(u, pool_w, pool_scale):
    b, s, _ = u.shape
    ug = u.reshape(b, s, N_POOL_GROUPS, POOL_GROUP_DIM)
    t = jnp.arange(s)
    pooled = []
    for g, w in enumerate(POOL_WINDOWS):
        ch = ug[:, :, g].astype(jnp.float32)
        cs = jnp.concatenate([jnp.zeros((b, 1, POOL_GROUP_DIM), jnp.float32),
                              jnp.cumsum(ch, axis=1)], axis=1)
        lo = jnp.maximum(t + 1 - w, 0)
        win_sum = cs[:, 1:] - cs[:, lo]
        count = jnp.minimum(t + 1, w).astype(jnp.float32)
        pooled.append(win_sum / count[None, :, None] - ch)
    m = jnp.stack(pooled, axis=2).astype(u.dtype)
    y = jnp.einsum('bsgc,gcd->bsgd', m, pool_w).reshape(b, s, D_POOL)
    return y * pool_scale


def hierarchical_moe(h, wg_r, bg_r, we_r, be_r, w_gate, w_up, w_down):
    b, s, d = h.shape
    t = h.reshape(-1, d)
    g_prob = jax.nn.softmax((t @ wg_r + bg_r).astype(jnp.float32), axis=-1)
    g_val, g_idx = lax.top_k(g_prob, 1)
    e_logits = (t @ we_r + be_r).astype(jnp.float32).reshape(-1, N_EXPERT_GROUPS, EXPERTS_PER_GROUP)
    e_sel = jnp.take_along_axis(e_logits, g_idx[:, :, None], axis=1)[:, 0]
    e_prob = jax.nn.softmax(e_sel, axis=-1)
    e_val, e_idx = lax.top_k(e_prob, TOP_K_INNER)
    e_val = e_val / jnp.sum(e_val, axis=-1, keepdims=True)
    weights = g_val * e_val
    expert_id = g_idx * EXPERTS_PER_GROUP + e_idx
    combine = jnp.sum(jax.nn.one_hot(expert_id, N_EXPERTS, dtype=jnp.float32)
                      * weights[..., None], axis=1)
    y = jnp.zeros(t.shape, jnp.float32)
    for e in range(N_EXPERTS):
        hid = jax.nn.silu(t @ w_gate[e]) * (t @ w_up[e])
        y = y + combine[:, e:e + 1] * (hid @ w_down[e]).astype(jnp.float32)
    return y.astype(h.dtype).reshape(b, s, d)


def setup_inputs(seed: int = 0) -> dict:
    key = jax.random.key(seed)
    ks = jax.random.split(key, 32)
    L, D = DEPTH, D_MODEL
    nrm = lambda k, shape, sc: jax.random.normal(k, shape, jnp.float32) * sc
    return {
        "x": nrm(ks[0], (BATCH, SEQ, D), 1.0),
        "c": nrm(ks[1], (BATCH, D), 1.0),
        "rel_bias": nrm(ks[2], (N_BUCKETS, N_HEADS_A), 0.5),
        "ada_w": nrm(ks[3], (L, D, 6 * D), 0.5 * D ** -0.5),
        "ada_b": nrm(ks[4], (L, 6 * D), 0.02),
        "norm1_g": 1.0 + nrm(ks[5], (L, D), 0.02),
        "w_in": nrm(ks[6], (L, D, D_IN), D ** -0.5),
        "q_norm_g": 1.0 + nrm(ks[7], (L, HEAD_DIM), 0.02),
        "k_norm_g": 1.0 + nrm(ks[8], (L, HEAD_DIM), 0.02),
        "lambda_q1": nrm(ks[9], (L, HEAD_DIM), 0.1),
        "lambda_k1": nrm(ks[10], (L, HEAD_DIM), 0.1),
        "lambda_q2": nrm(ks[11], (L, HEAD_DIM), 0.1),
        "lambda_k2": nrm(ks[12], (L, HEAD_DIM), 0.1),
        "subln_g": 1.0 + nrm(ks[13], (L, 2 * HEAD_DIM), 0.02),
        "w_branch_attn": nrm(ks[14], (L, D_ATTN, D), D_ATTN ** -0.5),
        "pool_w": nrm(ks[15], (L, N_POOL_GROUPS, POOL_GROUP_DIM, POOL_GROUP_DIM), POOL_GROUP_DIM ** -0.5),
        "pool_scale": 1.0 + nrm(ks[16], (L, D_POOL), 0.1),
        "w_branch_pool": nrm(ks[17], (L, D_POOL, D), D_POOL ** -0.5),
        "w_out": nrm(ks[18], (L, D, D), D ** -0.5),
        "norm2_g": 1.0 + nrm(ks[19], (L, D), 0.02),
        "router_group_w": nrm(ks[20], (L, D, N_EXPERT_GROUPS), D ** -0.5),
        "router_group_b": nrm(ks[21], (L, N_EXPERT_GROUPS), 0.01),
        "router_expert_w": nrm(ks[22], (L, D, N_EXPERTS), D ** -0.5),
        "router_expert_b": nrm(ks[23], (L, N_EXPERTS), 0.01),
        "expert_w_gate": nrm(ks[24], (L, N_EXPERTS, D, D_EXPERT), D ** -0.5),
        "expert_w_up": nrm(ks[25], (L, N_EXPERTS, D, D_EXPERT), D ** -0.5),
        "expert_w_down": nrm(ks[26], (L, N_EXPERTS, D_EXPERT, D), D_EXPERT ** -0.5),
    }


def reference(x, c, rel_bias, ada_w, ada_b, norm1_g, w_in, q_norm_g, k_norm_g,
              lambda_q1, lambda_k1, lambda_q2, lambda_k2, subln_g, w_branch_attn,
              pool_w, pool_scale, w_branch_pool, w_out, norm2_g,
              router_group_w, router_group_b, router_expert_w, router_expert_b,
              expert_w_gate, expert_w_up, expert_w_down):
    b, s, d = x.shape
    f32 = jnp.float32
    split_pts = [D_ATTN, 2 * D_ATTN, 3 * D_ATTN, 3 * D_ATTN + D_POOL, 3 * D_ATTN + D_POOL + D_MODEL]
    for l in range(DEPTH):
        mod = jax.nn.silu(c) @ ada_w[l] + ada_b[l]
        shift1, scale1, gate1, shift2, scale2, gate2 = jnp.split(mod[:, None, :], 6, axis=-1)

        h = rms_norm(x, norm1_g[l]) * (1 + scale1) + shift1
        proj = h @ w_in[l]
        q, k, v, u, g_attn, g_pool = jnp.split(proj, split_pts, axis=-1)

        q = rms_norm(q.reshape(b, s, N_HEADS_A, 2, HEAD_DIM), q_norm_g[l])
        k = rms_norm(k.reshape(b, s, N_HEADS_A, 2, HEAD_DIM), k_norm_g[l])
        v = v.reshape(b, s, N_HEADS_A, 2 * HEAD_DIM)
        lambda_init = 0.8 - 0.6 * math.exp(-0.3 * l)
        lam = (jnp.exp(jnp.sum(lambda_q1[l].astype(f32) * lambda_k1[l].astype(f32)))
               - jnp.exp(jnp.sum(lambda_q2[l].astype(f32) * lambda_k2[l].astype(f32)))
               + lambda_init)
        o = diff_attention(q, k, v, rel_bias, lam)
        o = rms_norm(o, subln_g[l]) * (1.0 - lambda_init)
        y_attn = o.reshape(b, s, D_ATTN) @ w_branch_attn[l]

        y_pool = multiscale_pool(u, pool_w[l], pool_scale[l]) @ w_branch_pool[l]

        merged = jax.nn.sigmoid(g_attn) * y_attn + jax.nn.sigmoid(g_pool) * y_pool
        x = x + gate1 * (merged @ w_out[l])

        h2 = rms_norm(x, norm2_g[l]) * (1 + scale2) + shift2
        x = x + gate2 * hierarchical_moe(h2, router_group_w[l], router_group_b[l],
                                         router_expert_w[l], router_expert_b[l],
                                         expert_w_gate[l], expert_w_up[l], expert_w_down[l])
    return x
```

```python
import math
from contextlib import ExitStack

import numpy as np
import concourse.bass as bass
import concourse.mybir as mybir
from concourse.bass_utils import run_bass_kernel_spmd

F32 = mybir.dt.float32
BF16 = mybir.dt.bfloat16
AF = mybir.ActivationFunctionType
ALU = mybir.AluOpType

D = 1024
NEG = -30000.0
EPS = 1e-6
NE = 16
DE = 512


class Tok:
    __slots__ = ("w", "rd")

    def __init__(self):
        self.w = None
        self.rd = {}


class Sched:
    STRICT_SAME = True

    def __init__(self, nc, stack):
        self.nc = nc
        self.stack = stack
        self.engs = {"pe": nc.tensor, "act": nc.scalar, "dve": nc.vector,
                     "pool": nc.gpsimd, "sp": nc.sync}
        self.sem = {k: stack.enter_context(nc.semaphore("sem_" + k)) for k in self.engs}
        self.cnt = {k: 0 for k in self.engs}
        self.seen = {k: {} for k in self.engs}
        self.dma_sems = {}
        self.dma_cnt = {}
        self.issuer = {}

    def _wait(self, e, kind, key, val):
        if kind == "eng":
            if key == e and not (self.STRICT_SAME and e in ("act", "dve", "pool")):
                return
            sem = self.sem[key]
        else:
            sem = self.dma_sems[key]
            val = self.dma_cnt[key]
        k = (kind, key)
        if self.seen[e].get(k, 0) >= val:
            return
        self.engs[e].wait_ge(sem, val)
        self.seen[e][k] = val

    def _deps(self, e, reads, writes):
        need = {}
        for b in reads:
            if b.w is not None:
                k = (b.w[0], b.w[1])
                need[k] = max(need.get(k, 0), b.w[2])
        for b in writes:
            if b.w is not None:
                k = (b.w[0], b.w[1])
                need[k] = max(need.get(k, 0), b.w[2])
            for k, v in b.rd.items():
                need[k] = max(need.get(k, 0), v)
        for (kind, key), val in need.items():
            self._wait(e, kind, key, val)

    def _mark(self, me, reads, writes):
        k = (me[0], me[1])
        for b in reads:
            b.rd[k] = max(b.rd.get(k, 0), me[2])
        for b in writes:
            b.w = me
            b.rd = {}

    def op(self, e, fn, reads=(), writes=()):
        self._deps(e, reads, writes)
        inst = fn(self.engs[e])
        self.cnt[e] += 1
        inst.then_inc(self.sem[e], 1)
        self._mark(("eng", e, self.cnt[e]), reads, writes)

    def dma(self, q, out, in_, sem, reads=(), writes=(), **kw):
        if sem not in self.dma_sems:
            self.dma_sems[sem] = self.stack.enter_context(self.nc.semaphore("dsem_" + sem))
            self.dma_cnt[sem] = 0
        self.issuer[sem] = q
        self._deps(q, reads, writes)
        inst = self.engs[q].dma_start(out=out, in_=in_, **kw)
        inst.then_inc(self.dma_sems[sem], 16)
        self.dma_cnt[sem] += 16
        self._mark(("dma", sem, self.dma_cnt[sem]), reads, writes)

    def indirect(self, sem, reads, writes, **kw):
        q = "pool"
        if sem not in self.dma_sems:
            self.dma_sems[sem] = self.stack.enter_context(self.nc.semaphore("dsem_" + sem))
            self.dma_cnt[sem] = 0
        self.issuer[sem] = q
        self._deps(q, reads, writes)
        inst = self.nc.gpsimd.indirect_dma_start(**kw)
        inst.then_inc(self.dma_sems[sem], 16)
        self.dma_cnt[sem] += 16
        self._mark(("dma", sem, self.dma_cnt[sem]), reads, writes)

    def cond_region(self, regs, thr, body):
        import copy
        snap_cnt = dict(self.cnt)
        snap_d = dict(self.dma_cnt)
        snap_seen = copy.deepcopy(self.seen)
        with self.nc.If_cmp(regs, thr, "IS_GT"):
            body()
        with self.nc.Else():
            for e in self.engs:
                d = self.cnt[e] - snap_cnt[e]
                if d:
                    if snap_cnt[e] > 0:
                        self.engs[e].wait_ge(self.sem[e], snap_cnt[e])
                    self.engs[e].sem_inc(self.sem[e], d)
            for sname, total in self.dma_cnt.items():
                dd = total - snap_d.get(sname, 0)
                if dd:
                    q = self.engs[self.issuer[sname]]
                    if snap_d.get(sname, 0) > 0:
                        q.wait_ge(self.dma_sems[sname], snap_d[sname])
                    q.sem_inc(self.dma_sems[sname], dd)
        self.seen = snap_seen

    def barrier(self):
        for e in self.engs:
            for o in self.engs:
                if o != e and self.cnt[o] > 0:
                    self._wait(e, "eng", o, self.cnt[o])
            for s in self.dma_sems:
                if self.dma_cnt[s] > 0:
                    self._wait(e, "dma", s, self.dma_cnt[s])


def _t5_bucket(rel):
    nb, max_exact = 16, 8
    bucket = np.where(rel > 0, nb, 0)
    n = np.abs(rel)
    n_f = np.maximum(n, max_exact).astype(np.float32)
    large = max_exact + (np.log(n_f / np.float32(max_exact)) / np.float32(math.log(128 / max_exact))
                         * np.float32(nb - max_exact)).astype(np.int32)
    large = np.minimum(large, nb - 1)
    return bucket + np.where(n < max_exact, n, large)


def _core_tables(r):
    oh = np.zeros((32, 16, 256), np.float32)
    mask = np.zeros((128, 16, 128), np.float32)
    n = np.arange(256)
    for m in range(8):
        for s in range(2):
            qb = r if s == 0 else 7 - r
            delta = m - qb
            t = m * 2 + s
            if delta > 0:
                oh[15, t, :] = 1.0
                mask[:, t, :] = NEG
            else:
                rel = 128 * delta + 127 - n
                b = _t5_bucket(rel.astype(np.int32))
                oh[b, t, n] = 1.0
                if delta == 0:
                    mask[0:64, t, 0:64] = NEG
    return oh.reshape(32, 4096), mask.reshape(128, 2048)


def _pool_tables(blocks):
    ns = len(blocks)
    hv = np.ones((128, ns, 16), np.float32)
    ic = np.zeros((128, 4, ns, 16), np.float32)
    for si, blk in enumerate(blocks):
        if blk == 0:
            hv[:, si, :] = 0.0
        for g, w in enumerate((2, 4, 8, 16)):
            t = blk * 128 + np.arange(16)
            ic[:, g, si, :] = 1.0 / np.minimum(t + 1, w).astype(np.float32)
    return hv.reshape(128, ns * 16), ic.reshape(128, 4 * ns * 16)


def build_program(S_LEN, dbg=False):
    NP = S_LEN // 1024
    NSLOT = 2 * NP
    NOWN = NSLOT * 128
    NKB = S_LEN // 128
    TG = min(512, NOWN)
    NTG = NOWN // TG
    TPG = TG // 128

    nc = bass.Bass("TRN2", target_bir_lowering=False)

    def din(name, shape, dt=F32):
        return nc.dram_tensor(name, list(shape), dt, kind="ExternalInput").ap()

    x_own = din("x_own", [NOWN, D])
    x_halo = din("x_halo", [NSLOT * 16, D])
    x_kv = din("x_kv", [S_LEN, D])
    c_row = din("c_row", [1, D])
    rel_bias = din("rel_bias", [32, 4])
    ada_w = din("ada_w", [D, 6 * D])
    ada_b = din("ada_b", [1, 6 * D])
    norm1_g = din("norm1_g", [1, D])
    w_in = din("w_in", [D, 4096])
    q_norm_g = din("q_norm_g", [1, 64])
    k_norm_g = din("k_norm_g", [1, 64])
    lam_in = din("lam_in", [1, 256])
    subln_g = din("subln_g", [1, 128])
    w_ba = din("w_ba", [512, D])
    pool_w = din("pool_w", [4, 128, 128])
    pool_scale = din("pool_scale", [1, 512])
    w_bb = din("w_bb", [512, D])
    w_out = din("w_out", [D, D])
    norm2_g = din("norm2_g", [1, D])
    r_w = din("r_w", [D, 20])
    r_b = din("r_b", [1, 20])
    e_wg = din("e_wg", [NE, D, DE])
    e_wu = din("e_wu", [NE, D, DE])
    e_wd = din("e_wd", [NE, DE, D])
    ident_in = din("ident_in", [128, 128])
    bones_in = din("bones_in", [128, 128])
    oh_in = din("oh_in", [32, 4096])
    mask_in = din("mask_in", [128, 2048])
    hv_in = din("hv_in", [128, NSLOT * 16])
    ic_in = din("ic_in", [128, 4 * NSLOT * 16])
    ustrict_in = din("ustrict_in", [128, 128])
    ebase_in = din("ebase_in", [128, 17])

    out_own = nc.dram_tensor("out_own", [NOWN, D], F32, kind="ExternalOutput").ap()

    KTd = nc.dram_tensor("KTd", [4, 128, S_LEN], BF16).ap()
    Vd = nc.dram_tensor("Vd", [4, 128, NKB * 129], BF16).ap()
    CAPR = NOWN + 128
    Xs = nc.dram_tensor("Xs", [NE * CAPR, D], BF16).ap()
    Ys = nc.dram_tensor("Ys", [NE * CAPR, D], F32).ap()
    modd = nc.dram_tensor("modd", [2, D], F32).ap()
    Gd_t = nc.dram_tensor("Gd", [4, 4096], F32)
    Gd = Gd_t.ap()

    dbg_outs = {}

    with ExitStack() as top:
        S = Sched(nc, top)
        blk = top.enter_context(nc.Block())

        def sbuf(st, name, shape, dt):
            return st.enter_context(nc.sbuf_tensor(name, list(shape), dt))

        banks = [top.enter_context(nc.psum_tensor(f"pb{i}", [128, 512], F32)) for i in range(8)]
        Tb = [Tok() for _ in range(8)]

        def bank_bf(i):
            return banks[i].bitcast(BF16)

        ident_f = sbuf(top, "ident_f", [128, 128], F32)
        ident_b = sbuf(top, "ident_b", [128, 128], BF16)
        bones_b = sbuf(top, "bones_b", [128, 128], BF16)
        ones_row = sbuf(top, "ones_row", [1, 128], F32)
        eps_t = sbuf(top, "eps_t", [128, 1], F32)
        modT = sbuf(top, "modT", [128, 32], F32)
        gs1 = sbuf(top, "gs1", [128, 8], F32)
        gs2 = sbuf(top, "gs2", [128, 8], F32)
        gate1_bc = sbuf(top, "gate1_bc", [128, D], F32)
        gate2_bc = sbuf(top, "gate2_bc", [128, D], F32)
        gq8 = sbuf(top, "gq8", [128, 1], F32)
        gk = sbuf(top, "gk", [128, 1], F32)
        neglam = sbuf(top, "neglam", [128, 1], F32)
        ch_bc = sbuf(top, "ch_bc", [128, 4], F32)
        subg = sbuf(top, "subg", [128, 1], F32)
        pscale = sbuf(top, "pscale", [128, 4], F32)
        rbias = sbuf(top, "rbias", [128, 20], F32)
        wr_f = sbuf(top, "wr_f", [128, 8, 20], F32)
        maskc = sbuf(top, "maskc", [128, 16, 128], F32)
        hv_t = sbuf(top, "hv_t", [128, NSLOT, 16], F32)
        ic_t = sbuf(top, "ic_t", [128, 4, NSLOT, 16], F32)
        ustrict_b = sbuf(top, "ustrict_b", [128, 128], BF16)
        ones_b = sbuf(top, "ones_b", [128, 128], BF16)
        ebase = sbuf(top, "ebase", [128, 17], F32)
        T_const = Tok()
        T_mod = Tok()
        T_modd = Tok()

        ARENA_F = 16896
        arena = sbuf(top, "arena", [128, ARENA_F], F32)
        arena_bf = arena.bitcast(BF16)
        x1 = arena[:, 0:NSLOT * D].rearrange("p (s d) -> p s d", d=D)
        T_x1 = [Tok() for _ in range(NSLOT)]

        def _body():
            with ExitStack() as pa:
                ld = lambda out, in_, **kw: S.dma("sp", out, in_, "cst", writes=[T_const], **kw)
                bones_f = sbuf(pa, "bones_f", [128, 128], F32)
                cT = sbuf(pa, "cT", [128, 8], F32)
                scT = sbuf(pa, "scT", [128, 8], F32)
                g1T = sbuf(pa, "g1T", [128, 8], F32)
                g2T = sbuf(pa, "g2T", [128, 8], F32)
                adab = sbuf(pa, "adab", [1, 6 * D], F32)
                modrow = sbuf(pa, "modrow", [1, 6 * D], F32)
                lamrow = sbuf(pa, "lamrow", [1, 256], F32)
                lamtmp = sbuf(pa, "lamtmp", [1, 128], F32)
                lam2 = sbuf(pa, "lam2", [1, 4], F32)
                one11 = sbuf(pa, "one11", [1, 1], F32)
                rb_t = sbuf(pa, "rb_t", [32, 4], F32)
                oh_t = sbuf(pa, "oh_t", [32, 4096], F32)
                Gs = sbuf(pa, "Gs", [4, 4096], F32)
                adaw = [arena_bf[:, i * 4096:(i + 1) * 4096].rearrange("p (c n) -> p c n", c=8) for i in range(3)]
                scTb = sbuf(pa, "scTb", [128, 8], BF16)
                T_adaw = [Tok() for _ in range(3)]
                T_tmp = Tok()
                T_row = Tok()

                ustrict_f = sbuf(pa, "ustrict_f", [128, 128], F32)
                g2row = sbuf(pa, "g2row", [1, D], F32)
                gs2row = sbuf(pa, "gs2row", [1, D], F32)
                ld(ident_f[:], ident_in)
                ld(ustrict_f[:], ustrict_in)
                ld(g2row[:], norm2_g)
                ld(ebase[:], ebase_in)
                ld(bones_f[:], bones_in)
                ld(cT[:], c_row.rearrange("o (c p) -> p (o c)", p=128))
                ld(g1T[:], norm1_g.rearrange("o (c p) -> p (o c)", p=128))
                ld(g2T[:], norm2_g.rearrange("o (c p) -> p (o c)", p=128))
                ld(adab[:], ada_b)
                ld(lamrow[:], lam_in)
                ld(rb_t[:], rel_bias)
                ld(oh_t[:], oh_in)
                ld(gq8[0:64, :], q_norm_g.rearrange("o d -> d o"))
                ld(gq8[64:128, :], q_norm_g.rearrange("o d -> d o"))
                ld(gk[0:64, :], k_norm_g.rearrange("o d -> d o"))
                ld(gk[64:128, :], k_norm_g.rearrange("o d -> d o"))
                ld(subg[:], subln_g.rearrange("o d -> d o"))
                ld(pscale[:], pool_scale.rearrange("o (g p) -> p (o g)", p=128))
                ld(ch_bc[:], rel_bias[15:16, :].broadcast_to([128, 4]))
                ld(rbias[:], r_b.broadcast_to([128, 20]))
                ld(wr_f[:], r_w.rearrange("(c p) n -> p c n", p=128))
                ld(maskc[:], mask_in.rearrange("p (t q) -> p t q", q=128))
                ld(hv_t[:], hv_in.rearrange("p (s t) -> p s t", t=16))
                ld(ic_t[:], ic_in.rearrange("p (g s t) -> p g s t", g=4, t=16))

                S.op("dve", lambda e: e.memset(ones_row[:], 1.0), writes=[T_const])
                S.op("dve", lambda e: e.memset(one11[:], 1.0), writes=[T_const])
                S.op("dve", lambda e: e.memset(eps_t[:], EPS), writes=[T_const])
                S.op("dve", lambda e: e.tensor_copy(out=ident_b[:], in_=ident_f[:]), reads=[T_const], writes=[T_const])
                S.op("dve", lambda e: e.tensor_copy(out=bones_b[:], in_=bones_f[:]), reads=[T_const], writes=[T_const])
                S.op("dve", lambda e: e.tensor_copy(out=ustrict_b[:], in_=ustrict_f[:]), reads=[T_const], writes=[T_const])
                S.op("dve", lambda e: e.memset(ones_b[:], 1.0), writes=[T_const])
                S.op("dve", lambda e: e.tensor_scalar(out=gq8[:], in0=gq8[:], scalar1=0.125, scalar2=None, op0=ALU.mult),
                     reads=[T_const], writes=[T_const])
                S.op("dve", lambda e: e.tensor_scalar(out=subg[:], in0=subg[:], scalar1=0.8, scalar2=None, op0=ALU.mult),
                     reads=[T_const], writes=[T_const])
                S.op("act", lambda e: e.activation(out=scT[:], in_=cT[:], func=AF.Silu), reads=[T_const], writes=[T_tmp])
                S.op("dve", lambda e: e.tensor_copy(out=scTb[:], in_=scT[:]), reads=[T_tmp], writes=[T_tmp])

                adaw_v = ada_w.rearrange("(c p) n -> p c n", p=128)
                NPIECE = 12
                for i in range(min(3, NPIECE)):
                    S.dma("pool", adaw[i], adaw_v[:, :, i * 512:(i + 1) * 512], f"adaw{i}", writes=[T_adaw[i]])
                for i in range(NPIECE):
                    bi = i % 3
                    pb = 0 + (i % 2)

                    def mm(e, bi=bi, pb=pb):
                        for kc in range(8):
                            ins = e.matmul(banks[pb][0:1, :], lhsT=scTb[:, kc:kc + 1], rhs=adaw[bi][:, kc, :],
                                           start=(kc == 0), stop=(kc == 7))
                        return ins
                    S.op("pe", mm, reads=[T_tmp, T_adaw[bi]], writes=[Tb[pb]])
                    S.op("dve", lambda e, i=i, pb=pb: e.tensor_tensor(out=modrow[:, i * 512:(i + 1) * 512], in0=banks[pb][0:1, :],
                                                                      in1=adab[:, i * 512:(i + 1) * 512], op=ALU.add),
                         reads=[Tb[pb], T_const], writes=[T_row])
                    if i + 3 < NPIECE:
                        S.dma("pool", adaw[bi], adaw_v[:, :, (i + 3) * 512:(i + 4) * 512], f"adaw{bi}", writes=[T_adaw[bi]])

                def mmT(e):
                    for vi, v in enumerate((0, 1, 3, 4)):
                        for kc in range(8):
                            ins = e.matmul(banks[2][:, vi * 8 + kc: vi * 8 + kc + 1],
                                           lhsT=modrow[0:1, v * D + kc * 128: v * D + (kc + 1) * 128],
                                           rhs=one11[:], start=True, stop=True)
                    return ins
                S.op("pe", mmT, reads=[T_row, T_const], writes=[Tb[2]])
                S.op("dve", lambda e: e.tensor_copy(out=modT[:], in_=banks[2][:, 0:32]), reads=[Tb[2]], writes=[T_mod])
                S.op("dve", lambda e: e.scalar_tensor_tensor(out=gs1[:], in0=modT[:, 8:16], scalar=1.0, in1=g1T[:],
                                                             op0=ALU.add, op1=ALU.mult), reads=[T_mod, T_const], writes=[T_mod])
                S.op("dve", lambda e: e.scalar_tensor_tensor(out=gs2[:], in0=modT[:, 24:32], scalar=1.0, in1=g2T[:],
                                                             op0=ALU.add, op1=ALU.mult), reads=[T_mod, T_const], writes=[T_mod])
                for gi, (v, dst) in enumerate(((2, gate1_bc), (5, gate2_bc))):
                    for half in range(2):
                        pb = 3 + half
                        S.op("pe", lambda e, v=v, half=half, pb=pb: e.matmul(
                            banks[pb][:], lhsT=ones_row[:], rhs=modrow[0:1, v * D + half * 512: v * D + (half + 1) * 512],
                            start=True, stop=True), reads=[T_row, T_const], writes=[Tb[pb]])
                        S.op("act", lambda e, dst=dst, half=half, pb=pb: e.copy(out=dst[:, half * 512:(half + 1) * 512], in_=banks[pb][:]),
                             reads=[Tb[pb]], writes=[T_mod])
                S.op("dve", lambda e: e.scalar_tensor_tensor(out=gs2row[:], in0=modrow[0:1, 4 * D:5 * D], scalar=1.0, in1=g2row[:],
                                                             op0=ALU.add, op1=ALU.mult), reads=[T_row, T_const], writes=[T_tmp])
                S.dma("sp", modd[0:1, :], gs2row[:], "modd", reads=[T_tmp], writes=[T_modd])
                S.dma("sp", modd[1:2, :], modrow[0:1, 3 * D:4 * D], "modd", reads=[T_row], writes=[T_modd])
                S.op("dve", lambda e: e.tensor_tensor(out=lamtmp[:].rearrange("o (a d) -> o a d", a=2),
                                                      in0=lamrow[:].rearrange("o (a t d) -> o a t d", a=2, t=2)[:, :, 0, :],
                                                      in1=lamrow[:].rearrange("o (a t d) -> o a t d", a=2, t=2)[:, :, 1, :],
                                                      op=ALU.mult), reads=[T_const], writes=[T_tmp])
                S.op("dve", lambda e: e.reduce_sum(out=lam2[:, 0:2], in_=lamtmp[:].rearrange("o (a d) -> o a d", a=2),
                                                   axis=mybir.AxisListType.X), reads=[T_tmp], writes=[T_tmp])
                S.op("act", lambda e: e.activation(out=lam2[:, 0:2], in_=lam2[:, 0:2], func=AF.Exp), reads=[T_tmp], writes=[T_tmp])
                S.op("dve", lambda e: e.tensor_tensor(out=lam2[:, 2:3], in0=lam2[:, 1:2], in1=lam2[:, 0:1], op=ALU.subtract),
                     reads=[T_tmp], writes=[T_tmp])
                S.op("dve", lambda e: e.tensor_scalar(out=lam2[:, 3:4], in0=lam2[:, 2:3], scalar1=-0.2, scalar2=None, op0=ALU.add),
                     reads=[T_tmp], writes=[T_tmp])
                S.op("pe", lambda e: e.matmul(banks[5][:, 0:1], lhsT=ones_row[:], rhs=lam2[:, 3:4], start=True, stop=True),
                     reads=[T_tmp, T_const], writes=[Tb[5]])
                S.op("dve", lambda e: e.tensor_copy(out=neglam[:], in_=banks[5][:, 0:1]), reads=[Tb[5]], writes=[T_const])
                for ci in range(8):
                    pb = 6 + (ci % 2)
                    S.op("pe", lambda e, ci=ci, pb=pb: e.matmul(banks[pb][0:4, :], lhsT=rb_t[:], rhs=oh_t[:, ci * 512:(ci + 1) * 512],
                                                               start=True, stop=True), reads=[T_const], writes=[Tb[pb]])
                    S.op("dve", lambda e, ci=ci, pb=pb: e.tensor_copy(out=Gs[:, ci * 512:(ci + 1) * 512], in_=banks[pb][0:4, :]),
                         reads=[Tb[pb]], writes=[T_tmp])
                T_G = Tok()
                S.dma("sp", Gd, Gs[:], "gd", reads=[T_tmp], writes=[T_G])
                S.barrier()

            def norm_tiles(st, tag, src_rows, ntiles, rows_per_tile, dst_fn, dst_tok_fn, after_tile=None):
                LA1 = 2
                NB = LA1 + 2
                NX = LA1 + 1
                xt = [sbuf(st, f"{tag}_x{i}", [128, D], F32) for i in range(NB)]
                xh = [sbuf(st, f"{tag}_xh{i}", [128, D], BF16) for i in range(NX)]
                junk = sbuf(st, f"{tag}_junk", [128, D], BF16)
                ss = [sbuf(st, f"{tag}_ss{i}", [128, 1], F32) for i in range(NX)]
                T_x = [Tok() for _ in range(NB)]
                T_xh = [Tok() for _ in range(NX)]
                T_junk = Tok()
                T_ss = [Tok() for _ in range(NX)]
                R = rows_per_tile
                for t in range(min(NB - 1, ntiles)):
                    S.dma("sp", xt[t % NB][0:R, :], src_rows(t), f"{tag}_x{t % NB}", writes=[T_x[t % NB]])
                def stage1(t):
                    b3, b2 = t % NB, t % NX
                    S.op("act", lambda e, b3=b3, b2=b2: e.activation(out=junk[0:R, :], in_=xt[b3][0:R, :], func=AF.Square,
                                                                     accum_out=ss[b2][0:R, :]),
                         reads=[T_x[b3]], writes=[T_junk, T_ss[b2]])
                    S.op("act", lambda e, b2=b2: e.activation(out=ss[b2][0:R, :], in_=ss[b2][0:R, :], func=AF.Sqrt,
                                                              bias=eps_t[0:R, :], scale=1.0 / D),
                         reads=[T_ss[b2], T_const], writes=[T_ss[b2]])
                    S.op("dve", lambda e, b2=b2: e.reciprocal(out=ss[b2][0:R, :], in_=ss[b2][0:R, :]),
                         reads=[T_ss[b2]], writes=[T_ss[b2]])
                    S.op("dve", lambda e, b3=b3, b2=b2: e.tensor_scalar(out=xh[b2][0:R, :], in0=xt[b3][0:R, :], scalar1=ss[b2][0:R, 0:1],
                                                                        scalar2=None, op0=ALU.mult),
                         reads=[T_x[b3], T_ss[b2]], writes=[T_xh[b2]])

                def stage2(t):
                    b2 = t % NX
                    pb = t % 2
                    pbf = bank_bf(pb)

                    def tr(e, b2=b2, pbf=pbf):
                        for kc in range(8):
                            ins = e.transpose(out=pbf[:, kc * 128: kc * 128 + R], in_=xh[b2][0:R, kc * 128:(kc + 1) * 128],
                                              identity=ident_b[0:R, 0:R])
                        return ins
                    S.op("pe", tr, reads=[T_xh[b2], T_const], writes=[Tb[pb]])
                    for kc in range(8):
                        eng = "dve" if kc % 4 == 3 else "act"
                        if eng == "act":
                            S.op("act", lambda e, kc=kc, t=t, pbf=pbf: e.activation(
                                out=dst_fn(t, kc), in_=pbf[:, kc * 128: kc * 128 + R], func=AF.Identity,
                                bias=modT[:, kc:kc + 1], scale=gs1[:, kc:kc + 1]),
                                reads=[Tb[pb], T_mod], writes=[dst_tok_fn(t)])
                        else:
                            S.op("dve", lambda e, kc=kc, t=t, pbf=pbf: e.tensor_scalar(
                                out=dst_fn(t, kc), in0=pbf[:, kc * 128: kc * 128 + R], scalar1=gs1[:, kc:kc + 1],
                                scalar2=modT[:, kc:kc + 1], op0=ALU.mult, op1=ALU.add),
                                reads=[Tb[pb], T_mod], writes=[dst_tok_fn(t)])

                for t in range(min(LA1, ntiles)):
                    stage1(t)
                for t in range(ntiles):
                    if t + NB - 1 < ntiles:
                        tn = t + NB - 1
                        S.dma("sp", xt[tn % NB][0:R, :], src_rows(tn), f"{tag}_x{tn % NB}", writes=[T_x[tn % NB]])
                    if t + LA1 < ntiles:
                        stage1(t + LA1)
                    stage2(t)
                    if after_tile is not None:
                        after_tile(t)

            def wslice(lo, hi):
                return w_in.rearrange("(c p) n -> p c n", p=128)[:, :, lo:hi]

            def qk_norm_sq(raw_bank, sq, T_sq, ncols):
                S.op("act", lambda e: e.activation(out=sq[:, 0:ncols], in_=banks[raw_bank][:, 0:ncols], func=AF.Square),
                     reads=[Tb[raw_bank]], writes=[T_sq])

            def qk_norm_rest(raw_bank, sq, T_sq, ssum_bank, rstd, T_rstd, ncols, gain, outs):
                S.op("pe", lambda e: e.matmul(banks[ssum_bank][:, 0:ncols], lhsT=bones_b[:], rhs=sq[:, 0:ncols], start=True, stop=True),
                     reads=[T_sq, T_const], writes=[Tb[ssum_bank]])
                S.op("act", lambda e: e.activation(out=rstd[:, 0:ncols], in_=banks[ssum_bank][:, 0:ncols], func=AF.Sqrt,
                                                   bias=eps_t[:], scale=1.0 / 64), reads=[Tb[ssum_bank], T_const], writes=[T_rstd])
                S.op("dve", lambda e: e.reciprocal(out=rstd[:, 0:ncols], in_=rstd[:, 0:ncols]), reads=[T_rstd], writes=[T_rstd])
                for dst, plo, phi, T_dst in outs:
                    S.op("dve", lambda e, dst=dst, plo=plo, phi=phi: e.scalar_tensor_tensor(
                        out=dst, in0=banks[raw_bank][plo:phi, 0:ncols], scalar=gain[plo:phi, 0:1], in1=rstd[plo:phi, 0:ncols],
                        op0=ALU.mult, op1=ALU.mult), reads=[Tb[raw_bank], T_rstd, T_const], writes=[T_dst])

            def qk_norm_group(st_tmp, raw_bank, T_raw, sq, T_sq, ssum_bank, rstd, T_rstd, ncols, gain, outs):
                qk_norm_sq(raw_bank, sq, T_sq, ncols)
                qk_norm_rest(raw_bank, sq, T_sq, ssum_bank, rstd, T_rstd, ncols, gain, outs)

            T_KTd = Tok()
            T_Vd = Tok()
            with ExitStack() as pbk:
                wk = sbuf(pbk, "wk", [128, 8, 512], BF16)
                wv = sbuf(pbk, "wv", [128, 8, 512], BF16)
                T_wkv = Tok()
                S.dma("pool", wk[:], wslice(512, 1024), "wkv", writes=[T_wkv])
                S.dma("pool", wv[:], wslice(1024, 1536), "wkv", writes=[T_wkv])
                hTg = [sbuf(pbk, f"hTg{i}", [128, 8, 512], BF16) for i in range(2)]
                T_hTg = [Tok() for _ in range(2)]
                kst = [sbuf(pbk, f"kst{i}", [128, 4, 512], BF16) for i in range(2)]
                T_kst = [Tok() for _ in range(2)]
                vst = [sbuf(pbk, f"vst{i}", [128, 4, 4, 129], BF16) for i in range(2)]
                T_vst = [Tok() for _ in range(2)]
                sqk = [sbuf(pbk, f"sqk{i}", [128, 512], BF16) for i in range(2)]
                T_sqk = [Tok() for _ in range(2)]
                rsk = [sbuf(pbk, f"rsk{i}", [128, 512], F32) for i in range(2)]
                T_rsk = [Tok() for _ in range(2)]
                for i in range(2):
                    S.op("pool", lambda e, i=i: e.memset(vst[i][:], 1.0), writes=[T_vst[i]])
                NG = S_LEN // 512

                NQ = NKB

                def item_A(q):
                    g, h = divmod(q, 4)
                    gb = g % 2
                    rb = 2 + (q % 3)

                    def mmk(e):
                        for kc in range(8):
                            ins = e.matmul(banks[rb][:], lhsT=wk[:, kc, h * 128:(h + 1) * 128], rhs=hTg[gb][:, kc, :],
                                           start=(kc == 0), stop=(kc == 7))
                        return ins
                    S.op("pe", mmk, reads=[T_wkv, T_hTg[gb]], writes=[Tb[rb]])
                    qk_norm_sq(rb, sqk[q % 2], T_sqk[q % 2], 512)
                    vb = 6 + (q % 2)

                    def mmv(e):
                        for kc in range(8):
                            ins = e.matmul(banks[vb][:], lhsT=hTg[gb][:, kc, h * 128:(h + 1) * 128], rhs=wv[:, kc, :],
                                           start=(kc == 0), stop=(kc == 7))
                        return ins
                    S.op("pe", mmv, reads=[T_wkv, T_hTg[gb]], writes=[Tb[vb]])
                    S.op("act", lambda e: e.copy(out=vst[gb][:, h, :, 0:128], in_=banks[vb][:].rearrange("p (h e) -> p h e", h=4)),
                         reads=[Tb[vb]], writes=[T_vst[gb]])

                def item_B(q):
                    sq, T_sq, rstd, T_rstd = sqk[q % 2], T_sqk[q % 2], rsk[q % 2], T_rsk[q % 2]
                    S.op("pe", lambda e: e.matmul(banks[5][:], lhsT=bones_b[:], rhs=sq[:], start=True, stop=True),
                         reads=[T_sq, T_const], writes=[Tb[5]])
                    S.op("act", lambda e: e.activation(out=rstd[:], in_=banks[5][:], func=AF.Sqrt, bias=eps_t[:], scale=1.0 / 64),
                         reads=[Tb[5], T_const], writes=[T_rstd])

                def item_C(q):
                    g, h = divmod(q, 4)
                    gb = g % 2
                    rb = 2 + (q % 3)
                    rstd, T_rstd = rsk[q % 2], T_rsk[q % 2]
                    S.op("dve", lambda e: e.reciprocal(out=rstd[:], in_=rstd[:]), reads=[T_rstd], writes=[T_rstd])
                    S.op("dve", lambda e: e.scalar_tensor_tensor(out=kst[gb][:, h, :], in0=banks[rb][:], scalar=gk[:, 0:1], in1=rstd[:],
                                                                 op0=ALU.mult, op1=ALU.mult), reads=[Tb[rb], T_rstd, T_const], writes=[T_kst[gb]])
                    if h == 3:
                        S.dma("pool", KTd[:, :, g * 512:(g + 1) * 512].rearrange("h p n -> p h n"), kst[gb][:], f"kst{gb}",
                              reads=[T_kst[gb]], writes=[T_KTd])
                        for hh in range(4):
                            S.dma("pool", Vd[hh, :, g * 516:(g + 1) * 516].rearrange("p (i e) -> p i e", e=129), vst[gb][:, :, hh, :], f"vst{gb}",
                                  reads=[T_vst[gb]], writes=[T_Vd])

                def after_tile(t):
                    if 0 <= t - 6 < NQ:
                        item_C(t - 6)
                    if 0 <= t - 5 < NQ:
                        item_B(t - 5)
                    if 0 <= t - 4 < NQ:
                        item_A(t - 4)

                norm_tiles(pbk, "kv", lambda t: x_kv[t * 128:(t + 1) * 128, :], NKB, 128,
                           lambda t, kc: hTg[(t // 4) % 2][:, kc, (t % 4) * 128:(t % 4 + 1) * 128],
                           lambda t: T_hTg[(t // 4) % 2], after_tile)
                for t in range(NKB, NKB + 7):
                    after_tile(t)
                S.barrier()

            with ExitStack() as pown:
                hT_own = sbuf(pown, "hT_own", [128, 8, NOWN], BF16)
                hT_halo = sbuf(pown, "hT_halo", [128, 8, NSLOT * 16], BF16)
                T_hTown = [Tok() for _ in range(NSLOT)]
                T_hThalo = Tok()
                oT = sbuf(pown, "oT", [128, 4, NOWN], BF16)
                T_oT = Tok()
                with ExitStack() as patt:
                    qT = sbuf(patt, "qT", [128, 4, NOWN], BF16)
                    T_qT = Tok()
                    with ExitStack() as pc:
                        wq = sbuf(pc, "wq", [128, 8, 512], BF16)
                        T_wq = Tok()
                        S.dma("pool", wq[:], wslice(0, 512), "wq", writes=[T_wq])
                        sqq = [sbuf(pc, f"sqq{i}", [128, 512], BF16) for i in range(2)]
                        T_sqq = [Tok() for _ in range(2)]
                        rsq = [sbuf(pc, f"rsq{i}", [128, 512], F32) for i in range(2)]
                        T_rsq = [Tok() for _ in range(2)]
                        with ExitStack() as pcn:
                            norm_tiles(pcn, "own", lambda t: x_own[t * 128:(t + 1) * 128, :], NSLOT, 128,
                                       lambda t, kc: hT_own[:, kc, t * 128:(t + 1) * 128], lambda t: T_hTown[t])
                        S.barrier()
                        NHT = (NSLOT * 16 + 127) // 128
                        for t in range(NHT):
                            rows = min(128, NSLOT * 16 - t * 128)
                            with ExitStack() as pcn:
                                norm_tiles(pcn, f"halo{t}", lambda tt, t=t, rows=rows: x_halo[t * 128: t * 128 + rows, :], 1, rows,
                                           lambda tt, kc, t=t, rows=rows: hT_halo[:, kc, t * 128: t * 128 + rows], lambda tt: T_hThalo)
                            S.barrier()
                        for tg in range(NTG):
                            for h in range(4):
                                rb = 2 + (h % 2)

                                def mmq(e, h=h, rb=rb, tg=tg):
                                    for kc in range(8):
                                        ins = e.matmul(banks[rb][:, 0:TG], lhsT=wq[:, kc, h * 128:(h + 1) * 128],
                                                       rhs=hT_own[:, kc, tg * TG:(tg + 1) * TG], start=(kc == 0), stop=(kc == 7))
                                    return ins
                                S.op("pe", mmq, reads=[T_wq] + T_hTown[tg * TPG:(tg + 1) * TPG], writes=[Tb[rb]])
                                qk_norm_group(None, rb, None, sqq[h % 2], T_sqq[h % 2], 4, rsq[h % 2], T_rsq[h % 2], TG, gq8,
                                              [(qT[:, h, tg * TG:(tg + 1) * TG], 0, 128, T_qT)])
                        S.barrier()

                    with ExitStack() as pat:
                        VW = NKB * 129
                        kt_sb = [arena_bf[:, i * S_LEN:(i + 1) * S_LEN] for i in range(2)]
                        v_sb = [arena_bf[:, 2 * S_LEN + i * VW: 2 * S_LEN + (i + 1) * VW].rearrange("p (k e) -> p k e", e=129) for i in range(2)]
                        T_kv = [Tok() for _ in range(2)]
                        qpad = [sbuf(pat, f"qpad{i}", [128, 2, NOWN], BF16) for i in range(2)]
                        T_qpad = [Tok() for _ in range(2)]
                        bT = [sbuf(pat, f"bT{i}", [128, 16, 128], F32) for i in range(2)]
                        T_bT = [Tok() for _ in range(2)]
                        NEB = 3
                        Eb = [sbuf(pat, f"Eb{i}", [128, 2, 256], BF16) for i in range(NEB)]
                        T_E = [Tok() for _ in range(NEB)]
                        tmpb = [sbuf(pat, f"tmpb{i}", [128, 2, 256], F32) for i in range(2)]
                        T_tmpb = [Tok() for _ in range(2)]
                        rs = [sbuf(pat, f"rs{i}", [128, 4], F32) for i in range(2)]
                        T_rs = [Tok() for _ in range(2)]
                        tO = [sbuf(pat, f"tO{i}", [128, 128], F32) for i in range(2)]
                        oO = [sbuf(pat, f"oO{i}", [128, 128], F32) for i in range(2)]
                        on = [sbuf(pat, f"on{i}", [128, 128], BF16) for i in range(2)]
                        jk = sbuf(pat, "att_jk", [128, 128], BF16)
                        ssq = [sbuf(pat, f"ssq{i}", [128, 1], F32) for i in range(2)]
                        T_post = [Tok() for _ in range(2)]
                        T_jk = Tok()
                        for i in range(2):
                            S.op("dve" if i == 0 else "pool", lambda e, i=i: e.memset(qpad[i][:], 0.0), writes=[T_qpad[i]])

                        def load_head(h):
                            hb = h % 2
                            NCH = max(1, S_LEN // 2048)
                            cw = S_LEN // NCH
                            for ci in range(NCH):
                                S.dma("sp", kt_sb[hb][:, ci * cw:(ci + 1) * cw], KTd[h, :, ci * cw:(ci + 1) * cw], f"kv{hb}",
                                      reads=[T_KTd], writes=[T_kv[hb]])
                            S.dma("sp", v_sb[hb].rearrange("p k e -> p (k e)"), Vd[h], f"kv{hb}", reads=[T_Vd], writes=[T_kv[hb]])
                            for t in range(16):
                                src = bass.AP(Gd_t, h * 4096 + t * 256, [[1, 128], [1, 128]])
                                S.dma("sp", bT[hb][:, t, :], src, f"bT{hb}", reads=[T_G], writes=[T_bT[hb]])
                            eng_ = "dve" if h == 0 else "pool"
                            S.op(eng_, lambda e, hb=hb: e.tensor_tensor(out=bT[hb][:], in0=bT[hb][:], in1=maskc[:], op=ALU.add),
                                 reads=[T_bT[hb], T_const], writes=[T_bT[hb]])
                            S.op(eng_, lambda e, hb=hb, h=h: e.tensor_copy(out=qpad[hb][0:64, 0, :], in_=qT[0:64, h, :]),
                                 reads=[T_qT], writes=[T_qpad[hb]])
                            S.op(eng_, lambda e, hb=hb, h=h: e.tensor_copy(out=qpad[hb][64:128, 1, :], in_=qT[64:128, h, :]),
                                 reads=[T_qT], writes=[T_qpad[hb]])

                        steps = []
                        unit = 0
                        for h in range(4):
                            for j in range(NP):
                                nkb = 8 * j + 8
                                ob = 3 + 2 * (unit % 2)
                                unit += 1
                                for kb in range(nkb):
                                    steps.append(dict(h=h, hb=h % 2, j=j, kb=kb, nkb=nkb, ob=ob, q0=256 * j))
                        nsteps = len(steps)
                        loaded = set()

                        def ensure_head(h):
                            if h < 4 and h not in loaded:
                                loaded.add(h)
                                load_head(h)

                        def emit_S(i):
                            st_ = steps[i]
                            h, hb, kb, q0 = st_["h"], st_["hb"], st_["kb"], st_["q0"]
                            ensure_head(h)
                            sbk = i % 3

                            def mms(e):
                                return e.matmul(banks[sbk][:].rearrange("p (m q) -> p m q", m=2), lhsT=kt_sb[hb][:, kb * 128:(kb + 1) * 128],
                                                rhs=qpad[hb][:, :, q0:q0 + 256], start=True, stop=True)
                            S.op("pe", mms, reads=[T_kv[hb], T_qpad[hb]], writes=[Tb[sbk]])

                        def emit_exp(i):
                            st_ = steps[i]
                            h, hb, kb, j = st_["h"], st_["hb"], st_["kb"], st_["j"]
                            sbk = i % 3
                            eb = i % NEB
                            Ev = Eb[eb][:].rearrange("p m q -> p (m q)")
                            if kb < 8 * j:
                                S.op("act", lambda e: e.activation(out=Ev, in_=banks[sbk][:], func=AF.Exp,
                                                                   bias=ch_bc[:, h:h + 1], scale=1.0),
                                     reads=[Tb[sbk], T_const], writes=[T_E[eb]])
                            else:
                                mi = kb - 8 * j
                                tb = i % 2
                                bias_ap = bT[hb][:, 2 * mi:2 * mi + 2, :].rearrange("p s q -> p (s q)").unsqueeze(1).broadcast_to([128, 2, 256])
                                S.op("dve", lambda e: e.scalar_tensor_tensor(
                                    out=tmpb[tb][:], in0=banks[sbk][:].rearrange("p (m q) -> p m q", m=2), scalar=1.0,
                                    in1=bias_ap, op0=ALU.mult, op1=ALU.add),
                                    reads=[Tb[sbk], T_bT[hb]], writes=[T_tmpb[tb]])
                                S.op("act", lambda e: e.activation(out=Ev, in_=tmpb[tb][:].rearrange("p m q -> p (m q)"), func=AF.Exp),
                                     reads=[T_tmpb[tb]], writes=[T_E[eb]])

                        def emit_PV(i):
                            st_ = steps[i]
                            hb, kb, nkb, ob = st_["hb"], st_["kb"], st_["nkb"], st_["ob"]
                            eb = i % NEB

                            def mmo(e):
                                for m in range(2):
                                    for s_ in range(2):
                                        ins = e.matmul(banks[ob + m][:, s_ * 129:(s_ + 1) * 129], lhsT=Eb[eb][:, m, s_ * 128:(s_ + 1) * 128],
                                                       rhs=v_sb[hb][:, kb, :], start=(kb == 0 and s_ == 0), stop=(kb == nkb - 1),
                                                       skip_group_check=True)
                                return ins
                            S.op("pe", mmo, reads=[T_E[eb], T_kv[hb]], writes=[Tb[ob], Tb[ob + 1]])

                        def post_A(h, j, ob):
                            for s_ in range(2):
                                pi = s_
                                c0 = s_ * 129
                                S.op("dve", lambda e, pi=pi, c0=c0: e.tensor_copy(out=rs[pi][:, 0:1], in_=banks[ob][:, c0 + 128: c0 + 129]),
                                     reads=[Tb[ob]], writes=[T_rs[pi]])
                                S.op("dve", lambda e, pi=pi, c0=c0: e.tensor_copy(out=rs[pi][:, 1:2], in_=banks[ob + 1][:, c0 + 128: c0 + 129]),
                                     reads=[Tb[ob + 1]], writes=[T_rs[pi]])
                                S.op("dve", lambda e, pi=pi: e.reciprocal(out=rs[pi][:, 0:2], in_=rs[pi][:, 0:2]), reads=[T_rs[pi]], writes=[T_rs[pi]])
                                S.op("dve", lambda e, pi=pi: e.tensor_tensor(out=rs[pi][:, 2:3], in0=rs[pi][:, 1:2], in1=neglam[:], op=ALU.mult),
                                     reads=[T_rs[pi], T_const], writes=[T_rs[pi]])
                                S.op("dve", lambda e, pi=pi, c0=c0: e.tensor_scalar(out=tO[pi][:], in0=banks[ob + 1][:, c0: c0 + 128],
                                                                                  scalar1=rs[pi][:, 2:3], scalar2=None, op0=ALU.mult),
                                     reads=[Tb[ob + 1], T_rs[pi]], writes=[T_post[pi]])
                                S.op("dve", lambda e, pi=pi, c0=c0: e.scalar_tensor_tensor(out=oO[pi][:], in0=banks[ob][:, c0: c0 + 128],
                                                                                         scalar=rs[pi][:, 0:1], in1=tO[pi][:],
                                                                                         op0=ALU.mult, op1=ALU.add),
                                     reads=[Tb[ob], T_rs[pi], T_post[pi]], writes=[T_post[pi]])
                                S.op("dve", lambda e, pi=pi: e.scalar_tensor_tensor(out=tO[pi][:], in0=oO[pi][:], scalar=1.0, in1=oO[pi][:],
                                                                                  op0=ALU.mult, op1=ALU.mult, accum_out=ssq[pi][:]),
                                     reads=[T_post[pi]], writes=[T_ssq[pi], T_tO2[pi]])

                        def post_B(h, j, ob):
                            for s_ in range(2):
                                pi = s_
                                S.op("act", lambda e, pi=pi: e.activation(out=ssq[pi][:], in_=ssq[pi][:], func=AF.Ln, bias=eps_t[:], scale=1.0 / 128),
                                     reads=[T_ssq[pi], T_const], writes=[T_ssq[pi]])
                                S.op("act", lambda e, pi=pi: e.activation(out=ssq[pi][:], in_=ssq[pi][:], func=AF.Exp, scale=-0.5),
                                     reads=[T_ssq[pi]], writes=[T_ssq[pi]])

                        def post_C(h, j, ob):
                            for s_ in range(2):
                                pi = s_
                                slot = 2 * j + s_
                                S.op("dve", lambda e, pi=pi: e.tensor_scalar(out=on[pi][:], in0=oO[pi][:], scalar1=ssq[pi][:, 0:1], scalar2=None, op0=ALU.mult),
                                     reads=[T_post[pi], T_ssq[pi]], writes=[T_on[pi]])
                                tbk = 7
                                tbf = bank_bf(tbk)
                                S.op("pe", lambda e, pi=pi, tbf=tbf: e.transpose(out=tbf[:, pi * 128:(pi + 1) * 128], in_=on[pi][:], identity=ident_b[:]),
                                     reads=[T_on[pi], T_const], writes=[Tb[tbk]])
                                S.op("dve", lambda e, slot=slot, tbf=tbf, pi=pi: e.tensor_scalar(out=oT[:, h, slot * 128:(slot + 1) * 128],
                                                                                               in0=tbf[:, pi * 128:(pi + 1) * 128],
                                                                                               scalar1=subg[:, 0:1], scalar2=None, op0=ALU.mult),
                                     reads=[Tb[tbk], T_const], writes=[T_oT])

                        T_ssq = [Tok() for _ in range(2)]
                        T_tO2 = T_post
                        T_on = [Tok() for _ in range(2)]
                        deferred = []
                        LA = 2
                        for i in range(min(LA, nsteps)):
                            emit_S(i)
                        for i in range(nsteps):
                            while deferred and deferred[0][0] <= i:
                                deferred.pop(0)[1]()
                            if steps[i]["kb"] == 0 and steps[i]["j"] == 0:
                                ensure_head(steps[i]["h"] + 1)
                            if i + LA < nsteps:
                                emit_S(i + LA)
                            emit_exp(i)
                            emit_PV(i)
                            st_ = steps[i]
                            if st_["kb"] == st_["nkb"] - 1:
                                h_, j_, ob_ = st_["h"], st_["j"], st_["ob"]
                                post_A(h_, j_, ob_)
                                deferred.append((i + 3, lambda h_=h_, j_=j_, ob_=ob_: post_B(h_, j_, ob_)))
                                deferred.append((i + 5, lambda h_=h_, j_=j_, ob_=ob_: post_C(h_, j_, ob_)))
                                deferred.sort(key=lambda x: x[0])
                        while deferred:
                            deferred.pop(0)[1]()
                        S.barrier()

                    if dbg:
                        d_oT = nc.dram_tensor("d_oT", [128, 4 * NOWN], BF16, kind="ExternalOutput").ap()
                        S.dma("sp", d_oT, oT[:].rearrange("p h n -> p (h n)"), "dbg", reads=[T_oT])
                        d_hT = nc.dram_tensor("d_hT", [128, 8 * NOWN], BF16, kind="ExternalOutput").ap()
                        S.dma("sp", d_hT, hT_own[:].rearrange("p c n -> p (c n)"), "dbg", reads=T_hTown)
                        d_q = nc.dram_tensor("d_q", [128, 4 * NOWN], BF16, kind="ExternalOutput").ap()
                        S.dma("sp", d_q, qT[:].rearrange("p h n -> p (h n)"), "dbg", reads=[T_qT])
                        S.barrier()

                with ExitStack() as pd:
                    arena2 = sbuf(pd, "arena2", [128, 8192], BF16)
                    yBT = arena2[:, 0:4 * NOWN].rearrange("p (g n) -> p g n", g=4)
                    T_yBT = Tok()
                    wga = arena_bf[:, 0:8192].rearrange("p (c n) -> p c n", c=8)
                    wgp = arena_bf[:, 8192:16384].rearrange("p (c n) -> p c n", c=8)
                    wba = arena_bf[:, 16384:20480].rearrange("p (c n) -> p c n", c=4)
                    wbb = arena_bf[:, 20480:24576].rearrange("p (c n) -> p c n", c=4)
                    T_wd = Tok()
                    T_wd2 = Tok()
                    S.dma("pool", wga, wslice(2048, 3072), "wd2", writes=[T_wd2])
                    S.dma("pool", wgp, wslice(3072, 4096), "wd2", writes=[T_wd2])
                    S.dma("pool", wba, w_ba.rearrange("(h p) n -> p h n", p=128), "wd2", writes=[T_wd2])
                    S.dma("pool", wbb, w_bb.rearrange("(h p) n -> p h n", p=128), "wd2", writes=[T_wd2])
                    with ExitStack() as pd1:
                        wu = sbuf(pd1, "wu", [128, 8, 512], BF16)
                        wpl = sbuf(pd1, "wpl", [128, 4, 128], BF16)
                        S.dma("pool", wu[:], wslice(1536, 2048), "wd", writes=[T_wd])
                        S.dma("pool", wpl[:], pool_w.rearrange("g c d -> c g d"), "wd", writes=[T_wd])
                        W = 144
                        ub = [sbuf(pd1, f"ub{i}", [128, NSLOT, W], F32) for i in range(3)]
                        T_ub = [Tok() for _ in range(3)]
                        pooledT = [sbuf(pd1, f"pooledT{i}", [128, NSLOT, 128], BF16) for i in range(2)]
                        T_pl = [Tok() for _ in range(2)]
                        for g in range(4):
                            w = 2 ** (g + 1)
                            u0 = ub[0]
                            for tg in range(NTG):
                                pb = tg % 2

                                def mmu(e, tg=tg, pb=pb, g=g):
                                    for kc in range(8):
                                        ins = e.matmul(banks[pb][:, 0:TG], lhsT=wu[:, kc, g * 128:(g + 1) * 128],
                                                       rhs=hT_own[:, kc, tg * TG:(tg + 1) * TG], start=(kc == 0), stop=(kc == 7))
                                    return ins
                                S.op("pe", mmu, reads=[T_wd] + T_hTown[tg * TPG:(tg + 1) * TPG], writes=[Tb[pb]])
                                S.op("act", lambda e, tg=tg, pb=pb: e.copy(out=u0[:, tg * TPG:(tg + 1) * TPG, 16:W],
                                                                           in_=banks[pb][:, 0:TG].rearrange("p (s t) -> p s t", t=128)),
                                     reads=[Tb[pb]], writes=[T_ub[0]])
                            NH = NSLOT * 16

                            def mmh(e, g=g):
                                for kc in range(8):
                                    ins = e.matmul(banks[2][:, 0:NH], lhsT=wu[:, kc, g * 128:(g + 1) * 128], rhs=hT_halo[:, kc, :],
                                                   start=(kc == 0), stop=(kc == 7))
                                return ins
                            S.op("pe", mmh, reads=[T_wd, T_hThalo], writes=[Tb[2]])
                            S.op("dve", lambda e: e.tensor_tensor(out=u0[:, :, 0:16], in0=banks[2][:, 0:NH].rearrange("p (s t) -> p s t", t=16),
                                                                  in1=hv_t[:], op=ALU.mult), reads=[Tb[2], T_const], writes=[T_ub[0]])
                            cur = 0
                            for k in range(g + 1):
                                sh = 2 ** k
                                nxt = 1 if cur != 1 else 2
                                if k > 0:
                                    pass
                                lo = 2 * sh - 1
                                S.op("dve", lambda e, cur=cur, nxt=nxt, sh=sh, lo=lo: e.tensor_tensor(
                                    out=ub[nxt][:, :, lo:W], in0=ub[cur][:, :, lo:W], in1=ub[cur][:, :, lo - sh:W - sh], op=ALU.add),
                                    reads=[T_ub[cur]], writes=[T_ub[nxt]])
                                cur = nxt
                            pl = pooledT[g % 2]
                            S.op("dve", lambda e, cur=cur, g=g: e.tensor_tensor(out=ub[cur][:, :, 16:32], in0=ub[cur][:, :, 16:32], in1=ic_t[:, g, :, :], op=ALU.mult),
                                 reads=[T_ub[cur], T_const], writes=[T_ub[cur]])
                            S.op("dve", lambda e, cur=cur, pl=pl: e.tensor_tensor(out=pl[:, :, 0:16], in0=ub[cur][:, :, 16:32], in1=u0[:, :, 16:32], op=ALU.subtract),
                                 reads=[T_ub[cur], T_ub[0]], writes=[T_pl[g % 2]])
                            S.op("dve", lambda e, cur=cur, pl=pl, w=w: e.scalar_tensor_tensor(out=pl[:, :, 16:128], in0=ub[cur][:, :, 32:W], scalar=1.0 / w,
                                                                                             in1=u0[:, :, 32:W], op0=ALU.mult, op1=ALU.subtract),
                                 reads=[T_ub[cur], T_ub[0]], writes=[T_pl[g % 2]])
                            for tg in range(NTG):
                                pb = 3 + (tg % 2)
                                S.op("pe", lambda e, tg=tg, pb=pb, pl=pl, g=g: e.matmul(
                                    banks[pb][:, 0:TG], lhsT=wpl[:, g, :], rhs=pl[:, tg * TPG:(tg + 1) * TPG, :].rearrange("p s t -> p (s t)"),
                                    start=True, stop=True), reads=[T_wd, T_pl[g % 2]], writes=[Tb[pb]])
                                S.op("act", lambda e, tg=tg, pb=pb, g=g: e.activation(out=yBT[:, g, tg * TG:(tg + 1) * TG], in_=banks[pb][:, 0:TG],
                                                                                     func=AF.Copy, scale=pscale[:, g:g + 1]),
                                     reads=[Tb[pb], T_const], writes=[T_yBT])
                        S.barrier()

                    mT = sbuf(pd, "mT", [128, 8, NOWN], BF16)
                    T_mT = [Tok() for _ in range(NTG)]
                    with ExitStack() as pd2:
                        sga = [sbuf(pd2, f"sga{i}", [128, TG], BF16) for i in range(2)]
                        sgp = [sbuf(pd2, f"sgp{i}", [128, TG], BF16) for i in range(2)]
                        t1 = [sbuf(pd2, f"t1_{i}", [128, TG], F32) for i in range(2)]
                        t2 = [sbuf(pd2, f"t2_{i}", [128, TG], F32) for i in range(2)]
                        T_sg = [Tok() for _ in range(2)]
                        T_sp = [Tok() for _ in range(2)]
                        T_t1 = [Tok() for _ in range(2)]
                        T_t2 = [Tok() for _ in range(2)]
                        it = 0
                        for tg in range(NTG):
                            tsl = slice(tg * TG, (tg + 1) * TG)
                            hdeps = T_hTown[tg * TPG:(tg + 1) * TPG]
                            for cc in range(8):
                                ib = it % 2
                                it += 1
                                csl = slice(cc * 128, (cc + 1) * 128)
                                bga, bgp, bya, byp = 0 + ib, 2 + ib, 4 + ib, 6 + ib

                                def mm_ga(e, csl=csl, bga=bga, tsl=tsl):
                                    for kc in range(8):
                                        ins = e.matmul(banks[bga][:, 0:TG], lhsT=wga[:, kc, csl], rhs=hT_own[:, kc, tsl], start=(kc == 0), stop=(kc == 7))
                                    return ins

                                def mm_gp(e, csl=csl, bgp=bgp, tsl=tsl):
                                    for kc in range(8):
                                        ins = e.matmul(banks[bgp][:, 0:TG], lhsT=wgp[:, kc, csl], rhs=hT_own[:, kc, tsl], start=(kc == 0), stop=(kc == 7))
                                    return ins

                                def mm_ya(e, csl=csl, tsl=tsl, bya=bya):
                                    for hh in range(4):
                                        ins = e.matmul(banks[bya][:, 0:TG], lhsT=wba[:, hh, csl], rhs=oT[:, hh, tsl], start=(hh == 0), stop=(hh == 3))
                                    return ins

                                def mm_yp(e, csl=csl, tsl=tsl, byp=byp):
                                    for gg in range(4):
                                        ins = e.matmul(banks[byp][:, 0:TG], lhsT=wbb[:, gg, csl], rhs=yBT[:, gg, tsl], start=(gg == 0), stop=(gg == 3))
                                    return ins
                                S.op("pe", mm_ga, reads=[T_wd2] + hdeps, writes=[Tb[bga]])
                                S.op("act", lambda e, ib=ib, bga=bga: e.activation(out=sga[ib][:], in_=banks[bga][:, 0:TG], func=AF.Sigmoid),
                                     reads=[Tb[bga]], writes=[T_sg[ib]])
                                S.op("pe", mm_gp, reads=[T_wd2] + hdeps, writes=[Tb[bgp]])
                                S.op("act", lambda e, ib=ib, bgp=bgp: e.activation(out=sgp[ib][:], in_=banks[bgp][:, 0:TG], func=AF.Sigmoid),
                                     reads=[Tb[bgp]], writes=[T_sp[ib]])
                                S.op("pe", mm_ya, reads=[T_wd2, T_oT], writes=[Tb[bya]])
                                S.op("dve", lambda e, ib=ib, bya=bya: e.tensor_tensor(out=t1[ib][:], in0=banks[bya][:, 0:TG], in1=sga[ib][:], op=ALU.mult),
                                     reads=[Tb[bya], T_sg[ib]], writes=[T_t1[ib]])
                                S.op("pe", mm_yp, reads=[T_wd2, T_yBT], writes=[Tb[byp]])
                                S.op("dve", lambda e, ib=ib, byp=byp: e.tensor_tensor(out=t2[ib][:], in0=banks[byp][:, 0:TG], in1=sgp[ib][:], op=ALU.mult),
                                     reads=[Tb[byp], T_sp[ib]], writes=[T_t2[ib]])
                                S.op("dve", lambda e, ib=ib, cc=cc, tsl=tsl: e.tensor_tensor(out=mT[:, cc, tsl], in0=t1[ib][:], in1=t2[ib][:], op=ALU.add),
                                     reads=[T_t1[ib], T_t2[ib]], writes=[T_mT[tg]])
                        S.barrier()

                    with ExitStack() as pd3:
                        wo = arena2[:, 0:8192].rearrange("p (c n) -> p c n", c=8)
                        T_wo = Tok()
                        S.dma("pool", wo, w_out.rearrange("(c p) n -> p c n", p=128), "wo", writes=[T_wo])
                        for t in range(NSLOT):
                            S.dma("sp", x1[:, t, :], x_own[t * 128:(t + 1) * 128, :], f"x1_{t % 4}", writes=[T_x1[t]])
                        tres = [sbuf(pd3, f"tres{i}", [128, 512], F32) for i in range(2)]
                        T_tres = [Tok() for _ in range(2)]
                        oi = 0
                        for slot in range(NSLOT):
                            tg = slot // TPG
                            for half in range(2):
                                ob_ = oi % 4
                                rb_ = oi % 2
                                oi += 1

                                def mm_o(e, slot=slot, half=half, ob_=ob_):
                                    for kc in range(8):
                                        ins = e.matmul(banks[ob_][:], lhsT=mT[:, kc, slot * 128:(slot + 1) * 128],
                                                       rhs=wo[:, kc, half * 512:(half + 1) * 512], start=(kc == 0), stop=(kc == 7))
                                    return ins
                                S.op("pe", mm_o, reads=[T_wo, T_mT[tg]], writes=[Tb[ob_]])
                                S.op("dve", lambda e, half=half, ob_=ob_, rb_=rb_: e.tensor_tensor(out=tres[rb_][:], in0=banks[ob_][:],
                                                                                                  in1=gate1_bc[:, half * 512:(half + 1) * 512], op=ALU.mult),
                                     reads=[Tb[ob_], T_mod], writes=[T_tres[rb_]])
                                S.op("dve", lambda e, half=half, rb_=rb_, slot=slot: e.tensor_tensor(
                                    out=x1[:, slot, half * 512:(half + 1) * 512], in0=x1[:, slot, half * 512:(half + 1) * 512], in1=tres[rb_][:], op=ALU.add),
                                    reads=[T_tres[rb_], T_x1[slot]], writes=[T_x1[slot]])
                        S.barrier()

            if dbg:
                d_x1 = nc.dram_tensor("d_x1", [NOWN, D], F32, kind="ExternalOutput").ap()
                for t in range(NSLOT):
                    S.dma("sp", d_x1[t * 128:(t + 1) * 128, :], x1[:, t, :], "dbg", reads=[T_x1[t]])
                S.barrier()

            I32 = mybir.dt.int32
            IOA = bass.IndirectOffsetOnAxis
            XROWS = NE * CAPR
            with ExitStack() as pe_:
                NS = NSLOT
                idx_i = sbuf(pe_, "idx_i", [128, 2, NS], I32)
                wts = sbuf(pe_, "wts", [128, 2, NS], F32)
                cnt_i = sbuf(pe_, "cnt_i", [128, 16], I32)
                zidx_i = sbuf(pe_, "zidx_i", [128, 16], I32)
                T_idx = Tok()
                T_sc = []
                wgt = [sbuf(pe_, "wgt0", [128, 8, DE], BF16)]
                wut = [sbuf(pe_, "wut0", [128, 8, DE], BF16)]
                wdt = [sbuf(pe_, "wdt0", [128, 4, D], BF16)]
                T_we = [Tok() for _ in range(2)]

                def load_expert(ei):
                    b = ei % 2
                    S.dma("pool", wgt[b][:], e_wg[ei].rearrange("(c p) n -> p c n", p=128), f"we{b}", writes=[T_we[b]])
                    S.dma("pool", wut[b][:], e_wu[ei].rearrange("(c p) n -> p c n", p=128), f"we{b}", writes=[T_we[b]])
                    S.dma("pool", wdt[b][:], e_wd[ei].rearrange("(c p) n -> p c n", p=128), f"we{b}", writes=[T_we[b]])
                load_expert(0)
                with ExitStack() as pn:
                    h2tm = sbuf(pn, "h2tm", [128, NS, D], BF16)
                    T_h2tm = [Tok() for _ in range(NS)]
                    logits = sbuf(pn, "logits", [128, NS, 20], F32)
                    T_lg = Tok()
                    xh2 = [sbuf(pn, f"xh2_{i}", [128, D], F32) for i in range(2)]
                    T_xh2 = [Tok() for _ in range(2)]
                    hrow = [sbuf(pn, f"hrow{i}", [128, D], F32) for i in range(2)]
                    T_hrow = [Tok() for _ in range(2)]
                    junk2 = sbuf(pn, "junk2", [128, D], BF16)
                    T_junk2 = Tok()
                    ss2 = [sbuf(pn, f"ss2_{i}", [128, 1], F32) for i in range(2)]
                    T_ss2 = [Tok() for _ in range(2)]
                    h2f = [sbuf(pn, f"h2f{i}", [128, 8, 128], F32) for i in range(2)]
                    T_h2f = [Tok() for _ in range(2)]
                    zt = sbuf(pn, "zt", [128, D], BF16)
                    T_zt = Tok()
                    gs2_bc = sbuf(pn, "gs2_bc", [128, D], F32)
                    sh2_bc = sbuf(pn, "sh2_bc", [128, D], F32)
                    T_bc2 = Tok()
                    S.dma("sp", gs2_bc[:], modd[0:1, :].broadcast_to([128, D]), "bc2", reads=[T_modd], writes=[T_bc2])
                    S.dma("sp", sh2_bc[:], modd[1:2, :].broadcast_to([128, D]), "bc2", reads=[T_modd], writes=[T_bc2])
                    S.op("pool", lambda e: e.memset(zt[:], 0.0), writes=[T_zt])

                    def n2_stage1(t):
                        b2 = t % 2
                        S.op("act", lambda e: e.activation(out=junk2[:], in_=x1[:, t, :], func=AF.Square, accum_out=ss2[b2][:]),
                             reads=[T_x1[t]], writes=[T_junk2, T_ss2[b2]])
                        S.op("act", lambda e: e.activation(out=ss2[b2][:], in_=ss2[b2][:], func=AF.Sqrt, bias=eps_t[:], scale=1.0 / D),
                             reads=[T_ss2[b2], T_const], writes=[T_ss2[b2]])
                        S.op("dve", lambda e: e.reciprocal(out=ss2[b2][:], in_=ss2[b2][:]), reads=[T_ss2[b2]], writes=[T_ss2[b2]])
                        S.op("dve", lambda e: e.tensor_scalar(out=xh2[b2][:], in0=x1[:, t, :], scalar1=ss2[b2][:, 0:1], scalar2=None, op0=ALU.mult),
                             reads=[T_x1[t], T_ss2[b2]], writes=[T_xh2[b2]])

                    def n2_stage2(t):
                        b2 = t % 2
                        pa_, pb_ = (0, 1) if b2 == 0 else (2, 3)

                        def tr2(e):
                            for kc in range(8):
                                bk = pa_ if kc < 4 else pb_
                                ins = e.transpose(out=banks[bk][:, (kc % 4) * 128:(kc % 4 + 1) * 128], in_=xh2[b2][:, kc * 128:(kc + 1) * 128],
                                                  identity=ident_f[:])
                            return ins
                        S.op("pe", tr2, reads=[T_xh2[b2], T_const], writes=[Tb[pa_], Tb[pb_]])
                        S.op("pool", lambda e: e.tensor_tensor(out=hrow[b2][:], in0=xh2[b2][:], in1=gs2_bc[:], op=ALU.mult),
                             reads=[T_xh2[b2], T_bc2], writes=[T_hrow[b2]])
                        S.op("pool", lambda e: e.tensor_tensor(out=h2tm[:, t, :], in0=hrow[b2][:], in1=sh2_bc[:], op=ALU.add),
                             reads=[T_hrow[b2], T_bc2], writes=[T_h2tm[t]])
                        for kc in range(8):
                            bk = pa_ if kc < 4 else pb_
                            S.op("act", lambda e, kc=kc, bk=bk: e.activation(
                                out=h2f[b2][:, kc, :], in_=banks[bk][:, (kc % 4) * 128:(kc % 4 + 1) * 128], func=AF.Identity,
                                bias=modT[:, 16 + kc:17 + kc], scale=gs2[:, kc:kc + 1]), reads=[Tb[bk], T_mod], writes=[T_h2f[b2]])

                        def mmr(e):
                            for kc in range(8):
                                ins = e.matmul(banks[4 + b2][:, 0:20], lhsT=h2f[b2][:, kc, :], rhs=wr_f[:, kc, :], start=(kc == 0), stop=(kc == 7))
                            return ins
                        S.op("pe", mmr, reads=[T_h2f[b2], T_const], writes=[Tb[4 + b2]])
                        S.op("dve", lambda e: e.tensor_tensor(out=logits[:, t, :], in0=banks[4 + b2][:, 0:20], in1=rbias[:], op=ALU.add),
                             reads=[Tb[4 + b2], T_const], writes=[T_lg])

                    n2_stage1(0)
                    for t in range(NS):
                        if t + 1 < NS:
                            n2_stage1(t + 1)
                        n2_stage2(t)

                    r1 = sbuf(pn, "r1", [128, NS, 16], F32)
                    r2 = sbuf(pn, "r2", [128, NS, 16], F32)
                    r3 = sbuf(pn, "r3", [128, NS, 16], F32)
                    oh1 = sbuf(pn, "oh1", [128, NS, 16], F32)
                    oh2 = sbuf(pn, "oh2", [128, NS, 16], F32)
                    Mb = sbuf(pn, "Mb", [128, NS, 16], BF16)
                    tot = sbuf(pn, "tot", [128, NS, 16], F32)
                    off = sbuf(pn, "off", [128, NS, 16], F32)
                    posb = sbuf(pn, "posb", [128, NS, 16], F32)
                    idxf = sbuf(pn, "idxf", [128, 2, NS], F32)
                    cntf = sbuf(pn, "cntf", [128, 16], F32)
                    zf = sbuf(pn, "zf", [128, 16], F32)
                    pen = sbuf(pn, "pen", [128, NS, 4], F32)
                    elc = sbuf(pn, "elc", [128, NS, 16], F32)
                    gmx = sbuf(pn, "gmx", [128, NS], F32)
                    gsm = sbuf(pn, "gsm", [128, NS], F32)
                    m1 = sbuf(pn, "m1", [128, NS], F32)
                    m2 = sbuf(pn, "m2", [128, NS], F32)
                    w1 = sbuf(pn, "w1", [128, NS], F32)
                    w2 = sbuf(pn, "w2", [128, NS], F32)
                    T_r = Tok()
                    X = mybir.AxisListType.X
                    gl = logits[:, :, 0:4]
                    el = logits[:, :, 4:20]

                    def R(fn, eng="dve", extra=()):
                        S.op(eng, fn, reads=[T_r, T_lg, T_const] + list(extra), writes=[T_r] + list(extra))
                    R(lambda e: e.tensor_reduce(out=gmx[:], in_=gl, axis=X, op=ALU.max))
                    R(lambda e: e.tensor_tensor(out=r1[:, :, 0:4], in0=gl, in1=gmx[:].unsqueeze(2).broadcast_to([128, NS, 4]), op=ALU.subtract))
                    R(lambda e: e.activation(out=r2[:, :, 0:4], in_=r1[:, :, 0:4], func=AF.Exp), eng="act")
                    R(lambda e: e.tensor_reduce(out=gsm[:], in_=r2[:, :, 0:4], axis=X, op=ALU.add))
                    R(lambda e: e.reciprocal(out=gsm[:], in_=gsm[:]))
                    R(lambda e: e.tensor_scalar(out=pen[:], in0=r1[:, :, 0:4], scalar1=0.0, scalar2=None, op0=ALU.is_lt))
                    R(lambda e: e.tensor_copy(out=elc[:], in_=el))
                    R(lambda e: e.scalar_tensor_tensor(out=r3[:].rearrange("p s (g k) -> p (s g) k", g=4),
                                                       in0=pen[:].rearrange("p s g -> p (s g)").unsqueeze(2).broadcast_to([128, NS * 4, 4]), scalar=NEG,
                                                       in1=elc[:].rearrange("p s (g k) -> p (s g) k", g=4), op0=ALU.mult, op1=ALU.add))
                    R(lambda e: e.tensor_reduce(out=m1[:], in_=r3[:], axis=X, op=ALU.max))
                    R(lambda e: e.tensor_tensor(out=oh1[:], in0=r3[:], in1=m1[:].unsqueeze(2).broadcast_to([128, NS, 16]), op=ALU.is_ge))
                    R(lambda e: e.scalar_tensor_tensor(out=r1[:], in0=oh1[:], scalar=NEG, in1=r3[:], op0=ALU.mult, op1=ALU.add))
                    R(lambda e: e.tensor_reduce(out=m2[:], in_=r1[:], axis=X, op=ALU.max))
                    R(lambda e: e.tensor_tensor(out=oh2[:], in0=r1[:], in1=m2[:].unsqueeze(2).broadcast_to([128, NS, 16]), op=ALU.is_ge))
                    R(lambda e: e.tensor_tensor(out=m2[:], in0=m2[:], in1=m1[:], op=ALU.subtract))
                    R(lambda e: e.activation(out=w2[:], in_=m2[:], func=AF.Sigmoid), eng="act")
                    R(lambda e: e.tensor_scalar(out=w1[:], in0=w2[:], scalar1=-1.0, scalar2=1.0, op0=ALU.mult, op1=ALU.add))
                    R(lambda e: e.tensor_tensor(out=wts[:, 0, :], in0=w1[:], in1=gsm[:], op=ALU.mult), extra=[T_idx])
                    R(lambda e: e.tensor_tensor(out=wts[:, 1, :], in0=w2[:], in1=gsm[:], op=ALU.mult), extra=[T_idx])
                    R(lambda e: e.tensor_tensor(out=Mb[:], in0=oh1[:], in1=oh2[:], op=ALU.add))
                    Mflat = Mb[:].rearrange("p s e -> p (s e)")
                    S.op("pe", lambda e: e.matmul(banks[6][:, 0:NS * 16], lhsT=ustrict_b[:], rhs=Mflat, start=True, stop=True),
                         reads=[T_r, T_const], writes=[Tb[6]])
                    S.op("pe", lambda e: e.matmul(banks[7][:, 0:NS * 16], lhsT=ones_b[:], rhs=Mflat, start=True, stop=True),
                         reads=[T_r, T_const], writes=[Tb[7]])
                    S.op("dve", lambda e: e.tensor_copy(out=tot[:].rearrange("p s e -> p (s e)"), in_=banks[7][:, 0:NS * 16]),
                         reads=[Tb[7], T_r], writes=[T_r])
                    R(lambda e: e.memset(off[:, 0, :], 0.0))
                    for t in range(1, NS):
                        R(lambda e, t=t: e.tensor_tensor(out=off[:, t, :], in0=off[:, t - 1, :], in1=tot[:, t - 1, :], op=ALU.add))
                    S.op("dve", lambda e: e.tensor_tensor(out=posb[:].rearrange("p s e -> p (s e)"), in0=banks[6][:, 0:NS * 16],
                                                          in1=off[:].rearrange("p s e -> p (s e)"), op=ALU.add),
                         reads=[Tb[6], T_r], writes=[T_r])
                    R(lambda e: e.tensor_tensor(out=posb[:], in0=posb[:], in1=ebase[:, 0:16].unsqueeze(1).broadcast_to([128, NS, 16]), op=ALU.add))
                    R(lambda e: e.tensor_tensor(out=r1[:], in0=oh1[:], in1=posb[:], op=ALU.mult))
                    R(lambda e: e.tensor_reduce(out=idxf[:, 0, :], in_=r1[:], axis=X, op=ALU.add))
                    R(lambda e: e.tensor_tensor(out=r2[:], in0=oh2[:], in1=posb[:], op=ALU.mult))
                    R(lambda e: e.tensor_reduce(out=idxf[:, 1, :], in_=r2[:], axis=X, op=ALU.add))
                    R(lambda e: e.tensor_copy(out=idx_i[:], in_=idxf[:]), extra=[T_idx])
                    R(lambda e: e.tensor_tensor(out=cntf[:], in0=off[:, NS - 1, :], in1=tot[:, NS - 1, :], op=ALU.add))
                    R(lambda e: e.tensor_copy(out=cnt_i[:], in_=cntf[:]), extra=[T_idx])
                    R(lambda e: e.scalar_tensor_tensor(out=zf[:], in0=cntf[:], scalar=ebase[:, 16:17], in1=ebase[:, 0:16], op0=ALU.add, op1=ALU.add))
                    R(lambda e: e.tensor_copy(out=zidx_i[:], in_=zf[:]), extra=[T_idx])
                    for ei in range(NE):
                        tk = Tok()
                        T_sc.append(tk)
                        S.indirect("zf", [T_zt, T_idx], [tk], out=Xs, out_offset=IOA(ap=zidx_i[:, ei:ei + 1], axis=0), in_=zt[:], in_offset=None)
                    for t in range(NS):
                        for k in range(2):
                            tk = Tok()
                            T_sc.append(tk)
                            S.indirect("sc", [T_h2tm[t], T_idx], [tk], out=Xs, out_offset=IOA(ap=idx_i[:, k, t:t + 1], axis=0), in_=h2tm[:, t, :],
                                       in_offset=None)
                    S.barrier()

                if dbg:
                    d_idx = nc.dram_tensor("d_idx", [128, 2 * NS], I32, kind="ExternalOutput").ap()
                    S.dma("sp", d_idx, idx_i[:].rearrange("p k s -> p (k s)"), "dbg", reads=[T_idx])
                    d_cnt = nc.dram_tensor("d_cnt", [128, 16], I32, kind="ExternalOutput").ap()
                    S.dma("sp", d_cnt, cnt_i[:], "dbg", reads=[T_idx])
                    d_w = nc.dram_tensor("d_w", [128, 2 * NS], F32, kind="ExternalOutput").ap()
                    S.dma("sp", d_w, wts[:].rearrange("p k s -> p (k s)"), "dbg", reads=[T_idx])

                wgt.append(sbuf(pe_, "wgt1", [128, 8, DE], BF16))
                wut.append(sbuf(pe_, "wut1", [128, 8, DE], BF16))
                wdt.append(sbuf(pe_, "wdt1", [128, 4, D], BF16))
                load_expert(1)
                xg = [sbuf(pe_, f"xg{i}", [128, D], BF16) for i in range(3)]
                T_xg = [Tok() for _ in range(3)]
                xgT = [sbuf(pe_, f"xgT{i}", [128, 8, 128], BF16) for i in range(2)]
                T_xgT = [Tok() for _ in range(2)]
                sgl = [sbuf(pe_, f"sgl{i}", [128, 512], BF16) for i in range(2)]
                T_sgl = [Tok() for _ in range(2)]
                hidT = [sbuf(pe_, f"hidT{i}", [128, 4, 128], BF16) for i in range(2)]
                T_hid = [Tok() for _ in range(2)]
                hid_tm = [sbuf(pe_, f"hid_tm{i}", [128, 512], BF16) for i in range(2)]
                T_htm = [Tok() for _ in range(2)]
                ysb = [sbuf(pe_, f"ysb{i}", [128, D], F32) for i in range(2)]
                T_ysb = [Tok() for _ in range(2)]
                T_ys = [Tok() for _ in range(2)]
                regsets = [bass.RegisterHandles([S.engs[e].alloc_register(f"necnt{i}_" + e) for e in S.engs]) for i in range(2)]
                tile_ctr = [0]

                def tile_body(ei, ti):
                    n = tile_ctr[0]
                    tile_ctr[0] += 1
                    wb = ei % 2
                    b3, b2 = n % 3, n % 2
                    row0 = ei * CAPR + ti * 128
                    S.dma("sp", xg[b3][:], Xs[row0:row0 + 128, :], f"xg{b3}", reads=T_sc, writes=[T_xg[b3]])
                    pbT = n % 2
                    pbf = bank_bf(pbT)

                    def trx(e):
                        for kc in range(8):
                            ins = e.transpose(out=pbf[:, kc * 128:(kc + 1) * 128], in_=xg[b3][:, kc * 128:(kc + 1) * 128], identity=ident_b[:])
                        return ins
                    S.op("pe", trx, reads=[T_xg[b3], T_const], writes=[Tb[pbT]])
                    S.op("act", lambda e: e.copy(out=xgT[b2][:, 0:4, :].rearrange("p c n -> p (c n)"), in_=pbf[:, 0:512]),
                         reads=[Tb[pbT]], writes=[T_xgT[b2]])
                    S.op("dve", lambda e: e.tensor_copy(out=xgT[b2][:, 4:8, :].rearrange("p c n -> p (c n)"), in_=pbf[:, 512:1024]),
                         reads=[Tb[pbT]], writes=[T_xgT[b2]])
                    pg, pu = 2 + b2, 4 + b2

                    def mm_gate(e):
                        for kc in range(8):
                            ins = e.matmul(banks[pg][:], lhsT=xgT[b2][:, kc, :], rhs=wgt[wb][:, kc, :], start=(kc == 0), stop=(kc == 7))
                        return ins

                    def mm_up(e):
                        for kc in range(8):
                            ins = e.matmul(banks[pu][:], lhsT=xgT[b2][:, kc, :], rhs=wut[wb][:, kc, :], start=(kc == 0), stop=(kc == 7))
                        return ins
                    S.op("pe", mm_gate, reads=[T_we[wb], T_xgT[b2]], writes=[Tb[pg]])
                    S.op("act", lambda e: e.activation(out=sgl[b2][:], in_=banks[pg][:], func=AF.Silu), reads=[Tb[pg]], writes=[T_sgl[b2]])
                    S.op("pe", mm_up, reads=[T_we[wb], T_xgT[b2]], writes=[Tb[pu]])
                    S.op("dve", lambda e: e.tensor_tensor(out=hid_tm[b2][:], in0=banks[pu][:], in1=sgl[b2][:], op=ALU.mult),
                         reads=[Tb[pu], T_sgl[b2]], writes=[T_htm[b2]])
                    pbf2 = bank_bf(pbT)

                    def trh(e):
                        for fc in range(4):
                            ins = e.transpose(out=pbf2[:, fc * 128:(fc + 1) * 128], in_=hid_tm[b2][:, fc * 128:(fc + 1) * 128], identity=ident_b[:])
                        return ins
                    S.op("pe", trh, reads=[T_htm[b2], T_const], writes=[Tb[pbT]])
                    S.op("act", lambda e: e.copy(out=hidT[b2][:].rearrange("p c n -> p (c n)"), in_=pbf2[:, 0:512]),
                         reads=[Tb[pbT]], writes=[T_hid[b2]])
                    for half in range(2):
                        py = 6 + half

                        def mm_y(e, half=half, py=py):
                            for fc in range(4):
                                ins = e.matmul(banks[py][:], lhsT=hidT[b2][:, fc, :], rhs=wdt[wb][:, fc, half * 512:(half + 1) * 512],
                                               start=(fc == 0), stop=(fc == 3))
                            return ins
                        S.op("pe", mm_y, reads=[T_we[wb], T_hid[b2]], writes=[Tb[py]])
                        if half == 0:
                            S.op("act", lambda e, py=py: e.copy(out=ysb[b2][:, 0:512], in_=banks[py][:]), reads=[Tb[py]], writes=[T_ysb[b2]])
                        else:
                            S.op("dve", lambda e, py=py: e.tensor_copy(out=ysb[b2][:, 512:1024], in_=banks[py][:]), reads=[Tb[py]], writes=[T_ysb[b2]])
                    S.dma("pool", Ys[row0:row0 + 128, :], ysb[b2][:], f"ys{b2}", reads=[T_ysb[b2]], writes=[T_ys[b2]])

                for e_ in S.engs:
                    S._deps(e_, [T_idx], [])
                nc.regs_load(regsets[0], cnt_i[0:1, 0:1])
                for ei in range(NE):
                    regs = regsets[ei % 2]
                    if ei + 1 < NE:
                        nc.regs_load(regsets[(ei + 1) % 2], cnt_i[0:1, ei + 1:ei + 2])
                    def nest(ti, ei=ei, regs=regs):
                        tile_body(ei, ti)
                        if ti + 1 < NS:
                            S.cond_region(regs, (ti + 1) * 128, lambda: nest(ti + 1))
                    S.cond_region(regs, 0, lambda: nest(0))
                    if ei + 2 < NE:
                        load_expert(ei + 2)

                NGB = 3
                yA = [sbuf(pe_, f"yA{i}", [128, D], F32) for i in range(NGB)]
                yB = [sbuf(pe_, f"yB{i}", [128, D], F32) for i in range(NGB)]
                T_yA = [Tok() for _ in range(NGB)]
                T_yB = [Tok() for _ in range(NGB)]
                T_out = Tok()

                def gather(t):
                    b = t % NGB
                    S.indirect(f"ga{b}", T_ys + [T_idx], [T_yA[b]], out=yA[b][:], out_offset=None, in_=Ys,
                               in_offset=IOA(ap=idx_i[:, 0, t:t + 1], axis=0))
                    S.indirect(f"gb{b}", T_ys + [T_idx], [T_yB[b]], out=yB[b][:], out_offset=None, in_=Ys,
                               in_offset=IOA(ap=idx_i[:, 1, t:t + 1], axis=0))
                for t in range(min(NGB - 1, NS)):
                    gather(t)
                for t in range(NS):
                    b = t % NGB
                    if t + NGB - 1 < NS:
                        gather(t + NGB - 1)
                    S.op("dve", lambda e, b=b, t=t: e.tensor_scalar(out=yA[b][:], in0=yA[b][:], scalar1=wts[:, 0, t:t + 1], scalar2=None, op0=ALU.mult),
                         reads=[T_yA[b], T_idx], writes=[T_yA[b]])
                    S.op("dve", lambda e, b=b, t=t: e.scalar_tensor_tensor(out=yB[b][:], in0=yB[b][:], scalar=wts[:, 1, t:t + 1], in1=yA[b][:],
                                                                           op0=ALU.mult, op1=ALU.add),
                         reads=[T_yA[b], T_yB[b], T_idx], writes=[T_yB[b]])
                    S.op("dve", lambda e, b=b: e.tensor_tensor(out=yB[b][:], in0=yB[b][:], in1=gate2_bc[:], op=ALU.mult),
                         reads=[T_yB[b], T_mod], writes=[T_yB[b]])
                    S.op("dve", lambda e, b=b, t=t: e.tensor_tensor(out=x1[:, t, :], in0=x1[:, t, :], in1=yB[b][:], op=ALU.add),
                         reads=[T_yB[b], T_x1[t]], writes=[T_x1[t]])
                    S.dma("sp", out_own[t * 128:(t + 1) * 128, :], x1[:, t, :], "out", reads=[T_x1[t]], writes=[T_out])
                S.barrier()

        @blk.sync
        def _(_unused):
            with nc.allow_non_contiguous_dma(reason="small one-time parameter layouts"):
                _body()

    return nc


def _own_blocks(r, npairs):
    blocks = []
    for j in range(npairs):
        blocks += [8 * j + r, 8 * j + 7 - r]
    return blocks


def make_in_maps(inputs, S_LEN):
    f = lambda a: np.ascontiguousarray(np.asarray(a, dtype=np.float32))
    x = f(inputs["x"])
    B = x.shape[0]
    npairs = S_LEN // 1024
    shared = {
        "rel_bias": f(inputs["rel_bias"]),
        "ada_w": f(inputs["ada_w"][0]),
        "ada_b": f(inputs["ada_b"][0]).reshape(1, -1),
        "norm1_g": f(inputs["norm1_g"][0]).reshape(1, -1),
        "w_in": f(inputs["w_in"][0]),
        "q_norm_g": f(inputs["q_norm_g"][0]).reshape(1, -1),
        "k_norm_g": f(inputs["k_norm_g"][0]).reshape(1, -1),
        "lam_in": f(np.concatenate([inputs["lambda_q1"][0], inputs["lambda_k1"][0],
                                    inputs["lambda_q2"][0], inputs["lambda_k2"][0]])).reshape(1, -1),
        "subln_g": f(inputs["subln_g"][0]).reshape(1, -1),
        "w_ba": f(inputs["w_branch_attn"][0]),
        "pool_w": f(inputs["pool_w"][0]),
        "pool_scale": f(inputs["pool_scale"][0]).reshape(1, -1),
        "w_bb": f(inputs["w_branch_pool"][0]),
        "w_out": f(inputs["w_out"][0]),
        "norm2_g": f(inputs["norm2_g"][0]).reshape(1, -1),
        "r_w": f(np.concatenate([inputs["router_group_w"][0], inputs["router_expert_w"][0]], axis=1)),
        "r_b": f(np.concatenate([inputs["router_group_b"][0], inputs["router_expert_b"][0]])).reshape(1, -1),
        "e_wg": f(inputs["expert_w_gate"][0]),
        "e_wu": f(inputs["expert_w_up"][0]),
        "e_wd": f(inputs["expert_w_down"][0]),
        "ident_in": np.eye(128, dtype=np.float32),
        "bones_in": np.kron(np.eye(2, dtype=np.float32), np.ones((64, 64), np.float32)),
        "ustrict_in": np.triu(np.ones((128, 128), np.float32), 1),
        "ebase_in": np.concatenate([np.tile(np.arange(16, dtype=np.float32) * (S_LEN // 4 + 128), (128, 1)),
                                    np.arange(128, dtype=np.float32)[:, None]], axis=1),
    }
    c = f(inputs["c"])
    in_maps = []
    metas = []
    for core in range(4 * B):
        b, r = core // 4, core % 4
        blocks = _own_blocks(r, npairs)
        xb = x[b]
        x_own = np.concatenate([xb[k * 128:(k + 1) * 128] for k in blocks], axis=0)
        halo = []
        for k in blocks:
            if k == 0:
                halo.append(np.zeros((16, D), np.float32))
            else:
                halo.append(xb[k * 128 - 16:k * 128])
        x_halo = np.concatenate(halo, axis=0)
        x_kv = xb.reshape(S_LEN // 128, 128, D)[:, ::-1, :].reshape(S_LEN, D)
        oh, mask = _core_tables(r)
        hv, ic = _pool_tables(blocks)
        m = dict(shared)
        m.update({
            "x_own": np.ascontiguousarray(x_own), "x_halo": np.ascontiguousarray(x_halo),
            "x_kv": np.ascontiguousarray(x_kv), "c_row": c[b:b + 1],
            "oh_in": oh, "mask_in": mask, "hv_in": hv, "ic_in": ic,
        })
        in_maps.append(m)
        metas.append((b, blocks))
    return in_maps, metas


def kernel(**inputs):
    x = np.asarray(inputs["x"])
    B, S_LEN, _ = x.shape
    nc = build_program(S_LEN)
    in_maps, metas = make_in_maps(inputs, S_LEN)
    res = run_bass_kernel_spmd(nc, in_maps, core_ids=list(range(len(in_maps))))
    out = np.empty((B, S_LEN, D), np.float32)
    for (b, blocks), r in zip(metas, res.results):
        o = np.asarray(r["out_own"])
        for si, k in enumerate(blocks):
            out[b, k * 128:(k + 1) * 128] = o[si * 128:(si + 1) * 128]
    return out
```

```python
import math
from contextlib import ExitStack

import numpy as np
import concourse.bass as bass
import concourse.mybir as mybir
from concourse.bass_utils import run_bass_kernel_spmd

F32 = mybir.dt.float32
BF16 = mybir.dt.bfloat16
AF = mybir.ActivationFunctionType
ALU = mybir.AluOpType

D = 1024
NEG = -30000.0
EPS = 1e-6
NE = 16
DE = 512


class Tok:
    __slots__ = ("w", "rd")

    def __init__(self):
        self.w = None
        self.rd = {}


class Sched:
    STRICT_SAME = True

    def __init__(self, nc, stack):
        self.nc = nc
        self.stack = stack
        self.engs = {"pe": nc.tensor, "act": nc.scalar, "dve": nc.vector,
                     "pool": nc.gpsimd, "sp": nc.sync}
        self.sem = {k: stack.enter_context(nc.semaphore("sem_" + k)) for k in self.engs}
        self.cnt = {k: 0 for k in self.engs}
        self.seen = {k: {} for k in self.engs}
        self.dma_sems = {}
        self.dma_cnt = {}
        self.issuer = {}

    def _wait(self, e, kind, key, val):
        if kind == "eng":
            if key == e and not (self.STRICT_SAME and e in ("act", "dve", "pool")):
                return
            sem = self.sem[key]
        else:
            sem = self.dma_sems[key]
            val = self.dma_cnt[key]
        k = (kind, key)
        if self.seen[e].get(k, 0) >= val:
            return
        self.engs[e].wait_ge(sem, val)
        self.seen[e][k] = val

    def _deps(self, e, reads, writes):
        need = {}
        for b in reads:
            if b.w is not None:
                k = (b.w[0], b.w[1])
                need[k] = max(need.get(k, 0), b.w[2])
        for b in writes:
            if b.w is not None:
                k = (b.w[0], b.w[1])
                need[k] = max(need.get(k, 0), b.w[2])
            for k, v in b.rd.items():
                need[k] = max(need.get(k, 0), v)
        for (kind, key), val in need.items():
            self._wait(e, kind, key, val)

    def _mark(self, me, reads, writes):
        k = (me[0], me[1])
        for b in reads:
            b.rd[k] = max(b.rd.get(k, 0), me[2])
        for b in writes:
            b.w = me
            b.rd = {}

    def op(self, e, fn, reads=(), writes=()):
        self._deps(e, reads, writes)
        inst = fn(self.engs[e])
        self.cnt[e] += 1
        inst.then_inc(self.sem[e], 1)
        self._mark(("eng", e, self.cnt[e]), reads, writes)

    def dma(self, q, out, in_, sem, reads=(), writes=(), **kw):
        if sem not in self.dma_sems:
            self.dma_sems[sem] = self.stack.enter_context(self.nc.semaphore("dsem_" + sem))
            self.dma_cnt[sem] = 0
        self.issuer[sem] = q
        self._deps(q, reads, writes)
        inst = self.engs[q].dma_start(out=out, in_=in_, **kw)
        inst.then_inc(self.dma_sems[sem], 16)
        self.dma_cnt[sem] += 16
        self._mark(("dma", sem, self.dma_cnt[sem]), reads, writes)

    def indirect(self, sem, reads, writes, **kw):
        q = "pool"
        if sem not in self.dma_sems:
            self.dma_sems[sem] = self.stack.enter_context(self.nc.semaphore("dsem_" + sem))
            self.dma_cnt[sem] = 0
        self.issuer[sem] = q
        self._deps(q, reads, writes)
        inst = self.nc.gpsimd.indirect_dma_start(**kw)
        inst.then_inc(self.dma_sems[sem], 16)
        self.dma_cnt[sem] += 16
        self._mark(("dma", sem, self.dma_cnt[sem]), reads, writes)

    def cond_region(self, regs, thr, body):
        import copy
        snap_cnt = dict(self.cnt)
        snap_d = dict(self.dma_cnt)
        snap_seen = copy.deepcopy(self.seen)
        with self.nc.If_cmp(regs, thr, "IS_GT"):
            body()
        with self.nc.Else():
            for e in self.engs:
                d = self.cnt[e] - snap_cnt[e]
                if d:
                    if snap_cnt[e] > 0:
                        self.engs[e].wait_ge(self.sem[e], snap_cnt[e])
                    self.engs[e].sem_inc(self.sem[e], d)
            for sname, total in self.dma_cnt.items():
                dd = total - snap_d.get(sname, 0)
                if dd:
                    q = self.engs[self.issuer[sname]]
                    if snap_d.get(sname, 0) > 0:
                        q.wait_ge(self.dma_sems[sname], snap_d[sname])
                    q.sem_inc(self.dma_sems[sname], dd)
        self.seen = snap_seen

    def barrier(self):
        for e in self.engs:
            for o in self.engs:
                if o != e and self.cnt[o] > 0:
                    self._wait(e, "eng", o, self.cnt[o])
            for s in self.dma_sems:
                if self.dma_cnt[s] > 0:
                    self._wait(e, "dma", s, self.dma_cnt[s])


def _t5_bucket(rel):
    nb, max_exact = 16, 8
    bucket = np.where(rel > 0, nb, 0)
    n = np.abs(rel)
    n_f = np.maximum(n, max_exact).astype(np.float32)
    large = max_exact + (np.log(n_f / np.float32(max_exact)) / np.float32(math.log(128 / max_exact))
                         * np.float32(nb - max_exact)).astype(np.int32)
    large = np.minimum(large, nb - 1)
    return bucket + np.where(n < max_exact, n, large)


def _core_tables(r):
    oh = np.zeros((32, 16, 256), np.float32)
    mask = np.zeros((128, 16, 128), np.float32)
    n = np.arange(256)
    for m in range(8):
        for s in range(2):
            qb = r if s == 0 else 7 - r
            delta = m - qb
            t = m * 2 + s
            if delta > 0:
                oh[15, t, :] = 1.0
                mask[:, t, :] = NEG
            else:
                rel = 128 * delta + 127 - n
                b = _t5_bucket(rel.astype(np.int32))
                oh[b, t, n] = 1.0
                if delta == 0:
                    mask[0:64, t, 0:64] = NEG
    return oh.reshape(32, 4096), mask.reshape(128, 2048)


def _pool_tables(blocks):
    ns = len(blocks)
    hv = np.ones((128, ns, 16), np.float32)
    ic = np.zeros((128, 4, ns, 16), np.float32)
    for si, blk in enumerate(blocks):
        if blk == 0:
            hv[:, si, :] = 0.0
        for g, w in enumerate((2, 4, 8, 16)):
            t = blk * 128 + np.arange(16)
            ic[:, g, si, :] = 1.0 / np.minimum(t + 1, w).astype(np.float32)
    return hv.reshape(128, ns * 16), ic.reshape(128, 4 * ns * 16)


def build_program(S_LEN, dbg=False):
    NP = S_LEN // 1024
    NSLOT = 2 * NP
    NOWN = NSLOT * 128
    NKB = S_LEN // 128
    TG = min(512, NOWN)
    NTG = NOWN // TG
    TPG = TG // 128

    nc = bass.Bass("TRN2", target_bir_lowering=False)

    def din(name, shape, dt=F32):
        return nc.dram_tensor(name, list(shape), dt, kind="ExternalInput").ap()

    x_own = din("x_own", [NOWN, D])
    x_halo = din("x_halo", [NSLOT * 16, D])
    x_kv = din("x_kv", [S_LEN, D])
    c_row = din("c_row", [1, D])
    rel_bias = din("rel_bias", [32, 4])
    ada_w = din("ada_w", [D, 6 * D])
    ada_b = din("ada_b", [1, 6 * D])
    norm1_g = din("norm1_g", [1, D])
    w_in = din("w_in", [D, 4096])
    q_norm_g = din("q_norm_g", [1, 64])
    k_norm_g = din("k_norm_g", [1, 64])
    lam_in = din("lam_in", [1, 256])
    subln_g = din("subln_g", [1, 128])
    w_ba = din("w_ba", [512, D])
    pool_w = din("pool_w", [4, 128, 128])
    pool_scale = din("pool_scale", [1, 512])
    w_bb = din("w_bb", [512, D])
    w_out = din("w_out", [D, D])
    norm2_g = din("norm2_g", [1, D])
    r_w = din("r_w", [D, 20])
    r_b = din("r_b", [1, 20])
    e_wg = din("e_wg", [NE, D, DE])
    e_wu = din("e_wu", [NE, D, DE])
    e_wd = din("e_wd", [NE, DE, D])
    ident_in = din("ident_in", [128, 128])
    bones_in = din("bones_in", [128, 128])
    oh_in = din("oh_in", [32, 4096])
    mask_in = din("mask_in", [128, 2048])
    hv_in = din("hv_in", [128, NSLOT * 16])
    ic_in = din("ic_in", [128, 4 * NSLOT * 16])
    ustrict_in = din("ustrict_in", [128, 128])
    ebase_in = din("ebase_in", [128, 17])

    out_own = nc.dram_tensor("out_own", [NOWN, D], F32, kind="ExternalOutput").ap()

    KTd = nc.dram_tensor("KTd", [4, 128, S_LEN], BF16).ap()
    Vd = nc.dram_tensor("Vd", [4, 128, NKB * 129], BF16).ap()
    CAPR = NOWN + 128
    Xs = nc.dram_tensor("Xs", [NE * CAPR, D], BF16).ap()
    Ys = nc.dram_tensor("Ys", [NE * CAPR, D], F32).ap()
    modd = nc.dram_tensor("modd", [2, D], F32).ap()
    Gd_t = nc.dram_tensor("Gd", [4, 4096], F32)
    Gd = Gd_t.ap()

    dbg_outs = {}

    with ExitStack() as top:
        S = Sched(nc, top)
        blk = top.enter_context(nc.Block())

        def sbuf(st, name, shape, dt):
            return st.enter_context(nc.sbuf_tensor(name, list(shape), dt))

        banks = [top.enter_context(nc.psum_tensor(f"pb{i}", [128, 512], F32)) for i in range(8)]
        Tb = [Tok() for _ in range(8)]

        def bank_bf(i):
            return banks[i].bitcast(BF16)

        ident_f = sbuf(top, "ident_f", [128, 128], F32)
        ident_b = sbuf(top, "ident_b", [128, 128], BF16)
        bones_b = sbuf(top, "bones_b", [128, 128], BF16)
        ones_row = sbuf(top, "ones_row", [1, 128], F32)
        eps_t = sbuf(top, "eps_t", [128, 1], F32)
        modT = sbuf(top, "modT", [128, 32], F32)
        gs1 = sbuf(top, "gs1", [128, 8], F32)
        gs2 = sbuf(top, "gs2", [128, 8], F32)
        gate1_bc = sbuf(top, "gate1_bc", [128, D], F32)
        gate2_bc = sbuf(top, "gate2_bc", [128, D], F32)
        gq8 = sbuf(top, "gq8", [128, 1], F32)
        gk = sbuf(top, "gk", [128, 1], F32)
        neglam = sbuf(top, "neglam", [128, 1], F32)
        ch_bc = sbuf(top, "ch_bc", [128, 4], F32)
        subg = sbuf(top, "subg", [128, 1], F32)
        pscale = sbuf(top, "pscale", [128, 4], F32)
        rbias = sbuf(top, "rbias", [128, 20], F32)
        wr_f = sbuf(top, "wr_f", [128, 8, 20], F32)
        maskc = sbuf(top, "maskc", [128, 16, 128], F32)
        hv_t = sbuf(top, "hv_t", [128, NSLOT, 16], F32)
        ic_t = sbuf(top, "ic_t", [128, 4, NSLOT, 16], F32)
        ustrict_b = sbuf(top, "ustrict_b", [128, 128], BF16)
        ones_b = sbuf(top, "ones_b", [128, 128], BF16)
        ebase = sbuf(top, "ebase", [128, 17], F32)
        T_const = Tok()
        T_mod = Tok()
        T_modd = Tok()

        ARENA_F = 16896
        arena = sbuf(top, "arena", [128, ARENA_F], F32)
        arena_bf = arena.bitcast(BF16)
        x1 = arena[:, 0:NSLOT * D].rearrange("p (s d) -> p s d", d=D)
        T_x1 = [Tok() for _ in range(NSLOT)]

        def _body():
            with ExitStack() as pa:
                ld = lambda out, in_, **kw: S.dma("sp", out, in_, "cst", writes=[T_const], **kw)
                bones_f = sbuf(pa, "bones_f", [128, 128], F32)
                cT = sbuf(pa, "cT", [128, 8], F32)
                scT = sbuf(pa, "scT", [128, 8], F32)
                g1T = sbuf(pa, "g1T", [128, 8], F32)
                g2T = sbuf(pa, "g2T", [128, 8], F32)
                adab = sbuf(pa, "adab", [1, 6 * D], F32)
                modrow = sbuf(pa, "modrow", [1, 6 * D], F32)
                lamrow = sbuf(pa, "lamrow", [1, 256], F32)
                lamtmp = sbuf(pa, "lamtmp", [1, 128], F32)
                lam2 = sbuf(pa, "lam2", [1, 4], F32)
                one11 = sbuf(pa, "one11", [1, 1], F32)
                rb_t = sbuf(pa, "rb_t", [32, 4], F32)
                oh_t = sbuf(pa, "oh_t", [32, 4096], F32)
                Gs = sbuf(pa, "Gs", [4, 4096], F32)
                adaw = [arena[:, i * 4096:(i + 1) * 4096].rearrange("p (c n) -> p c n", c=8) for i in range(3)]
                T_adaw = [Tok() for _ in range(3)]
                T_tmp = Tok()
                T_row = Tok()

                ustrict_f = sbuf(pa, "ustrict_f", [128, 128], F32)
                g2row = sbuf(pa, "g2row", [1, D], F32)
                gs2row = sbuf(pa, "gs2row", [1, D], F32)
                ld(ident_f[:], ident_in)
                ld(ustrict_f[:], ustrict_in)
                ld(g2row[:], norm2_g)
                ld(ebase[:], ebase_in)
                ld(bones_f[:], bones_in)
                ld(cT[:], c_row.rearrange("o (c p) -> p (o c)", p=128))
                ld(g1T[:], norm1_g.rearrange("o (c p) -> p (o c)", p=128))
                ld(g2T[:], norm2_g.rearrange("o (c p) -> p (o c)", p=128))
                ld(adab[:], ada_b)
                ld(lamrow[:], lam_in)
                ld(rb_t[:], rel_bias)
                ld(oh_t[:], oh_in)
                ld(gq8[0:64, :], q_norm_g.rearrange("o d -> d o"))
                ld(gq8[64:128, :], q_norm_g.rearrange("o d -> d o"))
                ld(gk[0:64, :], k_norm_g.rearrange("o d -> d o"))
                ld(gk[64:128, :], k_norm_g.rearrange("o d -> d o"))
                ld(subg[:], subln_g.rearrange("o d -> d o"))
                ld(pscale[:], pool_scale.rearrange("o (g p) -> p (o g)", p=128))
                ld(ch_bc[:], rel_bias[15:16, :].broadcast_to([128, 4]))
                ld(rbias[:], r_b.broadcast_to([128, 20]))
                ld(wr_f[:], r_w.rearrange("(c p) n -> p c n", p=128))
                ld(maskc[:], mask_in.rearrange("p (t q) -> p t q", q=128))
                ld(hv_t[:], hv_in.rearrange("p (s t) -> p s t", t=16))
                ld(ic_t[:], ic_in.rearrange("p (g s t) -> p g s t", g=4, t=16))

                S.op("dve", lambda e: e.memset(ones_row[:], 1.0), writes=[T_const])
                S.op("dve", lambda e: e.memset(one11[:], 1.0), writes=[T_const])
                S.op("dve", lambda e: e.memset(eps_t[:], EPS), writes=[T_const])
                S.op("dve", lambda e: e.tensor_copy(out=ident_b[:], in_=ident_f[:]), reads=[T_const], writes=[T_const])
                S.op("dve", lambda e: e.tensor_copy(out=bones_b[:], in_=bones_f[:]), reads=[T_const], writes=[T_const])
                S.op("dve", lambda e: e.tensor_copy(out=ustrict_b[:], in_=ustrict_f[:]), reads=[T_const], writes=[T_const])
                S.op("dve", lambda e: e.memset(ones_b[:], 1.0), writes=[T_const])
                S.op("dve", lambda e: e.tensor_scalar(out=gq8[:], in0=gq8[:], scalar1=0.125, scalar2=None, op0=ALU.mult),
                     reads=[T_const], writes=[T_const])
                S.op("dve", lambda e: e.tensor_scalar(out=subg[:], in0=subg[:], scalar1=0.8, scalar2=None, op0=ALU.mult),
                     reads=[T_const], writes=[T_const])
                S.op("act", lambda e: e.activation(out=scT[:], in_=cT[:], func=AF.Silu), reads=[T_const], writes=[T_tmp])

                adaw_v = ada_w.rearrange("(c p) n -> p c n", p=128)
                NPIECE = 12
                for i in range(min(3, NPIECE)):
                    S.dma("sp", adaw[i], adaw_v[:, :, i * 512:(i + 1) * 512], f"adaw{i}", writes=[T_adaw[i]])
                for i in range(NPIECE):
                    bi = i % 3
                    pb = 0 + (i % 2)

                    def mm(e, bi=bi, pb=pb):
                        for kc in range(8):
                            ins = e.matmul(banks[pb][0:1, :], lhsT=scT[:, kc:kc + 1], rhs=adaw[bi][:, kc, :],
                                           start=(kc == 0), stop=(kc == 7))
                        return ins
                    S.op("pe", mm, reads=[T_tmp, T_adaw[bi]], writes=[Tb[pb]])
                    S.op("dve", lambda e, i=i, pb=pb: e.tensor_tensor(out=modrow[:, i * 512:(i + 1) * 512], in0=banks[pb][0:1, :],
                                                                      in1=adab[:, i * 512:(i + 1) * 512], op=ALU.add),
                         reads=[Tb[pb], T_const], writes=[T_row])
                    if i + 3 < NPIECE:
                        S.dma("sp", adaw[bi], adaw_v[:, :, (i + 3) * 512:(i + 4) * 512], f"adaw{bi}", writes=[T_adaw[bi]])

                def mmT(e):
                    for vi, v in enumerate((0, 1, 3, 4)):
                        for kc in range(8):
                            ins = e.matmul(banks[2][:, vi * 8 + kc: vi * 8 + kc + 1],
                                           lhsT=modrow[0:1, v * D + kc * 128: v * D + (kc + 1) * 128],
                                           rhs=one11[:], start=True, stop=True)
                    return ins
                S.op("pe", mmT, reads=[T_row, T_const], writes=[Tb[2]])
                S.op("dve", lambda e: e.tensor_copy(out=modT[:], in_=banks[2][:, 0:32]), reads=[Tb[2]], writes=[T_mod])
                S.op("dve", lambda e: e.scalar_tensor_tensor(out=gs1[:], in0=modT[:, 8:16], scalar=1.0, in1=g1T[:],
                                                             op0=ALU.add, op1=ALU.mult), reads=[T_mod, T_const], writes=[T_mod])
                S.op("dve", lambda e: e.scalar_tensor_tensor(out=gs2[:], in0=modT[:, 24:32], scalar=1.0, in1=g2T[:],
                                                             op0=ALU.add, op1=ALU.mult), reads=[T_mod, T_const], writes=[T_mod])
                for gi, (v, dst) in enumerate(((2, gate1_bc), (5, gate2_bc))):
                    for half in range(2):
                        pb = 3 + half
                        S.op("pe", lambda e, v=v, half=half, pb=pb: e.matmul(
                            banks[pb][:], lhsT=ones_row[:], rhs=modrow[0:1, v * D + half * 512: v * D + (half + 1) * 512],
                            start=True, stop=True), reads=[T_row, T_const], writes=[Tb[pb]])
                        S.op("act", lambda e, dst=dst, half=half, pb=pb: e.copy(out=dst[:, half * 512:(half + 1) * 512], in_=banks[pb][:]),
                             reads=[Tb[pb]], writes=[T_mod])
                S.op("dve", lambda e: e.scalar_tensor_tensor(out=gs2row[:], in0=modrow[0:1, 4 * D:5 * D], scalar=1.0, in1=g2row[:],
                                                             op0=ALU.add, op1=ALU.mult), reads=[T_row, T_const], writes=[T_tmp])
                S.dma("sp", modd[0:1, :], gs2row[:], "modd", reads=[T_tmp], writes=[T_modd])
                S.dma("sp", modd[1:2, :], modrow[0:1, 3 * D:4 * D], "modd", reads=[T_row], writes=[T_modd])
                S.op("dve", lambda e: e.tensor_tensor(out=lamtmp[:].rearrange("o (a d) -> o a d", a=2),
                                                      in0=lamrow[:].rearrange("o (a t d) -> o a t d", a=2, t=2)[:, :, 0, :],
                                                      in1=lamrow[:].rearrange("o (a t d) -> o a t d", a=2, t=2)[:, :, 1, :],
                                                      op=ALU.mult), reads=[T_const], writes=[T_tmp])
                S.op("dve", lambda e: e.reduce_sum(out=lam2[:, 0:2], in_=lamtmp[:].rearrange("o (a d) -> o a d", a=2),
                                                   axis=mybir.AxisListType.X), reads=[T_tmp], writes=[T_tmp])
                S.op("act", lambda e: e.activation(out=lam2[:, 0:2], in_=lam2[:, 0:2], func=AF.Exp), reads=[T_tmp], writes=[T_tmp])
                S.op("dve", lambda e: e.tensor_tensor(out=lam2[:, 2:3], in0=lam2[:, 1:2], in1=lam2[:, 0:1], op=ALU.subtract),
                     reads=[T_tmp], writes=[T_tmp])
                S.op("dve", lambda e: e.tensor_scalar(out=lam2[:, 3:4], in0=lam2[:, 2:3], scalar1=-0.2, scalar2=None, op0=ALU.add),
                     reads=[T_tmp], writes=[T_tmp])
                S.op("pe", lambda e: e.matmul(banks[5][:, 0:1], lhsT=ones_row[:], rhs=lam2[:, 3:4], start=True, stop=True),
                     reads=[T_tmp, T_const], writes=[Tb[5]])
                S.op("dve", lambda e: e.tensor_copy(out=neglam[:], in_=banks[5][:, 0:1]), reads=[Tb[5]], writes=[T_const])
                for ci in range(8):
                    pb = 6 + (ci % 2)
                    S.op("pe", lambda e, ci=ci, pb=pb: e.matmul(banks[pb][0:4, :], lhsT=rb_t[:], rhs=oh_t[:, ci * 512:(ci + 1) * 512],
                                                               start=True, stop=True), reads=[T_const], writes=[Tb[pb]])
                    S.op("dve", lambda e, ci=ci, pb=pb: e.tensor_copy(out=Gs[:, ci * 512:(ci + 1) * 512], in_=banks[pb][0:4, :]),
                         reads=[Tb[pb]], writes=[T_tmp])
                T_G = Tok()
                S.dma("sp", Gd, Gs[:], "gd", reads=[T_tmp], writes=[T_G])
                S.barrier()

            def norm_tiles(st, tag, src_rows, ntiles, rows_per_tile, dst_fn, dst_tok_fn, after_tile=None):
                LA1 = 2
                NB = LA1 + 2
                NX = LA1 + 1
                xt = [sbuf(st, f"{tag}_x{i}", [128, D], F32) for i in range(NB)]
                xh = [sbuf(st, f"{tag}_xh{i}", [128, D], BF16) for i in range(NX)]
                junk = sbuf(st, f"{tag}_junk", [128, D], BF16)
                ss = [sbuf(st, f"{tag}_ss{i}", [128, 1], F32) for i in range(NX)]
                T_x = [Tok() for _ in range(NB)]
                T_xh = [Tok() for _ in range(NX)]
                T_junk = Tok()
                T_ss = [Tok() for _ in range(NX)]
                R = rows_per_tile
                for t in range(min(NB - 1, ntiles)):
                    S.dma("sp", xt[t % NB][0:R, :], src_rows(t), f"{tag}_x{t % NB}", writes=[T_x[t % NB]])
                def stage1(t):
                    b3, b2 = t % NB, t % NX
                    S.op("act", lambda e, b3=b3, b2=b2: e.activation(out=junk[0:R, :], in_=xt[b3][0:R, :], func=AF.Square,
                                                                     accum_out=ss[b2][0:R, :]),
                         reads=[T_x[b3]], writes=[T_junk, T_ss[b2]])
                    S.op("act", lambda e, b2=b2: e.activation(out=ss[b2][0:R, :], in_=ss[b2][0:R, :], func=AF.Sqrt,
                                                              bias=eps_t[0:R, :], scale=1.0 / D),
                         reads=[T_ss[b2], T_const], writes=[T_ss[b2]])
                    S.op("dve", lambda e, b2=b2: e.reciprocal(out=ss[b2][0:R, :], in_=ss[b2][0:R, :]),
                         reads=[T_ss[b2]], writes=[T_ss[b2]])
                    S.op("dve", lambda e, b3=b3, b2=b2: e.tensor_scalar(out=xh[b2][0:R, :], in0=xt[b3][0:R, :], scalar1=ss[b2][0:R, 0:1],
                                                                        scalar2=None, op0=ALU.mult),
                         reads=[T_x[b3], T_ss[b2]], writes=[T_xh[b2]])

                def stage2(t):
                    b2 = t % NX
                    pb = t % 2
                    pbf = bank_bf(pb)

                    def tr(e, b2=b2, pbf=pbf):
                        for kc in range(8):
                            ins = e.transpose(out=pbf[:, kc * 128: kc * 128 + R], in_=xh[b2][0:R, kc * 128:(kc + 1) * 128],
                                              identity=ident_b[0:R, 0:R])
                        return ins
                    S.op("pe", tr, reads=[T_xh[b2], T_const], writes=[Tb[pb]])
                    for kc in range(8):
                        eng = "dve" if kc % 4 == 3 else "act"
                        if eng == "act":
                            S.op("act", lambda e, kc=kc, t=t, pbf=pbf: e.activation(
                                out=dst_fn(t, kc), in_=pbf[:, kc * 128: kc * 128 + R], func=AF.Identity,
                                bias=modT[:, kc:kc + 1], scale=gs1[:, kc:kc + 1]),
                                reads=[Tb[pb], T_mod], writes=[dst_tok_fn(t)])
                        else:
                            S.op("dve", lambda e, kc=kc, t=t, pbf=pbf: e.tensor_scalar(
                                out=dst_fn(t, kc), in0=pbf[:, kc * 128: kc * 128 + R], scalar1=gs1[:, kc:kc + 1],
                                scalar2=modT[:, kc:kc + 1], op0=ALU.mult, op1=ALU.add),
                                reads=[Tb[pb], T_mod], writes=[dst_tok_fn(t)])

                for t in range(min(LA1, ntiles)):
                    stage1(t)
                for t in range(ntiles):
                    if t + NB - 1 < ntiles:
                        tn = t + NB - 1
                        S.dma("sp", xt[tn % NB][0:R, :], src_rows(tn), f"{tag}_x{tn % NB}", writes=[T_x[tn % NB]])
                    if t + LA1 < ntiles:
                        stage1(t + LA1)
                    stage2(t)
                    if after_tile is not None:
                        after_tile(t)

            def wslice(lo, hi):
                return w_in.rearrange("(c p) n -> p c n", p=128)[:, :, lo:hi]

            def qk_norm_sq(raw_bank, sq, T_sq, ncols):
                S.op("act", lambda e: e.activation(out=sq[:, 0:ncols], in_=banks[raw_bank][:, 0:ncols], func=AF.Square),
                     reads=[Tb[raw_bank]], writes=[T_sq])

            def qk_norm_rest(raw_bank, sq, T_sq, ssum_bank, rstd, T_rstd, ncols, gain, outs):
                S.op("pe", lambda e: e.matmul(banks[ssum_bank][:, 0:ncols], lhsT=bones_b[:], rhs=sq[:, 0:ncols], start=True, stop=True),
                     reads=[T_sq, T_const], writes=[Tb[ssum_bank]])
                S.op("act", lambda e: e.activation(out=rstd[:, 0:ncols], in_=banks[ssum_bank][:, 0:ncols], func=AF.Sqrt,
                                                   bias=eps_t[:], scale=1.0 / 64), reads=[Tb[ssum_bank], T_const], writes=[T_rstd])
                S.op("dve", lambda e: e.reciprocal(out=rstd[:, 0:ncols], in_=rstd[:, 0:ncols]), reads=[T_rstd], writes=[T_rstd])
                for dst, plo, phi, T_dst in outs:
                    S.op("dve", lambda e, dst=dst, plo=plo, phi=phi: e.scalar_tensor_tensor(
                        out=dst, in0=banks[raw_bank][plo:phi, 0:ncols], scalar=gain[plo:phi, 0:1], in1=rstd[plo:phi, 0:ncols],
                        op0=ALU.mult, op1=ALU.mult), reads=[Tb[raw_bank], T_rstd, T_const], writes=[T_dst])

            def qk_norm_group(st_tmp, raw_bank, T_raw, sq, T_sq, ssum_bank, rstd, T_rstd, ncols, gain, outs):
                qk_norm_sq(raw_bank, sq, T_sq, ncols)
                qk_norm_rest(raw_bank, sq, T_sq, ssum_bank, rstd, T_rstd, ncols, gain, outs)

            T_KTd = Tok()
            T_Vd = Tok()
            with ExitStack() as pbk:
                wk = sbuf(pbk, "wk", [128, 8, 512], BF16)
                wv = sbuf(pbk, "wv", [128, 8, 512], BF16)
                T_wkv = Tok()
                S.dma("pool", wk[:], wslice(512, 1024), "wkv", writes=[T_wkv])
                S.dma("pool", wv[:], wslice(1024, 1536), "wkv", writes=[T_wkv])
                hTg = [sbuf(pbk, f"hTg{i}", [128, 8, 512], BF16) for i in range(2)]
                T_hTg = [Tok() for _ in range(2)]
                kst = [sbuf(pbk, f"kst{i}", [128, 4, 512], BF16) for i in range(2)]
                T_kst = [Tok() for _ in range(2)]
                vst = [sbuf(pbk, f"vst{i}", [128, 4, 4, 129], BF16) for i in range(2)]
                T_vst = [Tok() for _ in range(2)]
                sqk = [sbuf(pbk, f"sqk{i}", [128, 512], BF16) for i in range(2)]
                T_sqk = [Tok() for _ in range(2)]
                rsk = [sbuf(pbk, f"rsk{i}", [128, 512], F32) for i in range(2)]
                T_rsk = [Tok() for _ in range(2)]
                for i in range(2):
                    S.op("pool", lambda e, i=i: e.memset(vst[i][:], 1.0), writes=[T_vst[i]])
                NG = S_LEN // 512

                NQ = NKB

                def item_A(q):
                    g, h = divmod(q, 4)
                    gb = g % 2
                    rb = 2 + (q % 3)

                    def mmk(e):
                        for kc in range(8):
                            ins = e.matmul(banks[rb][:], lhsT=wk[:, kc, h * 128:(h + 1) * 128], rhs=hTg[gb][:, kc, :],
                                           start=(kc == 0), stop=(kc == 7))
                        return ins
                    S.op("pe", mmk, reads=[T_wkv, T_hTg[gb]], writes=[Tb[rb]])
                    qk_norm_sq(rb, sqk[q % 2], T_sqk[q % 2], 512)
                    vb = 6 + (q % 2)

                    def mmv(e):
                        for kc in range(8):
                            ins = e.matmul(banks[vb][:], lhsT=hTg[gb][:, kc, h * 128:(h + 1) * 128], rhs=wv[:, kc, :],
                                           start=(kc == 0), stop=(kc == 7))
                        return ins
                    S.op("pe", mmv, reads=[T_wkv, T_hTg[gb]], writes=[Tb[vb]])
                    S.op("act", lambda e: e.copy(out=vst[gb][:, h, :, 0:128], in_=banks[vb][:].rearrange("p (h e) -> p h e", h=4)),
                         reads=[Tb[vb]], writes=[T_vst[gb]])

                def item_B(q):
                    sq, T_sq, rstd, T_rstd = sqk[q % 2], T_sqk[q % 2], rsk[q % 2], T_rsk[q % 2]
                    S.op("pe", lambda e: e.matmul(banks[5][:], lhsT=bones_b[:], rhs=sq[:], start=True, stop=True),
                         reads=[T_sq, T_const], writes=[Tb[5]])
                    S.op("act", lambda e: e.activation(out=rstd[:], in_=banks[5][:], func=AF.Sqrt, bias=eps_t[:], scale=1.0 / 64),
                         reads=[Tb[5], T_const], writes=[T_rstd])

                def item_C(q):
                    g, h = divmod(q, 4)
                    gb = g % 2
                    rb = 2 + (q % 3)
                    rstd, T_rstd = rsk[q % 2], T_rsk[q % 2]
                    S.op("dve", lambda e: e.reciprocal(out=rstd[:], in_=rstd[:]), reads=[T_rstd], writes=[T_rstd])
                    S.op("dve", lambda e: e.scalar_tensor_tensor(out=kst[gb][:, h, :], in0=banks[rb][:], scalar=gk[:, 0:1], in1=rstd[:],
                                                                 op0=ALU.mult, op1=ALU.mult), reads=[Tb[rb], T_rstd, T_const], writes=[T_kst[gb]])
                    if h == 3:
                        S.dma("pool", KTd[:, :, g * 512:(g + 1) * 512].rearrange("h p n -> p h n"), kst[gb][:], f"kst{gb}",
                              reads=[T_kst[gb]], writes=[T_KTd])
                        for hh in range(4):
                            S.dma("pool", Vd[hh, :, g * 516:(g + 1) * 516].rearrange("p (i e) -> p i e", e=129), vst[gb][:, :, hh, :], f"vst{gb}",
                                  reads=[T_vst[gb]], writes=[T_Vd])

                def after_tile(t):
                    if 0 <= t - 6 < NQ:
                        item_C(t - 6)
                    if 0 <= t - 5 < NQ:
                        item_B(t - 5)
                    if 0 <= t - 4 < NQ:
                        item_A(t - 4)

                norm_tiles(pbk, "kv", lambda t: x_kv[t * 128:(t + 1) * 128, :], NKB, 128,
                           lambda t, kc: hTg[(t // 4) % 2][:, kc, (t % 4) * 128:(t % 4 + 1) * 128],
                           lambda t: T_hTg[(t // 4) % 2], after_tile)
                for t in range(NKB, NKB + 7):
                    after_tile(t)
                S.barrier()

            with ExitStack() as pown:
                hT_own = sbuf(pown, "hT_own", [128, 8, NOWN], BF16)
                hT_halo = sbuf(pown, "hT_halo", [128, 8, NSLOT * 16], BF16)
                T_hTown = [Tok() for _ in range(NSLOT)]
                T_hThalo = Tok()
                oT = sbuf(pown, "oT", [128, 4, NOWN], BF16)
                T_oT = Tok()
                with ExitStack() as patt:
                    qT = sbuf(patt, "qT", [128, 4, NOWN], BF16)
                    T_qT = Tok()
                    with ExitStack() as pc:
                        wq = sbuf(pc, "wq", [128, 8, 512], BF16)
                        T_wq = Tok()
                        S.dma("pool", wq[:], wslice(0, 512), "wq", writes=[T_wq])
                        sqq = [sbuf(pc, f"sqq{i}", [128, 512], BF16) for i in range(2)]
                        T_sqq = [Tok() for _ in range(2)]
                        rsq = [sbuf(pc, f"rsq{i}", [128, 512], F32) for i in range(2)]
                        T_rsq = [Tok() for _ in range(2)]
                        with ExitStack() as pcn:
                            norm_tiles(pcn, "own", lambda t: x_own[t * 128:(t + 1) * 128, :], NSLOT, 128,
                                       lambda t, kc: hT_own[:, kc, t * 128:(t + 1) * 128], lambda t: T_hTown[t])
                        S.barrier()
                        NHT = (NSLOT * 16 + 127) // 128
                        for t in range(NHT):
                            rows = min(128, NSLOT * 16 - t * 128)
                            with ExitStack() as pcn:
                                norm_tiles(pcn, f"halo{t}", lambda tt, t=t, rows=rows: x_halo[t * 128: t * 128 + rows, :], 1, rows,
                                           lambda tt, kc, t=t, rows=rows: hT_halo[:, kc, t * 128: t * 128 + rows], lambda tt: T_hThalo)
                            S.barrier()
                        for tg in range(NTG):
                            for h in range(4):
                                rb = 2 + (h % 2)

                                def mmq(e, h=h, rb=rb, tg=tg):
                                    for kc in range(8):
                                        ins = e.matmul(banks[rb][:, 0:TG], lhsT=wq[:, kc, h * 128:(h + 1) * 128],
                                                       rhs=hT_own[:, kc, tg * TG:(tg + 1) * TG], start=(kc == 0), stop=(kc == 7))
                                    return ins
                                S.op("pe", mmq, reads=[T_wq] + T_hTown[tg * TPG:(tg + 1) * TPG], writes=[Tb[rb]])
                                qk_norm_group(None, rb, None, sqq[h % 2], T_sqq[h % 2], 4, rsq[h % 2], T_rsq[h % 2], TG, gq8,
                                              [(qT[:, h, tg * TG:(tg + 1) * TG], 0, 128, T_qT)])
                        S.barrier()

                    with ExitStack() as pat:
                        VW = NKB * 129
                        kt_sb = [arena_bf[:, i * S_LEN:(i + 1) * S_LEN] for i in range(2)]
                        v_sb = [arena_bf[:, 2 * S_LEN + i * VW: 2 * S_LEN + (i + 1) * VW].rearrange("p (k e) -> p k e", e=129) for i in range(2)]
                        T_kv = [Tok() for _ in range(2)]
                        qpad = [sbuf(pat, f"qpad{i}", [128, 2, NOWN], BF16) for i in range(2)]
                        T_qpad = [Tok() for _ in range(2)]
                        bT = [sbuf(pat, f"bT{i}", [128, 16, 128], F32) for i in range(2)]
                        T_bT = [Tok() for _ in range(2)]
                        NEB = 4
                        Eb = [sbuf(pat, f"Eb{i}", [128, 2, 256], BF16) for i in range(NEB)]
                        T_E = [Tok() for _ in range(NEB)]
                        tmpb = [sbuf(pat, f"tmpb{i}", [128, 2, 256], F32) for i in range(2)]
                        T_tmpb = [Tok() for _ in range(2)]
                        rs = [sbuf(pat, f"rs{i}", [128, 4], F32) for i in range(2)]
                        T_rs = [Tok() for _ in range(2)]
                        tO = [sbuf(pat, f"tO{i}", [128, 128], F32) for i in range(2)]
                        oO = [sbuf(pat, f"oO{i}", [128, 128], F32) for i in range(2)]
                        on = [sbuf(pat, f"on{i}", [128, 128], BF16) for i in range(2)]
                        jk = sbuf(pat, "att_jk", [128, 128], BF16)
                        ssq = [sbuf(pat, f"ssq{i}", [128, 1], F32) for i in range(2)]
                        T_post = [Tok() for _ in range(2)]
                        T_jk = Tok()
                        for i in range(2):
                            S.op("dve" if i == 0 else "pool", lambda e, i=i: e.memset(qpad[i][:], 0.0), writes=[T_qpad[i]])

                        def load_head(h):
                            hb = h % 2
                            NCH = max(1, S_LEN // 2048)
                            cw = S_LEN // NCH
                            for ci in range(NCH):
                                S.dma("sp", kt_sb[hb][:, ci * cw:(ci + 1) * cw], KTd[h, :, ci * cw:(ci + 1) * cw], f"kv{hb}",
                                      reads=[T_KTd], writes=[T_kv[hb]])
                            S.dma("sp", v_sb[hb].rearrange("p k e -> p (k e)"), Vd[h], f"kv{hb}", reads=[T_Vd], writes=[T_kv[hb]])
                            for t in range(16):
                                src = bass.AP(Gd_t, h * 4096 + t * 256, [[1, 128], [1, 128]])
                                S.dma("sp", bT[hb][:, t, :], src, f"bT{hb}", reads=[T_G], writes=[T_bT[hb]])
                            eng_ = "dve" if h == 0 else "pool"
                            S.op(eng_, lambda e, hb=hb: e.tensor_tensor(out=bT[hb][:], in0=bT[hb][:], in1=maskc[:], op=ALU.add),
                                 reads=[T_bT[hb], T_const], writes=[T_bT[hb]])
                            S.op(eng_, lambda e, hb=hb, h=h: e.tensor_copy(out=qpad[hb][0:64, 0, :], in_=qT[0:64, h, :]),
                                 reads=[T_qT], writes=[T_qpad[hb]])
                            S.op(eng_, lambda e, hb=hb, h=h: e.tensor_copy(out=qpad[hb][64:128, 1, :], in_=qT[64:128, h, :]),
                                 reads=[T_qT], writes=[T_qpad[hb]])

                        steps = []
                        unit = 0
                        for h in range(4):
                            for j in range(NP):
                                nkb = 8 * j + 8
                                ob = 3 + 2 * (unit % 2)
                                unit += 1
                                for kb in range(nkb):
                                    steps.append(dict(h=h, hb=h % 2, j=j, kb=kb, nkb=nkb, ob=ob, q0=256 * j))
                        nsteps = len(steps)
                        loaded = set()

                        def ensure_head(h):
                            if h < 4 and h not in loaded:
                                loaded.add(h)
                                load_head(h)

                        def emit_S(i):
                            st_ = steps[i]
                            h, hb, kb, q0 = st_["h"], st_["hb"], st_["kb"], st_["q0"]
                            ensure_head(h)
                            sbk = i % 3

                            def mms(e):
                                return e.matmul(banks[sbk][:].rearrange("p (m q) -> p m q", m=2), lhsT=kt_sb[hb][:, kb * 128:(kb + 1) * 128],
                                                rhs=qpad[hb][:, :, q0:q0 + 256], start=True, stop=True)
                            S.op("pe", mms, reads=[T_kv[hb], T_qpad[hb]], writes=[Tb[sbk]])

                        def emit_exp(i):
                            st_ = steps[i]
                            h, hb, kb, j = st_["h"], st_["hb"], st_["kb"], st_["j"]
                            sbk = i % 3
                            eb = i % NEB
                            Ev = Eb[eb][:].rearrange("p m q -> p (m q)")
                            if kb < 8 * j:
                                S.op("act", lambda e: e.activation(out=Ev, in_=banks[sbk][:], func=AF.Exp,
                                                                   bias=ch_bc[:, h:h + 1], scale=1.0),
                                     reads=[Tb[sbk], T_const], writes=[T_E[eb]])
                            else:
                                mi = kb - 8 * j
                                tb = i % 2
                                bias_ap = bT[hb][:, 2 * mi:2 * mi + 2, :].rearrange("p s q -> p (s q)").unsqueeze(1).broadcast_to([128, 2, 256])
                                S.op("dve", lambda e: e.scalar_tensor_tensor(
                                    out=tmpb[tb][:], in0=banks[sbk][:].rearrange("p (m q) -> p m q", m=2), scalar=1.0,
                                    in1=bias_ap, op0=ALU.mult, op1=ALU.add),
                                    reads=[Tb[sbk], T_bT[hb]], writes=[T_tmpb[tb]])
                                S.op("act", lambda e: e.activation(out=Ev, in_=tmpb[tb][:].rearrange("p m q -> p (m q)"), func=AF.Exp),
                                     reads=[T_tmpb[tb]], writes=[T_E[eb]])

                        def emit_PV(i):
                            st_ = steps[i]
                            hb, kb, nkb, ob = st_["hb"], st_["kb"], st_["nkb"], st_["ob"]
                            eb = i % NEB

                            def mmo(e):
                                for m in range(2):
                                    for s_ in range(2):
                                        ins = e.matmul(banks[ob + m][:, s_ * 129:(s_ + 1) * 129], lhsT=Eb[eb][:, m, s_ * 128:(s_ + 1) * 128],
                                                       rhs=v_sb[hb][:, kb, :], start=(kb == 0 and s_ == 0), stop=(kb == nkb - 1),
                                                       skip_group_check=True)
                                return ins
                            S.op("pe", mmo, reads=[T_E[eb], T_kv[hb]], writes=[Tb[ob], Tb[ob + 1]])

                        def post_A(h, j, ob):
                            for s_ in range(2):
                                pi = s_
                                c0 = s_ * 129
                                S.op("dve", lambda e, pi=pi, c0=c0: e.tensor_copy(out=rs[pi][:, 0:1], in_=banks[ob][:, c0 + 128: c0 + 129]),
                                     reads=[Tb[ob]], writes=[T_rs[pi]])
                                S.op("dve", lambda e, pi=pi, c0=c0: e.tensor_copy(out=rs[pi][:, 1:2], in_=banks[ob + 1][:, c0 + 128: c0 + 129]),
                                     reads=[Tb[ob + 1]], writes=[T_rs[pi]])
                                S.op("dve", lambda e, pi=pi: e.reciprocal(out=rs[pi][:, 0:2], in_=rs[pi][:, 0:2]), reads=[T_rs[pi]], writes=[T_rs[pi]])
                                S.op("dve", lambda e, pi=pi: e.tensor_tensor(out=rs[pi][:, 2:3], in0=rs[pi][:, 1:2], in1=neglam[:], op=ALU.mult),
                                     reads=[T_rs[pi], T_const], writes=[T_rs[pi]])
                                S.op("dve", lambda e, pi=pi, c0=c0: e.tensor_scalar(out=tO[pi][:], in0=banks[ob + 1][:, c0: c0 + 128],
                                                                                  scalar1=rs[pi][:, 2:3], scalar2=None, op0=ALU.mult),
                                     reads=[Tb[ob + 1], T_rs[pi]], writes=[T_post[pi]])
                                S.op("dve", lambda e, pi=pi, c0=c0: e.scalar_tensor_tensor(out=oO[pi][:], in0=banks[ob][:, c0: c0 + 128],
                                                                                         scalar=rs[pi][:, 0:1], in1=tO[pi][:],
                                                                                         op0=ALU.mult, op1=ALU.add),
                                     reads=[Tb[ob], T_rs[pi], T_post[pi]], writes=[T_post[pi]])
                                S.op("dve", lambda e, pi=pi: e.scalar_tensor_tensor(out=tO[pi][:], in0=oO[pi][:], scalar=1.0, in1=oO[pi][:],
                                                                                  op0=ALU.mult, op1=ALU.mult, accum_out=ssq[pi][:]),
                                     reads=[T_post[pi]], writes=[T_ssq[pi], T_tO2[pi]])

                        def post_B(h, j, ob):
                            for s_ in range(2):
                                pi = s_
                                S.op("act", lambda e, pi=pi: e.activation(out=ssq[pi][:], in_=ssq[pi][:], func=AF.Ln, bias=eps_t[:], scale=1.0 / 128),
                                     reads=[T_ssq[pi], T_const], writes=[T_ssq[pi]])
                                S.op("act", lambda e, pi=pi: e.activation(out=ssq[pi][:], in_=ssq[pi][:], func=AF.Exp, scale=-0.5),
                                     reads=[T_ssq[pi]], writes=[T_ssq[pi]])

                        def post_C(h, j, ob):
                            for s_ in range(2):
                                pi = s_
                                slot = 2 * j + s_
                                S.op("dve", lambda e, pi=pi: e.tensor_scalar(out=on[pi][:], in0=oO[pi][:], scalar1=ssq[pi][:, 0:1], scalar2=None, op0=ALU.mult),
                                     reads=[T_post[pi], T_ssq[pi]], writes=[T_on[pi]])
                                tbk = 7
                                tbf = bank_bf(tbk)
                                S.op("pe", lambda e, pi=pi, tbf=tbf: e.transpose(out=tbf[:, pi * 128:(pi + 1) * 128], in_=on[pi][:], identity=ident_b[:]),
                                     reads=[T_on[pi], T_const], writes=[Tb[tbk]])
                                S.op("dve", lambda e, slot=slot, tbf=tbf, pi=pi: e.tensor_scalar(out=oT[:, h, slot * 128:(slot + 1) * 128],
                                                                                               in0=tbf[:, pi * 128:(pi + 1) * 128],
                                                                                               scalar1=subg[:, 0:1], scalar2=None, op0=ALU.mult),
                                     reads=[Tb[tbk], T_const], writes=[T_oT])

                        T_ssq = [Tok() for _ in range(2)]
                        T_tO2 = T_post
                        T_on = [Tok() for _ in range(2)]
                        deferred = []
                        LA = 2
                        for i in range(min(LA, nsteps)):
                            emit_S(i)
                        for i in range(nsteps):
                            while deferred and deferred[0][0] <= i:
                                deferred.pop(0)[1]()
                            if steps[i]["kb"] == 0 and steps[i]["j"] == 0:
                                ensure_head(steps[i]["h"] + 1)
                            if i + LA < nsteps:
                                emit_S(i + LA)
                            emit_exp(i)
                            emit_PV(i)
                            st_ = steps[i]
                            if st_["kb"] == st_["nkb"] - 1:
                                h_, j_, ob_ = st_["h"], st_["j"], st_["ob"]
                                post_A(h_, j_, ob_)
                                deferred.append((i + 3, lambda h_=h_, j_=j_, ob_=ob_: post_B(h_, j_, ob_)))
                                deferred.append((i + 5, lambda h_=h_, j_=j_, ob_=ob_: post_C(h_, j_, ob_)))
                                deferred.sort(key=lambda x: x[0])
                        while deferred:
                            deferred.pop(0)[1]()
                        S.barrier()

                    if dbg:
                        d_oT = nc.dram_tensor("d_oT", [128, 4 * NOWN], BF16, kind="ExternalOutput").ap()
                        S.dma("sp", d_oT, oT[:].rearrange("p h n -> p (h n)"), "dbg", reads=[T_oT])
                        d_hT = nc.dram_tensor("d_hT", [128, 8 * NOWN], BF16, kind="ExternalOutput").ap()
                        S.dma("sp", d_hT, hT_own[:].rearrange("p c n -> p (c n)"), "dbg", reads=T_hTown)
                        d_q = nc.dram_tensor("d_q", [128, 4 * NOWN], BF16, kind="ExternalOutput").ap()
                        S.dma("sp", d_q, qT[:].rearrange("p h n -> p (h n)"), "dbg", reads=[T_qT])
                        S.barrier()

                with ExitStack() as pd:
                    arena2 = sbuf(pd, "arena2", [128, 8192], BF16)
                    yBT = arena2[:, 0:4 * NOWN].rearrange("p (g n) -> p g n", g=4)
                    T_yBT = Tok()
                    wga = arena_bf[:, 0:8192].rearrange("p (c n) -> p c n", c=8)
                    wgp = arena_bf[:, 8192:16384].rearrange("p (c n) -> p c n", c=8)
                    wba = arena_bf[:, 16384:20480].rearrange("p (c n) -> p c n", c=4)
                    wbb = arena_bf[:, 20480:24576].rearrange("p (c n) -> p c n", c=4)
                    T_wd = Tok()
                    T_wd2 = Tok()
                    S.dma("pool", wga, wslice(2048, 3072), "wd2", writes=[T_wd2])
                    S.dma("pool", wgp, wslice(3072, 4096), "wd2", writes=[T_wd2])
                    S.dma("pool", wba, w_ba.rearrange("(h p) n -> p h n", p=128), "wd2", writes=[T_wd2])
                    S.dma("pool", wbb, w_bb.rearrange("(h p) n -> p h n", p=128), "wd2", writes=[T_wd2])
                    with ExitStack() as pd1:
                        wu = sbuf(pd1, "wu", [128, 8, 512], BF16)
                        wpl = sbuf(pd1, "wpl", [128, 4, 128], BF16)
                        S.dma("pool", wu[:], wslice(1536, 2048), "wd", writes=[T_wd])
                        S.dma("pool", wpl[:], pool_w.rearrange("g c d -> c g d"), "wd", writes=[T_wd])
                        W = 144
                        ub = [sbuf(pd1, f"ub{i}", [128, NSLOT, W], F32) for i in range(3)]
                        T_ub = [Tok() for _ in range(3)]
                        pooledT = [sbuf(pd1, f"pooledT{i}", [128, NSLOT, 128], BF16) for i in range(2)]
                        T_pl = [Tok() for _ in range(2)]
                        for g in range(4):
                            w = 2 ** (g + 1)
                            u0 = ub[0]
                            for tg in range(NTG):
                                pb = tg % 2

                                def mmu(e, tg=tg, pb=pb, g=g):
                                    for kc in range(8):
                                        ins = e.matmul(banks[pb][:, 0:TG], lhsT=wu[:, kc, g * 128:(g + 1) * 128],
                                                       rhs=hT_own[:, kc, tg * TG:(tg + 1) * TG], start=(kc == 0), stop=(kc == 7))
                                    return ins
                                S.op("pe", mmu, reads=[T_wd] + T_hTown[tg * TPG:(tg + 1) * TPG], writes=[Tb[pb]])
                                S.op("act", lambda e, tg=tg, pb=pb: e.copy(out=u0[:, tg * TPG:(tg + 1) * TPG, 16:W],
                                                                           in_=banks[pb][:, 0:TG].rearrange("p (s t) -> p s t", t=128)),
                                     reads=[Tb[pb]], writes=[T_ub[0]])
                            NH = NSLOT * 16

                            def mmh(e, g=g):
                                for kc in range(8):
                                    ins = e.matmul(banks[2][:, 0:NH], lhsT=wu[:, kc, g * 128:(g + 1) * 128], rhs=hT_halo[:, kc, :],
                                                   start=(kc == 0), stop=(kc == 7))
                                return ins
                            S.op("pe", mmh, reads=[T_wd, T_hThalo], writes=[Tb[2]])
                            S.op("dve", lambda e: e.tensor_tensor(out=u0[:, :, 0:16], in0=banks[2][:, 0:NH].rearrange("p (s t) -> p s t", t=16),
                                                                  in1=hv_t[:], op=ALU.mult), reads=[Tb[2], T_const], writes=[T_ub[0]])
                            cur = 0
                            for k in range(g + 1):
                                sh = 2 ** k
                                nxt = 1 if cur != 1 else 2
                                if k > 0:
                                    pass
                                lo = 2 * sh - 1
                                S.op("dve", lambda e, cur=cur, nxt=nxt, sh=sh, lo=lo: e.tensor_tensor(
                                    out=ub[nxt][:, :, lo:W], in0=ub[cur][:, :, lo:W], in1=ub[cur][:, :, lo - sh:W - sh], op=ALU.add),
                                    reads=[T_ub[cur]], writes=[T_ub[nxt]])
                                cur = nxt
                            pl = pooledT[g % 2]
                            S.op("dve", lambda e, cur=cur, g=g: e.tensor_tensor(out=ub[cur][:, :, 16:32], in0=ub[cur][:, :, 16:32], in1=ic_t[:, g, :, :], op=ALU.mult),
                                 reads=[T_ub[cur], T_const], writes=[T_ub[cur]])
                            S.op("dve", lambda e, cur=cur, pl=pl: e.tensor_tensor(out=pl[:, :, 0:16], in0=ub[cur][:, :, 16:32], in1=u0[:, :, 16:32], op=ALU.subtract),
                                 reads=[T_ub[cur], T_ub[0]], writes=[T_pl[g % 2]])
                            S.op("dve", lambda e, cur=cur, pl=pl, w=w: e.scalar_tensor_tensor(out=pl[:, :, 16:128], in0=ub[cur][:, :, 32:W], scalar=1.0 / w,
                                                                                             in1=u0[:, :, 32:W], op0=ALU.mult, op1=ALU.subtract),
                                 reads=[T_ub[cur], T_ub[0]], writes=[T_pl[g % 2]])
                            for tg in range(NTG):
                                pb = 3 + (tg % 2)
                                S.op("pe", lambda e, tg=tg, pb=pb, pl=pl, g=g: e.matmul(
                                    banks[pb][:, 0:TG], lhsT=wpl[:, g, :], rhs=pl[:, tg * TPG:(tg + 1) * TPG, :].rearrange("p s t -> p (s t)"),
                                    start=True, stop=True), reads=[T_wd, T_pl[g % 2]], writes=[Tb[pb]])
                                S.op("act", lambda e, tg=tg, pb=pb, g=g: e.activation(out=yBT[:, g, tg * TG:(tg + 1) * TG], in_=banks[pb][:, 0:TG],
                                                                                     func=AF.Copy, scale=pscale[:, g:g + 1]),
                                     reads=[Tb[pb], T_const], writes=[T_yBT])
                        S.barrier()

                    mT = sbuf(pd, "mT", [128, 8, NOWN], BF16)
                    T_mT = [Tok() for _ in range(NTG)]
                    with ExitStack() as pd2:
                        sga = [sbuf(pd2, f"sga{i}", [128, TG], BF16) for i in range(2)]
                        sgp = [sbuf(pd2, f"sgp{i}", [128, TG], BF16) for i in range(2)]
                        t1 = [sbuf(pd2, f"t1_{i}", [128, TG], F32) for i in range(2)]
                        t2 = [sbuf(pd2, f"t2_{i}", [128, TG], F32) for i in range(2)]
                        T_sg = [Tok() for _ in range(2)]
                        T_sp = [Tok() for _ in range(2)]
                        T_t1 = [Tok() for _ in range(2)]
                        T_t2 = [Tok() for _ in range(2)]
                        it = 0
                        for tg in range(NTG):
                            tsl = slice(tg * TG, (tg + 1) * TG)
                            hdeps = T_hTown[tg * TPG:(tg + 1) * TPG]
                            for cc in range(8):
                                ib = it % 2
                                it += 1
                                csl = slice(cc * 128, (cc + 1) * 128)
                                bga, bgp, bya, byp = 0 + ib, 2 + ib, 4 + ib, 6 + ib

                                def mm_ga(e, csl=csl, bga=bga, tsl=tsl):
                                    for kc in range(8):
                                        ins = e.matmul(banks[bga][:, 0:TG], lhsT=wga[:, kc, csl], rhs=hT_own[:, kc, tsl], start=(kc == 0), stop=(kc == 7))
                                    return ins

                                def mm_gp(e, csl=csl, bgp=bgp, tsl=tsl):
                                    for kc in range(8):
                                        ins = e.matmul(banks[bgp][:, 0:TG], lhsT=wgp[:, kc, csl], rhs=hT_own[:, kc, tsl], start=(kc == 0), stop=(kc == 7))
                                    return ins

                                def mm_ya(e, csl=csl, tsl=tsl, bya=bya):
                                    for hh in range(4):
                                        ins = e.matmul(banks[bya][:, 0:TG], lhsT=wba[:, hh, csl], rhs=oT[:, hh, tsl], start=(hh == 0), stop=(hh == 3))
                                    return ins

                                def mm_yp(e, csl=csl, tsl=tsl, byp=byp):
                                    for gg in range(4):
                                        ins = e.matmul(banks[byp][:, 0:TG], lhsT=wbb[:, gg, csl], rhs=yBT[:, gg, tsl], start=(gg == 0), stop=(gg == 3))
                                    return ins
                                S.op("pe", mm_ga, reads=[T_wd2] + hdeps, writes=[Tb[bga]])
                                S.op("act", lambda e, ib=ib, bga=bga: e.activation(out=sga[ib][:], in_=banks[bga][:, 0:TG], func=AF.Sigmoid),
                                     reads=[Tb[bga]], writes=[T_sg[ib]])
                                S.op("pe", mm_gp, reads=[T_wd2] + hdeps, writes=[Tb[bgp]])
                                S.op("act", lambda e, ib=ib, bgp=bgp: e.activation(out=sgp[ib][:], in_=banks[bgp][:, 0:TG], func=AF.Sigmoid),
                                     reads=[Tb[bgp]], writes=[T_sp[ib]])
                                S.op("pe", mm_ya, reads=[T_wd2, T_oT], writes=[Tb[bya]])
                                S.op("dve", lambda e, ib=ib, bya=bya: e.tensor_tensor(out=t1[ib][:], in0=banks[bya][:, 0:TG], in1=sga[ib][:], op=ALU.mult),
                                     reads=[Tb[bya], T_sg[ib]], writes=[T_t1[ib]])
                                S.op("pe", mm_yp, reads=[T_wd2, T_yBT], writes=[Tb[byp]])
                                S.op("dve", lambda e, ib=ib, byp=byp: e.tensor_tensor(out=t2[ib][:], in0=banks[byp][:, 0:TG], in1=sgp[ib][:], op=ALU.mult),
                                     reads=[Tb[byp], T_sp[ib]], writes=[T_t2[ib]])
                                S.op("pool", lambda e, ib=ib, cc=cc, tsl=tsl: e.tensor_tensor(out=mT[:, cc, tsl], in0=t1[ib][:], in1=t2[ib][:], op=ALU.add),
                                     reads=[T_t1[ib], T_t2[ib]], writes=[T_mT[tg]])
                        S.barrier()

                    with ExitStack() as pd3:
                        wo = arena2[:, 0:8192].rearrange("p (c n) -> p c n", c=8)
                        T_wo = Tok()
                        S.dma("pool", wo, w_out.rearrange("(c p) n -> p c n", p=128), "wo", writes=[T_wo])
                        for t in range(NSLOT):
                            S.dma("sp", x1[:, t, :], x_own[t * 128:(t + 1) * 128, :], f"x1_{t % 4}", writes=[T_x1[t]])
                        tres = [sbuf(pd3, f"tres{i}", [128, 512], F32) for i in range(2)]
                        T_tres = [Tok() for _ in range(2)]
                        oi = 0
                        for slot in range(NSLOT):
                            tg = slot // TPG
                            for half in range(2):
                                ob_ = oi % 4
                                rb_ = oi % 2
                                oi += 1

                                def mm_o(e, slot=slot, half=half, ob_=ob_):
                                    for kc in range(8):
                                        ins = e.matmul(banks[ob_][:], lhsT=mT[:, kc, slot * 128:(slot + 1) * 128],
                                                       rhs=wo[:, kc, half * 512:(half + 1) * 512], start=(kc == 0), stop=(kc == 7))
                                    return ins
                                S.op("pe", mm_o, reads=[T_wo, T_mT[tg]], writes=[Tb[ob_]])
                                S.op("dve", lambda e, half=half, ob_=ob_, rb_=rb_: e.tensor_tensor(out=tres[rb_][:], in0=banks[ob_][:],
                                                                                                  in1=gate1_bc[:, half * 512:(half + 1) * 512], op=ALU.mult),
                                     reads=[Tb[ob_], T_mod], writes=[T_tres[rb_]])
                                S.op("pool", lambda e, half=half, rb_=rb_, slot=slot: e.tensor_tensor(
                                    out=x1[:, slot, half * 512:(half + 1) * 512], in0=x1[:, slot, half * 512:(half + 1) * 512], in1=tres[rb_][:], op=ALU.add),
                                    reads=[T_tres[rb_], T_x1[slot]], writes=[T_x1[slot]])
                        S.barrier()

            if dbg:
                d_x1 = nc.dram_tensor("d_x1", [NOWN, D], F32, kind="ExternalOutput").ap()
                for t in range(NSLOT):
                    S.dma("sp", d_x1[t * 128:(t + 1) * 128, :], x1[:, t, :], "dbg", reads=[T_x1[t]])
                S.barrier()

            I32 = mybir.dt.int32
            IOA = bass.IndirectOffsetOnAxis
            XROWS = NE * CAPR
            with ExitStack() as pe_:
                NS = NSLOT
                idx_i = sbuf(pe_, "idx_i", [128, 2, NS], I32)
                wts = sbuf(pe_, "wts", [128, 2, NS], F32)
                cnt_i = sbuf(pe_, "cnt_i", [128, 16], I32)
                zidx_i = sbuf(pe_, "zidx_i", [128, 16], I32)
                T_idx = Tok()
                T_sc = []
                wgt = [sbuf(pe_, "wgt0", [128, 8, DE], BF16)]
                wut = [sbuf(pe_, "wut0", [128, 8, DE], BF16)]
                wdt = [sbuf(pe_, "wdt0", [128, 4, D], BF16)]
                T_we = [Tok() for _ in range(2)]

                def load_expert(ei):
                    b = ei % 2
                    S.dma("pool", wgt[b][:], e_wg[ei].rearrange("(c p) n -> p c n", p=128), f"we{b}", writes=[T_we[b]])
                    S.dma("pool", wut[b][:], e_wu[ei].rearrange("(c p) n -> p c n", p=128), f"we{b}", writes=[T_we[b]])
                    S.dma("pool", wdt[b][:], e_wd[ei].rearrange("(c p) n -> p c n", p=128), f"we{b}", writes=[T_we[b]])
                load_expert(0)
                with ExitStack() as pn:
                    h2tm = sbuf(pn, "h2tm", [128, NS, D], BF16)
                    T_h2tm = [Tok() for _ in range(NS)]
                    logits = sbuf(pn, "logits", [128, NS, 20], F32)
                    T_lg = Tok()
                    xh2 = [sbuf(pn, f"xh2_{i}", [128, D], F32) for i in range(3)]
                    T_xh2 = [Tok() for _ in range(3)]
                    hrow = [sbuf(pn, f"hrow{i}", [128, D], F32) for i in range(2)]
                    T_hrow = [Tok() for _ in range(2)]
                    junk2 = sbuf(pn, "junk2", [128, D], BF16)
                    T_junk2 = Tok()
                    ss2 = [sbuf(pn, f"ss2_{i}", [128, 1], F32) for i in range(3)]
                    T_ss2 = [Tok() for _ in range(3)]
                    h2f = [sbuf(pn, f"h2f{i}", [128, 8, 128], F32) for i in range(2)]
                    T_h2f = [Tok() for _ in range(2)]
                    zt = sbuf(pn, "zt", [128, D], BF16)
                    T_zt = Tok()
                    gs2_bc = sbuf(pn, "gs2_bc", [128, D], F32)
                    sh2_bc = sbuf(pn, "sh2_bc", [128, D], F32)
                    T_bc2 = Tok()
                    S.dma("sp", gs2_bc[:], modd[0:1, :].broadcast_to([128, D]), "bc2", reads=[T_modd], writes=[T_bc2])
                    S.dma("sp", sh2_bc[:], modd[1:2, :].broadcast_to([128, D]), "bc2", reads=[T_modd], writes=[T_bc2])
                    S.op("pool", lambda e: e.memset(zt[:], 0.0), writes=[T_zt])

                    def n2_stage1(t):
                        b2 = t % 3
                        S.op("act", lambda e: e.activation(out=junk2[:], in_=x1[:, t, :], func=AF.Square, accum_out=ss2[b2][:]),
                             reads=[T_x1[t]], writes=[T_junk2, T_ss2[b2]])
                        S.op("act", lambda e: e.activation(out=ss2[b2][:], in_=ss2[b2][:], func=AF.Sqrt, bias=eps_t[:], scale=1.0 / D),
                             reads=[T_ss2[b2], T_const], writes=[T_ss2[b2]])
                        S.op("dve", lambda e: e.reciprocal(out=ss2[b2][:], in_=ss2[b2][:]), reads=[T_ss2[b2]], writes=[T_ss2[b2]])
                        S.op("dve", lambda e: e.tensor_scalar(out=xh2[b2][:], in0=x1[:, t, :], scalar1=ss2[b2][:, 0:1], scalar2=None, op0=ALU.mult),
                             reads=[T_x1[t], T_ss2[b2]], writes=[T_xh2[b2]])

                    def n2_stage2(t):
                        b2 = t % 3
                        bq = t % 2
                        pa_, pb_ = (0, 1) if bq == 0 else (2, 3)

                        def tr2(e):
                            for kc in range(8):
                                bk = pa_ if kc < 4 else pb_
                                ins = e.transpose(out=banks[bk][:, (kc % 4) * 128:(kc % 4 + 1) * 128], in_=xh2[b2][:, kc * 128:(kc + 1) * 128],
                                                  identity=ident_f[:])
                            return ins
                        S.op("pe", tr2, reads=[T_xh2[b2], T_const], writes=[Tb[pa_], Tb[pb_]])
                        S.op("pool", lambda e: e.tensor_tensor(out=hrow[bq][:], in0=xh2[b2][:], in1=gs2_bc[:], op=ALU.mult),
                             reads=[T_xh2[b2], T_bc2], writes=[T_hrow[bq]])
                        S.op("pool", lambda e: e.tensor_tensor(out=h2tm[:, t, :], in0=hrow[bq][:], in1=sh2_bc[:], op=ALU.add),
                             reads=[T_hrow[bq], T_bc2], writes=[T_h2tm[t]])
                        for kc in range(8):
                            bk = pa_ if kc < 4 else pb_
                            S.op("act", lambda e, kc=kc, bk=bk: e.activation(
                                out=h2f[bq][:, kc, :], in_=banks[bk][:, (kc % 4) * 128:(kc % 4 + 1) * 128], func=AF.Identity,
                                bias=modT[:, 16 + kc:17 + kc], scale=gs2[:, kc:kc + 1]), reads=[Tb[bk], T_mod], writes=[T_h2f[bq]])

                        def mmr(e):
                            for kc in range(8):
                                ins = e.matmul(banks[4 + bq][:, 0:20], lhsT=h2f[bq][:, kc, :], rhs=wr_f[:, kc, :], start=(kc == 0), stop=(kc == 7))
                            return ins
                        S.op("pe", mmr, reads=[T_h2f[bq], T_const], writes=[Tb[4 + bq]])
                        S.op("dve", lambda e: e.tensor_tensor(out=logits[:, t, :], in0=banks[4 + bq][:, 0:20], in1=rbias[:], op=ALU.add),
                             reads=[Tb[4 + bq], T_const], writes=[T_lg])

                    for t in range(min(2, NS)):
                        n2_stage1(t)
                    for t in range(NS):
                        if t + 2 < NS:
                            n2_stage1(t + 2)
                        n2_stage2(t)

                    r1 = sbuf(pn, "r1", [128, NS, 16], F32)
                    r2 = sbuf(pn, "r2", [128, NS, 16], F32)
                    r3 = sbuf(pn, "r3", [128, NS, 16], F32)
                    oh1 = sbuf(pn, "oh1", [128, NS, 16], F32)
                    oh2 = sbuf(pn, "oh2", [128, NS, 16], F32)
                    Mb = sbuf(pn, "Mb", [128, NS, 16], BF16)
                    tot = sbuf(pn, "tot", [128, NS, 16], F32)
                    off = sbuf(pn, "off", [128, NS, 16], F32)
                    posb = sbuf(pn, "posb", [128, NS, 16], F32)
                    idxf = sbuf(pn, "idxf", [128, 2, NS], F32)
                    cntf = sbuf(pn, "cntf", [128, 16], F32)
                    zf = sbuf(pn, "zf", [128, 16], F32)
                    pen = sbuf(pn, "pen", [128, NS, 4], F32)
                    elc = sbuf(pn, "elc", [128, NS, 16], F32)
                    gmx = sbuf(pn, "gmx", [128, NS], F32)
                    gsm = sbuf(pn, "gsm", [128, NS], F32)
                    m1 = sbuf(pn, "m1", [128, NS], F32)
                    m2 = sbuf(pn, "m2", [128, NS], F32)
                    w1 = sbuf(pn, "w1", [128, NS], F32)
                    w2 = sbuf(pn, "w2", [128, NS], F32)
                    T_r = Tok()
                    X = mybir.AxisListType.X
                    gl = logits[:, :, 0:4]
                    el = logits[:, :, 4:20]

                    def R(fn, eng="dve", extra=()):
                        S.op(eng, fn, reads=[T_r, T_lg, T_const] + list(extra), writes=[T_r] + list(extra))
                    R(lambda e: e.tensor_reduce(out=gmx[:], in_=gl, axis=X, op=ALU.max))
                    R(lambda e: e.tensor_tensor(out=r1[:, :, 0:4], in0=gl, in1=gmx[:].unsqueeze(2).broadcast_to([128, NS, 4]), op=ALU.subtract))
                    R(lambda e: e.activation(out=r2[:, :, 0:4], in_=r1[:, :, 0:4], func=AF.Exp), eng="act")
                    R(lambda e: e.tensor_reduce(out=gsm[:], in_=r2[:, :, 0:4], axis=X, op=ALU.add))
                    R(lambda e: e.reciprocal(out=gsm[:], in_=gsm[:]))
                    R(lambda e: e.tensor_scalar(out=pen[:], in0=r1[:, :, 0:4], scalar1=0.0, scalar2=None, op0=ALU.is_lt))
                    R(lambda e: e.tensor_copy(out=elc[:], in_=el))
                    R(lambda e: e.scalar_tensor_tensor(out=r3[:].rearrange("p s (g k) -> p (s g) k", g=4),
                                                       in0=pen[:].rearrange("p s g -> p (s g)").unsqueeze(2).broadcast_to([128, NS * 4, 4]), scalar=NEG,
                                                       in1=elc[:].rearrange("p s (g k) -> p (s g) k", g=4), op0=ALU.mult, op1=ALU.add))
                    R(lambda e: e.tensor_reduce(out=m1[:], in_=r3[:], axis=X, op=ALU.max))
                    R(lambda e: e.tensor_tensor(out=oh1[:], in0=r3[:], in1=m1[:].unsqueeze(2).broadcast_to([128, NS, 16]), op=ALU.is_ge))
                    R(lambda e: e.scalar_tensor_tensor(out=r1[:], in0=oh1[:], scalar=NEG, in1=r3[:], op0=ALU.mult, op1=ALU.add))
                    R(lambda e: e.tensor_reduce(out=m2[:], in_=r1[:], axis=X, op=ALU.max))
                    R(lambda e: e.tensor_tensor(out=oh2[:], in0=r1[:], in1=m2[:].unsqueeze(2).broadcast_to([128, NS, 16]), op=ALU.is_ge))
                    R(lambda e: e.tensor_tensor(out=m2[:], in0=m2[:], in1=m1[:], op=ALU.subtract))
                    R(lambda e: e.activation(out=w2[:], in_=m2[:], func=AF.Sigmoid), eng="act")
                    R(lambda e: e.tensor_scalar(out=w1[:], in0=w2[:], scalar1=-1.0, scalar2=1.0, op0=ALU.mult, op1=ALU.add))
                    R(lambda e: e.tensor_tensor(out=wts[:, 0, :], in0=w1[:], in1=gsm[:], op=ALU.mult), extra=[T_idx])
                    R(lambda e: e.tensor_tensor(out=wts[:, 1, :], in0=w2[:], in1=gsm[:], op=ALU.mult), extra=[T_idx])
                    R(lambda e: e.tensor_tensor(out=Mb[:], in0=oh1[:], in1=oh2[:], op=ALU.add))
                    Mflat = Mb[:].rearrange("p s e -> p (s e)")
                    S.op("pe", lambda e: e.matmul(banks[6][:, 0:NS * 16], lhsT=ustrict_b[:], rhs=Mflat, start=True, stop=True),
                         reads=[T_r, T_const], writes=[Tb[6]])
                    S.op("pe", lambda e: e.matmul(banks[7][:, 0:NS * 16], lhsT=ones_b[:], rhs=Mflat, start=True, stop=True),
                         reads=[T_r, T_const], writes=[Tb[7]])
                    S.op("dve", lambda e: e.tensor_copy(out=tot[:].rearrange("p s e -> p (s e)"), in_=banks[7][:, 0:NS * 16]),
                         reads=[Tb[7], T_r], writes=[T_r])
                    R(lambda e: e.memset(off[:, 0, :], 0.0))
                    for t in range(1, NS):
                        R(lambda e, t=t: e.tensor_tensor(out=off[:, t, :], in0=off[:, t - 1, :], in1=tot[:, t - 1, :], op=ALU.add))
                    S.op("dve", lambda e: e.tensor_tensor(out=posb[:].rearrange("p s e -> p (s e)"), in0=banks[6][:, 0:NS * 16],
                                                          in1=off[:].rearrange("p s e -> p (s e)"), op=ALU.add),
                         reads=[Tb[6], T_r], writes=[T_r])
                    R(lambda e: e.tensor_tensor(out=posb[:], in0=posb[:], in1=ebase[:, 0:16].unsqueeze(1).broadcast_to([128, NS, 16]), op=ALU.add))
                    R(lambda e: e.tensor_tensor(out=r1[:], in0=oh1[:], in1=posb[:], op=ALU.mult))
                    R(lambda e: e.tensor_reduce(out=idxf[:, 0, :], in_=r1[:], axis=X, op=ALU.add))
                    R(lambda e: e.tensor_tensor(out=r2[:], in0=oh2[:], in1=posb[:], op=ALU.mult))
                    R(lambda e: e.tensor_reduce(out=idxf[:, 1, :], in_=r2[:], axis=X, op=ALU.add))
                    R(lambda e: e.tensor_copy(out=idx_i[:], in_=idxf[:]), extra=[T_idx])
                    R(lambda e: e.tensor_tensor(out=cntf[:], in0=off[:, NS - 1, :], in1=tot[:, NS - 1, :], op=ALU.add))
                    R(lambda e: e.tensor_copy(out=cnt_i[:], in_=cntf[:]), extra=[T_idx])
                    R(lambda e: e.scalar_tensor_tensor(out=zf[:], in0=cntf[:], scalar=ebase[:, 16:17], in1=ebase[:, 0:16], op0=ALU.add, op1=ALU.add))
                    R(lambda e: e.tensor_copy(out=zidx_i[:], in_=zf[:]), extra=[T_idx])
                    for ei in range(NE):
                        tk = Tok()
                        T_sc.append(tk)
                        S.indirect("zf", [T_zt, T_idx], [tk], out=Xs, out_offset=IOA(ap=zidx_i[:, ei:ei + 1], axis=0), in_=zt[:], in_offset=None)
                    for t in range(NS):
                        for k in range(2):
                            tk = Tok()
                            T_sc.append(tk)
                            S.indirect("sc", [T_h2tm[t], T_idx], [tk], out=Xs, out_offset=IOA(ap=idx_i[:, k, t:t + 1], axis=0), in_=h2tm[:, t, :],
                                       in_offset=None)
                    S.barrier()

                if dbg:
                    d_idx = nc.dram_tensor("d_idx", [128, 2 * NS], I32, kind="ExternalOutput").ap()
                    S.dma("sp", d_idx, idx_i[:].rearrange("p k s -> p (k s)"), "dbg", reads=[T_idx])
                    d_cnt = nc.dram_tensor("d_cnt", [128, 16], I32, kind="ExternalOutput").ap()
                    S.dma("sp", d_cnt, cnt_i[:], "dbg", reads=[T_idx])
                    d_w = nc.dram_tensor("d_w", [128, 2 * NS], F32, kind="ExternalOutput").ap()
                    S.dma("sp", d_w, wts[:].rearrange("p k s -> p (k s)"), "dbg", reads=[T_idx])

                wgt.append(sbuf(pe_, "wgt1", [128, 8, DE], BF16))
                wut.append(sbuf(pe_, "wut1", [128, 8, DE], BF16))
                wdt.append(sbuf(pe_, "wdt1", [128, 4, D], BF16))
                load_expert(1)
                xg = [sbuf(pe_, f"xg{i}", [128, D], BF16) for i in range(3)]
                T_xg = [Tok() for _ in range(3)]
                xgT = [sbuf(pe_, f"xgT{i}", [128, 8, 128], BF16) for i in range(2)]
                T_xgT = [Tok() for _ in range(2)]
                sgl = [sbuf(pe_, f"sgl{i}", [128, 512], BF16) for i in range(2)]
                T_sgl = [Tok() for _ in range(2)]
                hidT = [sbuf(pe_, f"hidT{i}", [128, 4, 128], BF16) for i in range(2)]
                T_hid = [Tok() for _ in range(2)]
                hid_tm = [sbuf(pe_, f"hid_tm{i}", [128, 512], BF16) for i in range(2)]
                T_htm = [Tok() for _ in range(2)]
                ysb = [sbuf(pe_, f"ysb{i}", [128, D], F32) for i in range(2)]
                T_ysb = [Tok() for _ in range(2)]
                T_ys = [Tok() for _ in range(2)]
                regsets = [bass.RegisterHandles([S.engs[e].alloc_register(f"necnt{i}_" + e) for e in S.engs]) for i in range(2)]
                tile_ctr = [0]

                def tile_body(ei, ti):
                    n = tile_ctr[0]
                    tile_ctr[0] += 1
                    wb = ei % 2
                    b3, b2 = n % 3, n % 2
                    row0 = ei * CAPR + ti * 128
                    S.dma("sp", xg[b3][:], Xs[row0:row0 + 128, :], f"xg{b3}", reads=T_sc, writes=[T_xg[b3]])
                    pbT = n % 2
                    pbf = bank_bf(pbT)

                    def trx(e):
                        for kc in range(8):
                            ins = e.transpose(out=pbf[:, kc * 128:(kc + 1) * 128], in_=xg[b3][:, kc * 128:(kc + 1) * 128], identity=ident_b[:])
                        return ins
                    S.op("pe", trx, reads=[T_xg[b3], T_const], writes=[Tb[pbT]])
                    S.op("act", lambda e: e.copy(out=xgT[b2][:, 0:4, :].rearrange("p c n -> p (c n)"), in_=pbf[:, 0:512]),
                         reads=[Tb[pbT]], writes=[T_xgT[b2]])
                    S.op("dve", lambda e: e.tensor_copy(out=xgT[b2][:, 4:8, :].rearrange("p c n -> p (c n)"), in_=pbf[:, 512:1024]),
                         reads=[Tb[pbT]], writes=[T_xgT[b2]])
                    pg, pu = 2 + b2, 4 + b2

                    def mm_gate(e):
                        for kc in range(8):
                            ins = e.matmul(banks[pg][:], lhsT=xgT[b2][:, kc, :], rhs=wgt[wb][:, kc, :], start=(kc == 0), stop=(kc == 7))
                        return ins

                    def mm_up(e):
                        for kc in range(8):
                            ins = e.matmul(banks[pu][:], lhsT=xgT[b2][:, kc, :], rhs=wut[wb][:, kc, :], start=(kc == 0), stop=(kc == 7))
                        return ins
                    S.op("pe", mm_gate, reads=[T_we[wb], T_xgT[b2]], writes=[Tb[pg]])
                    S.op("act", lambda e: e.activation(out=sgl[b2][:], in_=banks[pg][:], func=AF.Silu), reads=[Tb[pg]], writes=[T_sgl[b2]])
                    S.op("pe", mm_up, reads=[T_we[wb], T_xgT[b2]], writes=[Tb[pu]])
                    S.op("dve", lambda e: e.tensor_tensor(out=hid_tm[b2][:], in0=banks[pu][:], in1=sgl[b2][:], op=ALU.mult),
                         reads=[Tb[pu], T_sgl[b2]], writes=[T_htm[b2]])
                    pbf2 = bank_bf(pbT)

                    def trh(e):
                        for fc in range(4):
                            ins = e.transpose(out=pbf2[:, fc * 128:(fc + 1) * 128], in_=hid_tm[b2][:, fc * 128:(fc + 1) * 128], identity=ident_b[:])
                        return ins
                    S.op("pe", trh, reads=[T_htm[b2], T_const], writes=[Tb[pbT]])
                    S.op("act", lambda e: e.copy(out=hidT[b2][:].rearrange("p c n -> p (c n)"), in_=pbf2[:, 0:512]),
                         reads=[Tb[pbT]], writes=[T_hid[b2]])
                    for half in range(2):
                        py = 6 + half

                        def mm_y(e, half=half, py=py):
                            for fc in range(4):
                                ins = e.matmul(banks[py][:], lhsT=hidT[b2][:, fc, :], rhs=wdt[wb][:, fc, half * 512:(half + 1) * 512],
                                               start=(fc == 0), stop=(fc == 3))
                            return ins
                        S.op("pe", mm_y, reads=[T_we[wb], T_hid[b2]], writes=[Tb[py]])
                        if half == 0:
                            S.op("act", lambda e, py=py: e.copy(out=ysb[b2][:, 0:512], in_=banks[py][:]), reads=[Tb[py]], writes=[T_ysb[b2]])
                        else:
                            S.op("dve", lambda e, py=py: e.tensor_copy(out=ysb[b2][:, 512:1024], in_=banks[py][:]), reads=[Tb[py]], writes=[T_ysb[b2]])
                    S.dma("pool", Ys[row0:row0 + 128, :], ysb[b2][:], f"ys{b2}", reads=[T_ysb[b2]], writes=[T_ys[b2]])

                for e_ in S.engs:
                    S._deps(e_, [T_idx], [])
                nc.regs_load(regsets[0], cnt_i[0:1, 0:1])
                for ei in range(NE):
                    regs = regsets[ei % 2]
                    if ei + 1 < NE:
                        nc.regs_load(regsets[(ei + 1) % 2], cnt_i[0:1, ei + 1:ei + 2])
                    def nest(ti, ei=ei, regs=regs):
                        tile_body(ei, ti)
                        if ti + 1 < NS:
                            S.cond_region(regs, (ti + 1) * 128, lambda: nest(ti + 1))
                    S.cond_region(regs, 0, lambda: nest(0))
                    if ei + 2 < NE:
                        load_expert(ei + 2)

                NGB = 3
                yA = [sbuf(pe_, f"yA{i}", [128, D], F32) for i in range(NGB)]
                yB = [sbuf(pe_, f"yB{i}", [128, D], F32) for i in range(NGB)]
                T_yA = [Tok() for _ in range(NGB)]
                T_yB = [Tok() for _ in range(NGB)]
                T_out = Tok()

                def gather(t):
                    b = t % NGB
                    S.indirect(f"ga{b}", T_ys + [T_idx], [T_yA[b]], out=yA[b][:], out_offset=None, in_=Ys,
                               in_offset=IOA(ap=idx_i[:, 0, t:t + 1], axis=0))
                    S.indirect(f"gb{b}", T_ys + [T_idx], [T_yB[b]], out=yB[b][:], out_offset=None, in_=Ys,
                               in_offset=IOA(ap=idx_i[:, 1, t:t + 1], axis=0))
                for t in range(min(NGB - 1, NS)):
                    gather(t)
                for t in range(NS):
                    b = t % NGB
                    if t + NGB - 1 < NS:
                        gather(t + NGB - 1)
                    S.op("dve", lambda e, b=b, t=t: e.tensor_scalar(out=yA[b][:], in0=yA[b][:], scalar1=wts[:, 0, t:t + 1], scalar2=None, op0=ALU.mult),
                         reads=[T_yA[b], T_idx], writes=[T_yA[b]])
                    S.op("dve", lambda e, b=b, t=t: e.scalar_tensor_tensor(out=yB[b][:], in0=yB[b][:], scalar=wts[:, 1, t:t + 1], in1=yA[b][:],
                                                                           op0=ALU.mult, op1=ALU.add),
                         reads=[T_yA[b], T_yB[b], T_idx], writes=[T_yB[b]])
                    S.op("dve", lambda e, b=b: e.tensor_tensor(out=yB[b][:], in0=yB[b][:], in1=gate2_bc[:], op=ALU.mult),
                         reads=[T_yB[b], T_mod], writes=[T_yB[b]])
                    S.op("dve", lambda e, b=b, t=t: e.tensor_tensor(out=x1[:, t, :], in0=x1[:, t, :], in1=yB[b][:], op=ALU.add),
                         reads=[T_yB[b], T_x1[t]], writes=[T_x1[t]])
                    S.dma("sp", out_own[t * 128:(t + 1) * 128, :], x1[:, t, :], "out", reads=[T_x1[t]], writes=[T_out])
                S.barrier()

        @blk.sync
        def _(_unused):
            with nc.allow_non_contiguous_dma(reason="small one-time parameter layouts"):
                _body()

    return nc


def _own_blocks(r, npairs):
    blocks = []
    for j in range(npairs):
        blocks += [8 * j + r, 8 * j + 7 - r]
    return blocks


def make_in_maps(inputs, S_LEN):
    f = lambda a: np.ascontiguousarray(np.asarray(a, dtype=np.float32))
    x = f(inputs["x"])
    B = x.shape[0]
    npairs = S_LEN // 1024
    shared = {
        "rel_bias": f(inputs["rel_bias"]),
        "ada_w": f(inputs["ada_w"][0]),
        "ada_b": f(inputs["ada_b"][0]).reshape(1, -1),
        "norm1_g": f(inputs["norm1_g"][0]).reshape(1, -1),
        "w_in": f(inputs["w_in"][0]),
        "q_norm_g": f(inputs["q_norm_g"][0]).reshape(1, -1),
        "k_norm_g": f(inputs["k_norm_g"][0]).reshape(1, -1),
        "lam_in": f(np.concatenate([inputs["lambda_q1"][0], inputs["lambda_k1"][0],
                                    inputs["lambda_q2"][0], inputs["lambda_k2"][0]])).reshape(1, -1),
        "subln_g": f(inputs["subln_g"][0]).reshape(1, -1),
        "w_ba": f(inputs["w_branch_attn"][0]),
        "pool_w": f(inputs["pool_w"][0]),
        "pool_scale": f(inputs["pool_scale"][0]).reshape(1, -1),
        "w_bb": f(inputs["w_branch_pool"][0]),
        "w_out": f(inputs["w_out"][0]),
        "norm2_g": f(inputs["norm2_g"][0]).reshape(1, -1),
        "r_w": f(np.concatenate([inputs["router_group_w"][0], inputs["router_expert_w"][0]], axis=1)),
        "r_b": f(np.concatenate([inputs["router_group_b"][0], inputs["router_expert_b"][0]])).reshape(1, -1),
        "e_wg": f(inputs["expert_w_gate"][0]),
        "e_wu": f(inputs["expert_w_up"][0]),
        "e_wd": f(inputs["expert_w_down"][0]),
        "ident_in": np.eye(128, dtype=np.float32),
        "bones_in": np.kron(np.eye(2, dtype=np.float32), np.ones((64, 64), np.float32)),
        "ustrict_in": np.triu(np.ones((128, 128), np.float32), 1),
        "ebase_in": np.concatenate([np.tile(np.arange(16, dtype=np.float32) * (S_LEN // 4 + 128), (128, 1)),
                                    np.arange(128, dtype=np.float32)[:, None]], axis=1),
    }
    c = f(inputs["c"])
    in_maps = []
    metas = []
    for core in range(4 * B):
        b, r = core // 4, core % 4
        blocks = _own_blocks(r, npairs)
        xb = x[b]
        x_own = np.concatenate([xb[k * 128:(k + 1) * 128] for k in blocks], axis=0)
        halo = []
        for k in blocks:
            if k == 0:
                halo.append(np.zeros((16, D), np.float32))
            else:
                halo.append(xb[k * 128 - 16:k * 128])
        x_halo = np.concatenate(halo, axis=0)
        x_kv = xb.reshape(S_LEN // 128, 128, D)[:, ::-1, :].reshape(S_LEN, D)
        oh, mask = _core_tables(r)
        hv, ic = _pool_tables(blocks)
        m = dict(shared)
        m.update({
            "x_own": np.ascontiguousarray(x_own), "x_halo": np.ascontiguousarray(x_halo),
            "x_kv": np.ascontiguousarray(x_kv), "c_row": c[b:b + 1],
            "oh_in": oh, "mask_in": mask, "hv_in": hv, "ic_in": ic,
        })
        in_maps.append(m)
        metas.append((b, blocks))
    return in_maps, metas


def kernel(**inputs):
    x = np.asarray(inputs["x"])
    B, S_LEN, _ = x.shape
    nc = build_program(S_LEN)
    in_maps, metas = make_in_maps(inputs, S_LEN)
    res = run_bass_kernel_spmd(nc, in_maps, core_ids=list(range(len(in_maps))))
    out = np.empty((B, S_LEN, D), np.float32)
    for (b, blocks), r in zip(metas, res.results):
        o = np.asarray(r["out_own"])
        for si, k in enumerate(blocks):
            out[b, k * 128:(k + 1) * 128] = o[si * 128:(si + 1) * 128]
    return out
```

```python
import math
from contextlib import ExitStack

import numpy as np
import concourse.bass as bass
import concourse.mybir as mybir
from concourse.bass_utils import run_bass_kernel_spmd

F32 = mybir.dt.float32
BF16 = mybir.dt.bfloat16
AF = mybir.ActivationFunctionType
ALU = mybir.AluOpType

D = 1024
NEG = -30000.0
EPS = 1e-6
NE = 16
DE = 512


class Tok:
    __slots__ = ("w", "rd")

    def __init__(self):
        self.w = None
        self.rd = {}


class Sched:
    STRICT_SAME = True

    def __init__(self, nc, stack):
        self.nc = nc
        self.stack = stack
        self.engs = {"pe": nc.tensor, "act": nc.scalar, "dve": nc.vector,
                     "pool": nc.gpsimd, "sp": nc.sync}
        self.sem = {k: stack.enter_context(nc.semaphore("sem_" + k)) for k in self.engs}
        self.cnt = {k: 0 for k in self.engs}
        self.seen = {k: {} for k in self.engs}
        self.dma_sems = {}
        self.dma_cnt = {}
        self.issuer = {}

    def _wait(self, e, kind, key, val):
        if kind == "eng":
            if key == e and not (self.STRICT_SAME and e in ("act", "dve", "pool")):
                return
            sem = self.sem[key]
        else:
            sem = self.dma_sems[key]
            val = self.dma_cnt[key]
        k = (kind, key)
        if self.seen[e].get(k, 0) >= val:
            return
        self.engs[e].wait_ge(sem, val)
        self.seen[e][k] = val

    def _deps(self, e, reads, writes):
        need = {}
        for b in reads:
            if b.w is not None:
                k = (b.w[0], b.w[1])
                need[k] = max(need.get(k, 0), b.w[2])
        for b in writes:
            if b.w is not None:
                k = (b.w[0], b.w[1])
                need[k] = max(need.get(k, 0), b.w[2])
            for k, v in b.rd.items():
                need[k] = max(need.get(k, 0), v)
        for (kind, key), val in need.items():
            self._wait(e, kind, key, val)

    def _mark(self, me, reads, writes):
        k = (me[0], me[1])
        for b in reads:
            b.rd[k] = max(b.rd.get(k, 0), me[2])
        for b in writes:
            b.w = me
            b.rd = {}

    def op(self, e, fn, reads=(), writes=()):
        self._deps(e, reads, writes)
        inst = fn(self.engs[e])
        self.cnt[e] += 1
        inst.then_inc(self.sem[e], 1)
        self._mark(("eng", e, self.cnt[e]), reads, writes)

    def dma(self, q, out, in_, sem, reads=(), writes=(), **kw):
        if sem not in self.dma_sems:
            self.dma_sems[sem] = self.stack.enter_context(self.nc.semaphore("dsem_" + sem))
            self.dma_cnt[sem] = 0
        self.issuer[sem] = q
        self._deps(q, reads, writes)
        inst = self.engs[q].dma_start(out=out, in_=in_, **kw)
        inst.then_inc(self.dma_sems[sem], 16)
        self.dma_cnt[sem] += 16
        self._mark(("dma", sem, self.dma_cnt[sem]), reads, writes)

    def indirect(self, sem, reads, writes, **kw):
        q = "pool"
        if sem not in self.dma_sems:
            self.dma_sems[sem] = self.stack.enter_context(self.nc.semaphore("dsem_" + sem))
            self.dma_cnt[sem] = 0
        self.issuer[sem] = q
        self._deps(q, reads, writes)
        inst = self.nc.gpsimd.indirect_dma_start(**kw)
        inst.then_inc(self.dma_sems[sem], 16)
        self.dma_cnt[sem] += 16
        self._mark(("dma", sem, self.dma_cnt[sem]), reads, writes)

    def cond_region(self, regs, thr, body):
        import copy
        snap_cnt = dict(self.cnt)
        snap_d = dict(self.dma_cnt)
        snap_seen = copy.deepcopy(self.seen)
        with self.nc.If_cmp(regs, thr, "IS_GT"):
            body()
        with self.nc.Else():
            for e in self.engs:
                d = self.cnt[e] - snap_cnt[e]
                if d:
                    if snap_cnt[e] > 0:
                        self.engs[e].wait_ge(self.sem[e], snap_cnt[e])
                    self.engs[e].sem_inc(self.sem[e], d)
            for sname, total in self.dma_cnt.items():
                dd = total - snap_d.get(sname, 0)
                if dd:
                    q = self.engs[self.issuer[sname]]
                    if snap_d.get(sname, 0) > 0:
                        q.wait_ge(self.dma_sems[sname], snap_d[sname])
                    q.sem_inc(self.dma_sems[sname], dd)
        self.seen = snap_seen

    def barrier(self):
        for e in self.engs:
            for o in self.engs:
                if o != e and self.cnt[o] > 0:
                    self._wait(e, "eng", o, self.cnt[o])
            for s in self.dma_sems:
                if self.dma_cnt[s] > 0:
                    self._wait(e, "dma", s, self.dma_cnt[s])


def _t5_bucket(rel):
    nb, max_exact = 16, 8
    bucket = np.where(rel > 0, nb, 0)
    n = np.abs(rel)
    n_f = np.maximum(n, max_exact).astype(np.float32)
    large = max_exact + (np.log(n_f / np.float32(max_exact)) / np.float32(math.log(128 / max_exact))
                         * np.float32(nb - max_exact)).astype(np.int32)
    large = np.minimum(large, nb - 1)
    return bucket + np.where(n < max_exact, n, large)


def _core_tables(r):
    oh = np.zeros((32, 16, 256), np.float32)
    mask = np.zeros((128, 16, 128), np.float32)
    n = np.arange(256)
    for m in range(8):
        for s in range(2):
            qb = r if s == 0 else 7 - r
            delta = m - qb
            t = m * 2 + s
            if delta > 0:
                oh[15, t, :] = 1.0
                mask[:, t, :] = NEG
            else:
                rel = 128 * delta + 127 - n
                b = _t5_bucket(rel.astype(np.int32))
                oh[b, t, n] = 1.0
                if delta == 0:
                    mask[0:64, t, 0:64] = NEG
    return oh.reshape(32, 4096), mask.reshape(128, 2048)


def _pool_tables(blocks):
    ns = len(blocks)
    hv = np.ones((128, ns, 16), np.float32)
    ic = np.zeros((128, 4, ns, 16), np.float32)
    for si, blk in enumerate(blocks):
        if blk == 0:
            hv[:, si, :] = 0.0
        for g, w in enumerate((2, 4, 8, 16)):
            t = blk * 128 + np.arange(16)
            ic[:, g, si, :] = 1.0 / np.minimum(t + 1, w).astype(np.float32)
    return hv.reshape(128, ns * 16), ic.reshape(128, 4 * ns * 16)


def build_program(S_LEN, dbg=False):
    NP = S_LEN // 1024
    NSLOT = 2 * NP
    NOWN = NSLOT * 128
    NKB = S_LEN // 128
    TG = min(512, NOWN)
    NTG = NOWN // TG
    TPG = TG // 128

    nc = bass.Bass("TRN2", target_bir_lowering=False)

    def din(name, shape, dt=F32):
        return nc.dram_tensor(name, list(shape), dt, kind="ExternalInput").ap()

    x_own = din("x_own", [NOWN, D])
    x_halo = din("x_halo", [NSLOT * 16, D])
    x_kv = din("x_kv", [S_LEN, D])
    c_row = din("c_row", [1, D])
    rel_bias = din("rel_bias", [32, 4])
    ada_w = din("ada_w", [D, 6 * D])
    ada_b = din("ada_b", [1, 6 * D])
    norm1_g = din("norm1_g", [1, D])
    w_in = din("w_in", [D, 4096])
    q_norm_g = din("q_norm_g", [1, 64])
    k_norm_g = din("k_norm_g", [1, 64])
    lam_in = din("lam_in", [1, 256])
    subln_g = din("subln_g", [1, 128])
    w_ba = din("w_ba", [512, D])
    pool_w = din("pool_w", [4, 128, 128])
    pool_scale = din("pool_scale", [1, 512])
    w_bb = din("w_bb", [512, D])
    w_out = din("w_out", [D, D])
    norm2_g = din("norm2_g", [1, D])
    r_w = din("r_w", [D, 20])
    r_b = din("r_b", [1, 20])
    e_wg = din("e_wg", [NE, D, DE])
    e_wu = din("e_wu", [NE, D, DE])
    e_wd = din("e_wd", [NE, DE, D])
    ident_in = din("ident_in", [128, 128])
    bones_in = din("bones_in", [128, 128])
    oh_in = din("oh_in", [32, 4096])
    mask_in = din("mask_in", [128, 2048])
    hv_in = din("hv_in", [128, NSLOT * 16])
    ic_in = din("ic_in", [128, 4 * NSLOT * 16])
    ustrict_in = din("ustrict_in", [128, 128])
    ebase_in = din("ebase_in", [128, 17])

    out_own = nc.dram_tensor("out_own", [NOWN, D], F32, kind="ExternalOutput").ap()

    KTd = nc.dram_tensor("KTd", [4, 128, S_LEN], BF16).ap()
    Vd = nc.dram_tensor("Vd", [4, 128, NKB * 129], BF16).ap()
    CAPR = NOWN + 128
    Xs = nc.dram_tensor("Xs", [NE * CAPR, D], BF16).ap()
    Ys = nc.dram_tensor("Ys", [NE * CAPR, D], F32).ap()
    modd = nc.dram_tensor("modd", [2, D], F32).ap()
    Gd_t = nc.dram_tensor("Gd", [4, 4096], F32)
    Gd = Gd_t.ap()

    dbg_outs = {}

    with ExitStack() as top:
        S = Sched(nc, top)
        blk = top.enter_context(nc.Block())

        def sbuf(st, name, shape, dt):
            return st.enter_context(nc.sbuf_tensor(name, list(shape), dt))

        banks = [top.enter_context(nc.psum_tensor(f"pb{i}", [128, 512], F32)) for i in range(8)]
        Tb = [Tok() for _ in range(8)]

        def bank_bf(i):
            return banks[i].bitcast(BF16)

        ident_f = sbuf(top, "ident_f", [128, 128], F32)
        ident_b = sbuf(top, "ident_b", [128, 128], BF16)
        bones_b = sbuf(top, "bones_b", [128, 128], BF16)
        ones_row = sbuf(top, "ones_row", [1, 128], F32)
        eps_t = sbuf(top, "eps_t", [128, 1], F32)
        modT = sbuf(top, "modT", [128, 32], F32)
        gs1 = sbuf(top, "gs1", [128, 8], F32)
        gs2 = sbuf(top, "gs2", [128, 8], F32)
        gate1_bc = sbuf(top, "gate1_bc", [128, D], F32)
        gate2_bc = sbuf(top, "gate2_bc", [128, D], F32)
        gq8 = sbuf(top, "gq8", [128, 1], F32)
        gk = sbuf(top, "gk", [128, 1], F32)
        neglam = sbuf(top, "neglam", [128, 1], F32)
        ch_bc = sbuf(top, "ch_bc", [128, 4], F32)
        subg = sbuf(top, "subg", [128, 1], F32)
        pscale = sbuf(top, "pscale", [128, 4], F32)
        rbias = sbuf(top, "rbias", [128, 20], F32)
        wr_f = sbuf(top, "wr_f", [128, 8, 20], F32)
        maskc = sbuf(top, "maskc", [128, 16, 128], F32)
        hv_t = sbuf(top, "hv_t", [128, NSLOT, 16], F32)
        ic_t = sbuf(top, "ic_t", [128, 4, NSLOT, 16], F32)
        ustrict_b = sbuf(top, "ustrict_b", [128, 128], BF16)
        ones_b = sbuf(top, "ones_b", [128, 128], BF16)
        ebase = sbuf(top, "ebase", [128, 17], F32)
        T_const = Tok()
        T_mod = Tok()
        T_modd = Tok()

        ARENA_F = 16896
        arena = sbuf(top, "arena", [128, ARENA_F], F32)
        arena_bf = arena.bitcast(BF16)
        x1 = arena[:, 0:NSLOT * D].rearrange("p (s d) -> p s d", d=D)
        T_x1 = [Tok() for _ in range(NSLOT)]

        def _body():
            with ExitStack() as pa:
                ld = lambda out, in_, **kw: S.dma("sp", out, in_, "cst", writes=[T_const], **kw)
                bones_f = sbuf(pa, "bones_f", [128, 128], F32)
                cT = sbuf(pa, "cT", [128, 8], F32)
                scT = sbuf(pa, "scT", [128, 8], F32)
                g1T = sbuf(pa, "g1T", [128, 8], F32)
                g2T = sbuf(pa, "g2T", [128, 8], F32)
                adab = sbuf(pa, "adab", [1, 6 * D], F32)
                modrow = sbuf(pa, "modrow", [1, 6 * D], F32)
                lamrow = sbuf(pa, "lamrow", [1, 256], F32)
                lamtmp = sbuf(pa, "lamtmp", [1, 128], F32)
                lam2 = sbuf(pa, "lam2", [1, 4], F32)
                one11 = sbuf(pa, "one11", [1, 1], F32)
                rb_t = sbuf(pa, "rb_t", [32, 4], F32)
                oh_t = sbuf(pa, "oh_t", [32, 4096], F32)
                Gs = sbuf(pa, "Gs", [4, 4096], F32)
                adaw = [arena[:, i * 4096:(i + 1) * 4096].rearrange("p (c n) -> p c n", c=8) for i in range(3)]
                T_adaw = [Tok() for _ in range(3)]
                T_tmp = Tok()
                T_row = Tok()

                ustrict_f = sbuf(pa, "ustrict_f", [128, 128], F32)
                g2row = sbuf(pa, "g2row", [1, D], F32)
                gs2row = sbuf(pa, "gs2row", [1, D], F32)
                ld(ident_f[:], ident_in)
                ld(ustrict_f[:], ustrict_in)
                ld(g2row[:], norm2_g)
                ld(ebase[:], ebase_in)
                ld(bones_f[:], bones_in)
                ld(cT[:], c_row.rearrange("o (c p) -> p (o c)", p=128))
                ld(g1T[:], norm1_g.rearrange("o (c p) -> p (o c)", p=128))
                ld(g2T[:], norm2_g.rearrange("o (c p) -> p (o c)", p=128))
                ld(adab[:], ada_b)
                ld(lamrow[:], lam_in)
                ld(rb_t[:], rel_bias)
                ld(oh_t[:], oh_in)
                ld(gq8[0:64, :], q_norm_g.rearrange("o d -> d o"))
                ld(gq8[64:128, :], q_norm_g.rearrange("o d -> d o"))
                ld(gk[0:64, :], k_norm_g.rearrange("o d -> d o"))
                ld(gk[64:128, :], k_norm_g.rearrange("o d -> d o"))
                ld(subg[:], subln_g.rearrange("o d -> d o"))
                ld(pscale[:], pool_scale.rearrange("o (g p) -> p (o g)", p=128))
                ld(ch_bc[:], rel_bias[15:16, :].broadcast_to([128, 4]))
                ld(rbias[:], r_b.broadcast_to([128, 20]))
                ld(wr_f[:], r_w.rearrange("(c p) n -> p c n", p=128))
                ld(maskc[:], mask_in.rearrange("p (t q) -> p t q", q=128))
                ld(hv_t[:], hv_in.rearrange("p (s t) -> p s t", t=16))
                ld(ic_t[:], ic_in.rearrange("p (g s t) -> p g s t", g=4, t=16))

                S.op("dve", lambda e: e.memset(ones_row[:], 1.0), writes=[T_const])
                S.op("dve", lambda e: e.memset(one11[:], 1.0), writes=[T_const])
                S.op("dve", lambda e: e.memset(eps_t[:], EPS), writes=[T_const])
                S.op("dve", lambda e: e.tensor_copy(out=ident_b[:], in_=ident_f[:]), reads=[T_const], writes=[T_const])
                S.op("dve", lambda e: e.tensor_copy(out=bones_b[:], in_=bones_f[:]), reads=[T_const], writes=[T_const])
                S.op("dve", lambda e: e.tensor_copy(out=ustrict_b[:], in_=ustrict_f[:]), reads=[T_const], writes=[T_const])
                S.op("dve", lambda e: e.memset(ones_b[:], 1.0), writes=[T_const])
                S.op("dve", lambda e: e.tensor_scalar(out=gq8[:], in0=gq8[:], scalar1=0.125, scalar2=None, op0=ALU.mult),
                     reads=[T_const], writes=[T_const])
                S.op("dve", lambda e: e.tensor_scalar(out=subg[:], in0=subg[:], scalar1=0.8, scalar2=None, op0=ALU.mult),
                     reads=[T_const], writes=[T_const])
                S.op("act", lambda e: e.activation(out=scT[:], in_=cT[:], func=AF.Silu), reads=[T_const], writes=[T_tmp])

                adaw_v = ada_w.rearrange("(c p) n -> p c n", p=128)
                NPIECE = 12
                for i in range(min(3, NPIECE)):
                    S.dma("sp", adaw[i], adaw_v[:, :, i * 512:(i + 1) * 512], f"adaw{i}", writes=[T_adaw[i]])
                for i in range(NPIECE):
                    bi = i % 3
                    pb = 0 + (i % 2)

                    def mm(e, bi=bi, pb=pb):
                        for kc in range(8):
                            ins = e.matmul(banks[pb][0:1, :], lhsT=scT[:, kc:kc + 1], rhs=adaw[bi][:, kc, :],
                                           start=(kc == 0), stop=(kc == 7))
                        return ins
                    S.op("pe", mm, reads=[T_tmp, T_adaw[bi]], writes=[Tb[pb]])
                    S.op("dve", lambda e, i=i, pb=pb: e.tensor_tensor(out=modrow[:, i * 512:(i + 1) * 512], in0=banks[pb][0:1, :],
                                                                      in1=adab[:, i * 512:(i + 1) * 512], op=ALU.add),
                         reads=[Tb[pb], T_const], writes=[T_row])
                    if i + 3 < NPIECE:
                        S.dma("sp", adaw[bi], adaw_v[:, :, (i + 3) * 512:(i + 4) * 512], f"adaw{bi}", writes=[T_adaw[bi]])

                def mmT(e):
                    for vi, v in enumerate((0, 1, 3, 4)):
                        for kc in range(8):
                            ins = e.matmul(banks[2][:, vi * 8 + kc: vi * 8 + kc + 1],
                                           lhsT=modrow[0:1, v * D + kc * 128: v * D + (kc + 1) * 128],
                                           rhs=one11[:], start=True, stop=True)
                    return ins
                S.op("pe", mmT, reads=[T_row, T_const], writes=[Tb[2]])
                S.op("dve", lambda e: e.tensor_copy(out=modT[:], in_=banks[2][:, 0:32]), reads=[Tb[2]], writes=[T_mod])
                S.op("dve", lambda e: e.scalar_tensor_tensor(out=gs1[:], in0=modT[:, 8:16], scalar=1.0, in1=g1T[:],
                                                             op0=ALU.add, op1=ALU.mult), reads=[T_mod, T_const], writes=[T_mod])
                S.op("dve", lambda e: e.scalar_tensor_tensor(out=gs2[:], in0=modT[:, 24:32], scalar=1.0, in1=g2T[:],
                                                             op0=ALU.add, op1=ALU.mult), reads=[T_mod, T_const], writes=[T_mod])
                for gi, (v, dst) in enumerate(((2, gate1_bc), (5, gate2_bc))):
                    for half in range(2):
                        pb = 3 + half
                        S.op("pe", lambda e, v=v, half=half, pb=pb: e.matmul(
                            banks[pb][:], lhsT=ones_row[:], rhs=modrow[0:1, v * D + half * 512: v * D + (half + 1) * 512],
                            start=True, stop=True), reads=[T_row, T_const], writes=[Tb[pb]])
                        S.op("act", lambda e, dst=dst, half=half, pb=pb: e.copy(out=dst[:, half * 512:(half + 1) * 512], in_=banks[pb][:]),
                             reads=[Tb[pb]], writes=[T_mod])
                S.op("dve", lambda e: e.scalar_tensor_tensor(out=gs2row[:], in0=modrow[0:1, 4 * D:5 * D], scalar=1.0, in1=g2row[:],
                                                             op0=ALU.add, op1=ALU.mult), reads=[T_row, T_const], writes=[T_tmp])
                S.dma("sp", modd[0:1, :], gs2row[:], "modd", reads=[T_tmp], writes=[T_modd])
                S.dma("sp", modd[1:2, :], modrow[0:1, 3 * D:4 * D], "modd", reads=[T_row], writes=[T_modd])
                S.op("dve", lambda e: e.tensor_tensor(out=lamtmp[:].rearrange("o (a d) -> o a d", a=2),
                                                      in0=lamrow[:].rearrange("o (a t d) -> o a t d", a=2, t=2)[:, :, 0, :],
                                                      in1=lamrow[:].rearrange("o (a t d) -> o a t d", a=2, t=2)[:, :, 1, :],
                                                      op=ALU.mult), reads=[T_const], writes=[T_tmp])
                S.op("dve", lambda e: e.reduce_sum(out=lam2[:, 0:2], in_=lamtmp[:].rearrange("o (a d) -> o a d", a=2),
                                                   axis=mybir.AxisListType.X), reads=[T_tmp], writes=[T_tmp])
                S.op("act", lambda e: e.activation(out=lam2[:, 0:2], in_=lam2[:, 0:2], func=AF.Exp), reads=[T_tmp], writes=[T_tmp])
                S.op("dve", lambda e: e.tensor_tensor(out=lam2[:, 2:3], in0=lam2[:, 1:2], in1=lam2[:, 0:1], op=ALU.subtract),
                     reads=[T_tmp], writes=[T_tmp])
                S.op("dve", lambda e: e.tensor_scalar(out=lam2[:, 3:4], in0=lam2[:, 2:3], scalar1=-0.2, scalar2=None, op0=ALU.add),
                     reads=[T_tmp], writes=[T_tmp])
                S.op("pe", lambda e: e.matmul(banks[5][:, 0:1], lhsT=ones_row[:], rhs=lam2[:, 3:4], start=True, stop=True),
                     reads=[T_tmp, T_const], writes=[Tb[5]])
                S.op("dve", lambda e: e.tensor_copy(out=neglam[:], in_=banks[5][:, 0:1]), reads=[Tb[5]], writes=[T_const])
                for ci in range(8):
                    pb = 6 + (ci % 2)
                    S.op("pe", lambda e, ci=ci, pb=pb: e.matmul(banks[pb][0:4, :], lhsT=rb_t[:], rhs=oh_t[:, ci * 512:(ci + 1) * 512],
                                                               start=True, stop=True), reads=[T_const], writes=[Tb[pb]])
                    S.op("dve", lambda e, ci=ci, pb=pb: e.tensor_copy(out=Gs[:, ci * 512:(ci + 1) * 512], in_=banks[pb][0:4, :]),
                         reads=[Tb[pb]], writes=[T_tmp])
                T_G = Tok()
                S.dma("sp", Gd, Gs[:], "gd", reads=[T_tmp], writes=[T_G])
                S.barrier()

            def norm_tiles(st, tag, src_rows, ntiles, rows_per_tile, dst_fn, dst_tok_fn, after_tile=None):
                LA1 = 2
                NB = LA1 + 2
                NX = LA1 + 1
                xt = [sbuf(st, f"{tag}_x{i}", [128, D], F32) for i in range(NB)]
                xh = [sbuf(st, f"{tag}_xh{i}", [128, D], BF16) for i in range(NX)]
                junk = sbuf(st, f"{tag}_junk", [128, D], BF16)
                ss = [sbuf(st, f"{tag}_ss{i}", [128, 1], F32) for i in range(NX)]
                T_x = [Tok() for _ in range(NB)]
                T_xh = [Tok() for _ in range(NX)]
                T_junk = Tok()
                T_ss = [Tok() for _ in range(NX)]
                R = rows_per_tile
                for t in range(min(NB - 1, ntiles)):
                    S.dma("sp", xt[t % NB][0:R, :], src_rows(t), f"{tag}_x{t % NB}", writes=[T_x[t % NB]])
                def stage1(t):
                    b3, b2 = t % NB, t % NX
                    S.op("act", lambda e, b3=b3, b2=b2: e.activation(out=junk[0:R, :], in_=xt[b3][0:R, :], func=AF.Square,
                                                                     accum_out=ss[b2][0:R, :]),
                         reads=[T_x[b3]], writes=[T_junk, T_ss[b2]])
                    S.op("act", lambda e, b2=b2: e.activation(out=ss[b2][0:R, :], in_=ss[b2][0:R, :], func=AF.Sqrt,
                                                              bias=eps_t[0:R, :], scale=1.0 / D),
                         reads=[T_ss[b2], T_const], writes=[T_ss[b2]])
                    S.op("dve", lambda e, b2=b2: e.reciprocal(out=ss[b2][0:R, :], in_=ss[b2][0:R, :]),
                         reads=[T_ss[b2]], writes=[T_ss[b2]])
                    S.op("dve", lambda e, b3=b3, b2=b2: e.tensor_scalar(out=xh[b2][0:R, :], in0=xt[b3][0:R, :], scalar1=ss[b2][0:R, 0:1],
                                                                        scalar2=None, op0=ALU.mult),
                         reads=[T_x[b3], T_ss[b2]], writes=[T_xh[b2]])

                def stage2(t):
                    b2 = t % NX
                    pb = t % 2
                    pbf = bank_bf(pb)

                    def tr(e, b2=b2, pbf=pbf):
                        for kc in range(8):
                            ins = e.transpose(out=pbf[:, kc * 128: kc * 128 + R], in_=xh[b2][0:R, kc * 128:(kc + 1) * 128],
                                              identity=ident_b[0:R, 0:R])
                        return ins
                    S.op("pe", tr, reads=[T_xh[b2], T_const], writes=[Tb[pb]])
                    act_kcs = [kc for kc in range(8) if kc % 4 != 3]
                    dve_kcs = [kc for kc in range(8) if kc % 4 == 3]

                    def ev_act(e, t=t, pbf=pbf):
                        for kc in act_kcs:
                            ins = e.activation(out=dst_fn(t, kc), in_=pbf[:, kc * 128: kc * 128 + R], func=AF.Identity,
                                               bias=modT[:, kc:kc + 1], scale=gs1[:, kc:kc + 1])
                        return ins

                    def ev_dve(e, t=t, pbf=pbf):
                        for kc in dve_kcs:
                            ins = e.tensor_scalar(out=dst_fn(t, kc), in0=pbf[:, kc * 128: kc * 128 + R], scalar1=gs1[:, kc:kc + 1],
                                                  scalar2=modT[:, kc:kc + 1], op0=ALU.mult, op1=ALU.add)
                        return ins
                    S.op("act", ev_act, reads=[Tb[pb], T_mod], writes=[dst_tok_fn(t)])
                    S.op("dve", ev_dve, reads=[Tb[pb], T_mod], writes=[dst_tok_fn(t)])

                for t in range(min(LA1, ntiles)):
                    stage1(t)
                for t in range(ntiles):
                    if t + NB - 1 < ntiles:
                        tn = t + NB - 1
                        S.dma("sp", xt[tn % NB][0:R, :], src_rows(tn), f"{tag}_x{tn % NB}", writes=[T_x[tn % NB]])
                    if t + LA1 < ntiles:
                        stage1(t + LA1)
                    stage2(t)
                    if after_tile is not None:
                        after_tile(t)

            def wslice(lo, hi):
                return w_in.rearrange("(c p) n -> p c n", p=128)[:, :, lo:hi]

            def qk_norm_sq(raw_bank, sq, T_sq, ncols):
                S.op("act", lambda e: e.activation(out=sq[:, 0:ncols], in_=banks[raw_bank][:, 0:ncols], func=AF.Square),
                     reads=[Tb[raw_bank]], writes=[T_sq])

            def qk_norm_rest(raw_bank, sq, T_sq, ssum_bank, rstd, T_rstd, ncols, gain, outs):
                S.op("pe", lambda e: e.matmul(banks[ssum_bank][:, 0:ncols], lhsT=bones_b[:], rhs=sq[:, 0:ncols], start=True, stop=True),
                     reads=[T_sq, T_const], writes=[Tb[ssum_bank]])
                S.op("act", lambda e: e.activation(out=rstd[:, 0:ncols], in_=banks[ssum_bank][:, 0:ncols], func=AF.Sqrt,
                                                   bias=eps_t[:], scale=1.0 / 64), reads=[Tb[ssum_bank], T_const], writes=[T_rstd])
                S.op("dve", lambda e: e.reciprocal(out=rstd[:, 0:ncols], in_=rstd[:, 0:ncols]), reads=[T_rstd], writes=[T_rstd])
                for dst, plo, phi, T_dst in outs:
                    S.op("dve", lambda e, dst=dst, plo=plo, phi=phi: e.scalar_tensor_tensor(
                        out=dst, in0=banks[raw_bank][plo:phi, 0:ncols], scalar=gain[plo:phi, 0:1], in1=rstd[plo:phi, 0:ncols],
                        op0=ALU.mult, op1=ALU.mult), reads=[Tb[raw_bank], T_rstd, T_const], writes=[T_dst])

            def qk_norm_group(st_tmp, raw_bank, T_raw, sq, T_sq, ssum_bank, rstd, T_rstd, ncols, gain, outs):
                qk_norm_sq(raw_bank, sq, T_sq, ncols)
                qk_norm_rest(raw_bank, sq, T_sq, ssum_bank, rstd, T_rstd, ncols, gain, outs)

            T_KTd = Tok()
            T_Vd = Tok()
            with ExitStack() as pbk:
                wk = sbuf(pbk, "wk", [128, 8, 512], BF16)
                wv = sbuf(pbk, "wv", [128, 8, 512], BF16)
                T_wkv = Tok()
                S.dma("pool", wk[:], wslice(512, 1024), "wkv", writes=[T_wkv])
                S.dma("pool", wv[:], wslice(1024, 1536), "wkv", writes=[T_wkv])
                hTg = [sbuf(pbk, f"hTg{i}", [128, 8, 512], BF16) for i in range(2)]
                T_hTg = [Tok() for _ in range(2)]
                kst = [sbuf(pbk, f"kst{i}", [128, 4, 512], BF16) for i in range(2)]
                T_kst = [Tok() for _ in range(2)]
                vst = [sbuf(pbk, f"vst{i}", [128, 4, 4, 129], BF16) for i in range(2)]
                T_vst = [Tok() for _ in range(2)]
                sqk = [sbuf(pbk, f"sqk{i}", [128, 512], BF16) for i in range(2)]
                T_sqk = [Tok() for _ in range(2)]
                rsk = [sbuf(pbk, f"rsk{i}", [128, 512], F32) for i in range(2)]
                T_rsk = [Tok() for _ in range(2)]
                for i in range(2):
                    S.op("pool", lambda e, i=i: e.memset(vst[i][:], 1.0), writes=[T_vst[i]])
                NG = S_LEN // 512

                NQ = NKB

                def item_A(q):
                    g, h = divmod(q, 4)
                    gb = g % 2
                    rb = 2 + (q % 3)

                    def mmk(e):
                        for kc in range(8):
                            ins = e.matmul(banks[rb][:], lhsT=wk[:, kc, h * 128:(h + 1) * 128], rhs=hTg[gb][:, kc, :],
                                           start=(kc == 0), stop=(kc == 7))
                        return ins
                    S.op("pe", mmk, reads=[T_wkv, T_hTg[gb]], writes=[Tb[rb]])
                    qk_norm_sq(rb, sqk[q % 2], T_sqk[q % 2], 512)
                    vb = 6 + (q % 2)

                    def mmv(e):
                        for kc in range(8):
                            ins = e.matmul(banks[vb][:], lhsT=hTg[gb][:, kc, h * 128:(h + 1) * 128], rhs=wv[:, kc, :],
                                           start=(kc == 0), stop=(kc == 7))
                        return ins
                    S.op("pe", mmv, reads=[T_wkv, T_hTg[gb]], writes=[Tb[vb]])
                    S.op("act", lambda e: e.copy(out=vst[gb][:, h, :, 0:128], in_=banks[vb][:].rearrange("p (h e) -> p h e", h=4)),
                         reads=[Tb[vb]], writes=[T_vst[gb]])

                def item_B(q):
                    sq, T_sq, rstd, T_rstd = sqk[q % 2], T_sqk[q % 2], rsk[q % 2], T_rsk[q % 2]
                    S.op("pe", lambda e: e.matmul(banks[5][:], lhsT=bones_b[:], rhs=sq[:], start=True, stop=True),
                         reads=[T_sq, T_const], writes=[Tb[5]])
                    S.op("act", lambda e: e.activation(out=rstd[:], in_=banks[5][:], func=AF.Sqrt, bias=eps_t[:], scale=1.0 / 64),
                         reads=[Tb[5], T_const], writes=[T_rstd])

                def item_C(q):
                    g, h = divmod(q, 4)
                    gb = g % 2
                    rb = 2 + (q % 3)
                    rstd, T_rstd = rsk[q % 2], T_rsk[q % 2]
                    S.op("dve", lambda e: e.reciprocal(out=rstd[:], in_=rstd[:]), reads=[T_rstd], writes=[T_rstd])
                    S.op("dve", lambda e: e.scalar_tensor_tensor(out=kst[gb][:, h, :], in0=banks[rb][:], scalar=gk[:, 0:1], in1=rstd[:],
                                                                 op0=ALU.mult, op1=ALU.mult), reads=[Tb[rb], T_rstd, T_const], writes=[T_kst[gb]])
                    if h == 3:
                        S.dma("pool", KTd[:, :, g * 512:(g + 1) * 512].rearrange("h p n -> p h n"), kst[gb][:], f"kst{gb}",
                              reads=[T_kst[gb]], writes=[T_KTd])
                        for hh in range(4):
                            S.dma("pool", Vd[hh, :, g * 516:(g + 1) * 516].rearrange("p (i e) -> p i e", e=129), vst[gb][:, :, hh, :], f"vst{gb}",
                                  reads=[T_vst[gb]], writes=[T_Vd])

                def after_tile(t):
                    if 0 <= t - 6 < NQ:
                        item_C(t - 6)
                    if 0 <= t - 5 < NQ:
                        item_B(t - 5)
                    if 0 <= t - 4 < NQ:
                        item_A(t - 4)

                norm_tiles(pbk, "kv", lambda t: x_kv[t * 128:(t + 1) * 128, :], NKB, 128,
                           lambda t, kc: hTg[(t // 4) % 2][:, kc, (t % 4) * 128:(t % 4 + 1) * 128],
                           lambda t: T_hTg[(t // 4) % 2], after_tile)
                for t in range(NKB, NKB + 7):
                    after_tile(t)
                S.barrier()

            with ExitStack() as pown:
                hT_own = sbuf(pown, "hT_own", [128, 8, NOWN], BF16)
                hT_halo = sbuf(pown, "hT_halo", [128, 8, NSLOT * 16], BF16)
                T_hTown = [Tok() for _ in range(NSLOT)]
                T_hThalo = Tok()
                oT = sbuf(pown, "oT", [128, 4, NOWN], BF16)
                T_oT = Tok()
                with ExitStack() as patt:
                    qT = sbuf(patt, "qT", [128, 4, NOWN], BF16)
                    T_qT = Tok()
                    with ExitStack() as pc:
                        wq = sbuf(pc, "wq", [128, 8, 512], BF16)
                        T_wq = Tok()
                        S.dma("pool", wq[:], wslice(0, 512), "wq", writes=[T_wq])
                        sqq = [sbuf(pc, f"sqq{i}", [128, 512], BF16) for i in range(2)]
                        T_sqq = [Tok() for _ in range(2)]
                        rsq = [sbuf(pc, f"rsq{i}", [128, 512], F32) for i in range(2)]
                        T_rsq = [Tok() for _ in range(2)]
                        with ExitStack() as pcn:
                            norm_tiles(pcn, "own", lambda t: x_own[t * 128:(t + 1) * 128, :], NSLOT, 128,
                                       lambda t, kc: hT_own[:, kc, t * 128:(t + 1) * 128], lambda t: T_hTown[t])
                        S.barrier()
                        NHT = (NSLOT * 16 + 127) // 128
                        for t in range(NHT):
                            rows = min(128, NSLOT * 16 - t * 128)
                            with ExitStack() as pcn:
                                norm_tiles(pcn, f"halo{t}", lambda tt, t=t, rows=rows: x_halo[t * 128: t * 128 + rows, :], 1, rows,
                                           lambda tt, kc, t=t, rows=rows: hT_halo[:, kc, t * 128: t * 128 + rows], lambda tt: T_hThalo)
                            S.barrier()
                        for tg in range(NTG):
                            for h in range(4):
                                rb = 2 + (h % 2)

                                def mmq(e, h=h, rb=rb, tg=tg):
                                    for kc in range(8):
                                        ins = e.matmul(banks[rb][:, 0:TG], lhsT=wq[:, kc, h * 128:(h + 1) * 128],
                                                       rhs=hT_own[:, kc, tg * TG:(tg + 1) * TG], start=(kc == 0), stop=(kc == 7))
                                    return ins
                                S.op("pe", mmq, reads=[T_wq] + T_hTown[tg * TPG:(tg + 1) * TPG], writes=[Tb[rb]])
                                qk_norm_group(None, rb, None, sqq[h % 2], T_sqq[h % 2], 4, rsq[h % 2], T_rsq[h % 2], TG, gq8,
                                              [(qT[:, h, tg * TG:(tg + 1) * TG], 0, 128, T_qT)])
                        S.barrier()

                    with ExitStack() as pat:
                        VW = NKB * 129
                        kt_sb = [arena_bf[:, i * S_LEN:(i + 1) * S_LEN] for i in range(2)]
                        v_sb = [arena_bf[:, 2 * S_LEN + i * VW: 2 * S_LEN + (i + 1) * VW].rearrange("p (k e) -> p k e", e=129) for i in range(2)]
                        T_kv = [Tok() for _ in range(2)]
                        qpad = [sbuf(pat, f"qpad{i}", [128, 2, NOWN], BF16) for i in range(2)]
                        T_qpad = [Tok() for _ in range(2)]
                        bT = [sbuf(pat, f"bT{i}", [128, 16, 128], F32) for i in range(2)]
                        T_bT = [Tok() for _ in range(2)]
                        NEB = 3
                        Eb = [sbuf(pat, f"Eb{i}", [128, 2, 256], BF16) for i in range(NEB)]
                        T_E = [Tok() for _ in range(NEB)]
                        tmpb = [sbuf(pat, f"tmpb{i}", [128, 2, 256], F32) for i in range(2)]
                        T_tmpb = [Tok() for _ in range(2)]
                        rs = [sbuf(pat, f"rs{i}", [128, 4], F32) for i in range(2)]
                        T_rs = [Tok() for _ in range(2)]
                        tO = [sbuf(pat, f"tO{i}", [128, 128], F32) for i in range(2)]
                        oO = [sbuf(pat, f"oO{i}", [128, 128], F32) for i in range(2)]
                        on = [sbuf(pat, f"on{i}", [128, 128], BF16) for i in range(2)]
                        jk = sbuf(pat, "att_jk", [128, 128], BF16)
                        ssq = [sbuf(pat, f"ssq{i}", [128, 1], F32) for i in range(2)]
                        T_post = [Tok() for _ in range(2)]
                        T_jk = Tok()
                        for i in range(2):
                            S.op("dve" if i == 0 else "pool", lambda e, i=i: e.memset(qpad[i][:], 0.0), writes=[T_qpad[i]])

                        def load_head(h):
                            hb = h % 2
                            NCH = max(1, S_LEN // 2048)
                            cw = S_LEN // NCH
                            for ci in range(NCH):
                                S.dma("sp", kt_sb[hb][:, ci * cw:(ci + 1) * cw], KTd[h, :, ci * cw:(ci + 1) * cw], f"kv{hb}",
                                      reads=[T_KTd], writes=[T_kv[hb]])
                            S.dma("sp", v_sb[hb].rearrange("p k e -> p (k e)"), Vd[h], f"kv{hb}", reads=[T_Vd], writes=[T_kv[hb]])
                            for t in range(16):
                                src = bass.AP(Gd_t, h * 4096 + t * 256, [[1, 128], [1, 128]])
                                S.dma("sp", bT[hb][:, t, :], src, f"bT{hb}", reads=[T_G], writes=[T_bT[hb]])
                            eng_ = "dve" if h == 0 else "pool"
                            S.op(eng_, lambda e, hb=hb: e.tensor_tensor(out=bT[hb][:], in0=bT[hb][:], in1=maskc[:], op=ALU.add),
                                 reads=[T_bT[hb], T_const], writes=[T_bT[hb]])
                            S.op(eng_, lambda e, hb=hb, h=h: e.tensor_copy(out=qpad[hb][0:64, 0, :], in_=qT[0:64, h, :]),
                                 reads=[T_qT], writes=[T_qpad[hb]])
                            S.op(eng_, lambda e, hb=hb, h=h: e.tensor_copy(out=qpad[hb][64:128, 1, :], in_=qT[64:128, h, :]),
                                 reads=[T_qT], writes=[T_qpad[hb]])

                        steps = []
                        unit = 0
                        for h in range(4):
                            for j in range(NP):
                                nkb = 8 * j + 8
                                ob = 3 + 2 * (unit % 2)
                                unit += 1
                                for kb in range(nkb):
                                    steps.append(dict(h=h, hb=h % 2, j=j, kb=kb, nkb=nkb, ob=ob, q0=256 * j))
                        nsteps = len(steps)
                        loaded = set()

                        def ensure_head(h):
                            if h < 4 and h not in loaded:
                                loaded.add(h)
                                load_head(h)

                        def emit_S(i):
                            st_ = steps[i]
                            h, hb, kb, q0 = st_["h"], st_["hb"], st_["kb"], st_["q0"]
                            ensure_head(h)
                            sbk = i % 3

                            def mms(e):
                                return e.matmul(banks[sbk][:].rearrange("p (m q) -> p m q", m=2), lhsT=kt_sb[hb][:, kb * 128:(kb + 1) * 128],
                                                rhs=qpad[hb][:, :, q0:q0 + 256], start=True, stop=True)
                            S.op("pe", mms, reads=[T_kv[hb], T_qpad[hb]], writes=[Tb[sbk]])

                        def emit_exp(i):
                            st_ = steps[i]
                            h, hb, kb, j = st_["h"], st_["hb"], st_["kb"], st_["j"]
                            sbk = i % 3
                            eb = i % NEB
                            Ev = Eb[eb][:].rearrange("p m q -> p (m q)")
                            if kb < 8 * j:
                                S.op("act", lambda e: e.activation(out=Ev, in_=banks[sbk][:], func=AF.Exp,
                                                                   bias=ch_bc[:, h:h + 1], scale=1.0),
                                     reads=[Tb[sbk], T_const], writes=[T_E[eb]])
                            else:
                                mi = kb - 8 * j
                                tb = i % 2
                                bias_ap = bT[hb][:, 2 * mi:2 * mi + 2, :].rearrange("p s q -> p (s q)").unsqueeze(1).broadcast_to([128, 2, 256])
                                S.op("dve", lambda e: e.scalar_tensor_tensor(
                                    out=tmpb[tb][:], in0=banks[sbk][:].rearrange("p (m q) -> p m q", m=2), scalar=1.0,
                                    in1=bias_ap, op0=ALU.mult, op1=ALU.add),
                                    reads=[Tb[sbk], T_bT[hb]], writes=[T_tmpb[tb]])
                                S.op("act", lambda e: e.activation(out=Ev, in_=tmpb[tb][:].rearrange("p m q -> p (m q)"), func=AF.Exp),
                                     reads=[T_tmpb[tb]], writes=[T_E[eb]])

                        def emit_PV(i):
                            st_ = steps[i]
                            hb, kb, nkb, ob = st_["hb"], st_["kb"], st_["nkb"], st_["ob"]
                            eb = i % NEB

                            def mmo(e):
                                for m in range(2):
                                    for s_ in range(2):
                                        ins = e.matmul(banks[ob + m][:, s_ * 129:(s_ + 1) * 129], lhsT=Eb[eb][:, m, s_ * 128:(s_ + 1) * 128],
                                                       rhs=v_sb[hb][:, kb, :], start=(kb == 0 and s_ == 0), stop=(kb == nkb - 1),
                                                       skip_group_check=True)
                                return ins
                            S.op("pe", mmo, reads=[T_E[eb], T_kv[hb]], writes=[Tb[ob], Tb[ob + 1]])

                        def post_A(h, j, ob):
                            for s_ in range(2):
                                pi = s_
                                c0 = s_ * 129
                                S.op("dve", lambda e, pi=pi, c0=c0: e.tensor_copy(out=rs[pi][:, 0:1], in_=banks[ob][:, c0 + 128: c0 + 129]),
                                     reads=[Tb[ob]], writes=[T_rs[pi]])
                                S.op("dve", lambda e, pi=pi, c0=c0: e.tensor_copy(out=rs[pi][:, 1:2], in_=banks[ob + 1][:, c0 + 128: c0 + 129]),
                                     reads=[Tb[ob + 1]], writes=[T_rs[pi]])
                                S.op("dve", lambda e, pi=pi: e.reciprocal(out=rs[pi][:, 0:2], in_=rs[pi][:, 0:2]), reads=[T_rs[pi]], writes=[T_rs[pi]])
                                S.op("dve", lambda e, pi=pi: e.tensor_tensor(out=rs[pi][:, 2:3], in0=rs[pi][:, 1:2], in1=neglam[:], op=ALU.mult),
                                     reads=[T_rs[pi], T_const], writes=[T_rs[pi]])
                                S.op("dve", lambda e, pi=pi, c0=c0: e.tensor_scalar(out=tO[pi][:], in0=banks[ob + 1][:, c0: c0 + 128],
                                                                                  scalar1=rs[pi][:, 2:3], scalar2=None, op0=ALU.mult),
                                     reads=[Tb[ob + 1], T_rs[pi]], writes=[T_post[pi]])
                                S.op("dve", lambda e, pi=pi, c0=c0: e.scalar_tensor_tensor(out=oO[pi][:], in0=banks[ob][:, c0: c0 + 128],
                                                                                         scalar=rs[pi][:, 0:1], in1=tO[pi][:],
                                                                                         op0=ALU.mult, op1=ALU.add),
                                     reads=[Tb[ob], T_rs[pi], T_post[pi]], writes=[T_post[pi]])
                                S.op("dve", lambda e, pi=pi: e.scalar_tensor_tensor(out=tO[pi][:], in0=oO[pi][:], scalar=1.0, in1=oO[pi][:],
                                                                                  op0=ALU.mult, op1=ALU.mult, accum_out=ssq[pi][:]),
                                     reads=[T_post[pi]], writes=[T_ssq[pi], T_tO2[pi]])

                        def post_B(h, j, ob):
                            for s_ in range(2):
                                pi = s_
                                S.op("act", lambda e, pi=pi: e.activation(out=ssq[pi][:], in_=ssq[pi][:], func=AF.Ln, bias=eps_t[:], scale=1.0 / 128),
                                     reads=[T_ssq[pi], T_const], writes=[T_ssq[pi]])
                                S.op("act", lambda e, pi=pi: e.activation(out=ssq[pi][:], in_=ssq[pi][:], func=AF.Exp, scale=-0.5),
                                     reads=[T_ssq[pi]], writes=[T_ssq[pi]])

                        def post_C(h, j, ob):
                            for s_ in range(2):
                                pi = s_
                                slot = 2 * j + s_
                                S.op("dve", lambda e, pi=pi: e.tensor_scalar(out=on[pi][:], in0=oO[pi][:], scalar1=ssq[pi][:, 0:1], scalar2=None, op0=ALU.mult),
                                     reads=[T_post[pi], T_ssq[pi]], writes=[T_on[pi]])
                                tbk = 7
                                tbf = bank_bf(tbk)
                                S.op("pe", lambda e, pi=pi, tbf=tbf: e.transpose(out=tbf[:, pi * 128:(pi + 1) * 128], in_=on[pi][:], identity=ident_b[:]),
                                     reads=[T_on[pi], T_const], writes=[Tb[tbk]])
                                S.op("dve", lambda e, slot=slot, tbf=tbf, pi=pi: e.tensor_scalar(out=oT[:, h, slot * 128:(slot + 1) * 128],
                                                                                               in0=tbf[:, pi * 128:(pi + 1) * 128],
                                                                                               scalar1=subg[:, 0:1], scalar2=None, op0=ALU.mult),
                                     reads=[Tb[tbk], T_const], writes=[T_oT])

                        T_ssq = [Tok() for _ in range(2)]
                        T_tO2 = T_post
                        T_on = [Tok() for _ in range(2)]
                        deferred = []
                        LA = 2
                        for i in range(min(LA, nsteps)):
                            emit_S(i)
                        for i in range(nsteps):
                            while deferred and deferred[0][0] <= i:
                                deferred.pop(0)[1]()
                            if steps[i]["kb"] == 0 and steps[i]["j"] == 0:
                                ensure_head(steps[i]["h"] + 1)
                            if i + LA < nsteps:
                                emit_S(i + LA)
                            emit_exp(i)
                            emit_PV(i)
                            st_ = steps[i]
                            if st_["kb"] == st_["nkb"] - 1:
                                h_, j_, ob_ = st_["h"], st_["j"], st_["ob"]
                                post_A(h_, j_, ob_)
                                deferred.append((i + 3, lambda h_=h_, j_=j_, ob_=ob_: post_B(h_, j_, ob_)))
                                deferred.append((i + 5, lambda h_=h_, j_=j_, ob_=ob_: post_C(h_, j_, ob_)))
                                deferred.sort(key=lambda x: x[0])
                        while deferred:
                            deferred.pop(0)[1]()
                        S.barrier()

                    if dbg:
                        d_oT = nc.dram_tensor("d_oT", [128, 4 * NOWN], BF16, kind="ExternalOutput").ap()
                        S.dma("sp", d_oT, oT[:].rearrange("p h n -> p (h n)"), "dbg", reads=[T_oT])
                        d_hT = nc.dram_tensor("d_hT", [128, 8 * NOWN], BF16, kind="ExternalOutput").ap()
                        S.dma("sp", d_hT, hT_own[:].rearrange("p c n -> p (c n)"), "dbg", reads=T_hTown)
                        d_q = nc.dram_tensor("d_q", [128, 4 * NOWN], BF16, kind="ExternalOutput").ap()
                        S.dma("sp", d_q, qT[:].rearrange("p h n -> p (h n)"), "dbg", reads=[T_qT])
                        S.barrier()

                with ExitStack() as pd:
                    arena2 = sbuf(pd, "arena2", [128, 8192], BF16)
                    yBT = arena2[:, 0:4 * NOWN].rearrange("p (g n) -> p g n", g=4)
                    T_yBT = Tok()
                    wga = arena_bf[:, 0:8192].rearrange("p (c n) -> p c n", c=8)
                    wgp = arena_bf[:, 8192:16384].rearrange("p (c n) -> p c n", c=8)
                    wba = arena_bf[:, 16384:20480].rearrange("p (c n) -> p c n", c=4)
                    wbb = arena_bf[:, 20480:24576].rearrange("p (c n) -> p c n", c=4)
                    T_wd = Tok()
                    T_wd2 = Tok()
                    S.dma("pool", wga, wslice(2048, 3072), "wd2", writes=[T_wd2])
                    S.dma("pool", wgp, wslice(3072, 4096), "wd2", writes=[T_wd2])
                    S.dma("pool", wba, w_ba.rearrange("(h p) n -> p h n", p=128), "wd2", writes=[T_wd2])
                    S.dma("pool", wbb, w_bb.rearrange("(h p) n -> p h n", p=128), "wd2", writes=[T_wd2])
                    with ExitStack() as pd1:
                        wu = sbuf(pd1, "wu", [128, 8, 512], BF16)
                        wpl = sbuf(pd1, "wpl", [128, 4, 128], BF16)
                        S.dma("pool", wu[:], wslice(1536, 2048), "wd", writes=[T_wd])
                        S.dma("pool", wpl[:], pool_w.rearrange("g c d -> c g d"), "wd", writes=[T_wd])
                        W = 144
                        ub = [sbuf(pd1, f"ub{i}", [128, NSLOT, W], F32) for i in range(3)]
                        T_ub = [Tok() for _ in range(3)]
                        pooledT = [sbuf(pd1, f"pooledT{i}", [128, NSLOT, 128], BF16) for i in range(2)]
                        T_pl = [Tok() for _ in range(2)]
                        for g in range(4):
                            w = 2 ** (g + 1)
                            u0 = ub[0]
                            for tg in range(NTG):
                                pb = tg % 2

                                def mmu(e, tg=tg, pb=pb, g=g):
                                    for kc in range(8):
                                        ins = e.matmul(banks[pb][:, 0:TG], lhsT=wu[:, kc, g * 128:(g + 1) * 128],
                                                       rhs=hT_own[:, kc, tg * TG:(tg + 1) * TG], start=(kc == 0), stop=(kc == 7))
                                    return ins
                                S.op("pe", mmu, reads=[T_wd] + T_hTown[tg * TPG:(tg + 1) * TPG], writes=[Tb[pb]])
                                S.op("act", lambda e, tg=tg, pb=pb: e.copy(out=u0[:, tg * TPG:(tg + 1) * TPG, 16:W],
                                                                           in_=banks[pb][:, 0:TG].rearrange("p (s t) -> p s t", t=128)),
                                     reads=[Tb[pb]], writes=[T_ub[0]])
                            NH = NSLOT * 16

                            def mmh(e, g=g):
                                for kc in range(8):
                                    ins = e.matmul(banks[2][:, 0:NH], lhsT=wu[:, kc, g * 128:(g + 1) * 128], rhs=hT_halo[:, kc, :],
                                                   start=(kc == 0), stop=(kc == 7))
                                return ins
                            S.op("pe", mmh, reads=[T_wd, T_hThalo], writes=[Tb[2]])
                            S.op("dve", lambda e: e.tensor_tensor(out=u0[:, :, 0:16], in0=banks[2][:, 0:NH].rearrange("p (s t) -> p s t", t=16),
                                                                  in1=hv_t[:], op=ALU.mult), reads=[Tb[2], T_const], writes=[T_ub[0]])
                            cur = 0
                            for k in range(g + 1):
                                sh = 2 ** k
                                nxt = 1 if cur != 1 else 2
                                if k > 0:
                                    pass
                                lo = 2 * sh - 1
                                S.op("dve", lambda e, cur=cur, nxt=nxt, sh=sh, lo=lo: e.tensor_tensor(
                                    out=ub[nxt][:, :, lo:W], in0=ub[cur][:, :, lo:W], in1=ub[cur][:, :, lo - sh:W - sh], op=ALU.add),
                                    reads=[T_ub[cur]], writes=[T_ub[nxt]])
                                cur = nxt
                            pl = pooledT[g % 2]
                            S.op("dve", lambda e, cur=cur, g=g: e.tensor_tensor(out=ub[cur][:, :, 16:32], in0=ub[cur][:, :, 16:32], in1=ic_t[:, g, :, :], op=ALU.mult),
                                 reads=[T_ub[cur], T_const], writes=[T_ub[cur]])
                            S.op("dve", lambda e, cur=cur, pl=pl: e.tensor_tensor(out=pl[:, :, 0:16], in0=ub[cur][:, :, 16:32], in1=u0[:, :, 16:32], op=ALU.subtract),
                                 reads=[T_ub[cur], T_ub[0]], writes=[T_pl[g % 2]])
                            S.op("dve", lambda e, cur=cur, pl=pl, w=w: e.scalar_tensor_tensor(out=pl[:, :, 16:128], in0=ub[cur][:, :, 32:W], scalar=1.0 / w,
                                                                                             in1=u0[:, :, 32:W], op0=ALU.mult, op1=ALU.subtract),
                                 reads=[T_ub[cur], T_ub[0]], writes=[T_pl[g % 2]])
                            for tg in range(NTG):
                                pb = 3 + (tg % 2)
                                S.op("pe", lambda e, tg=tg, pb=pb, pl=pl, g=g: e.matmul(
                                    banks[pb][:, 0:TG], lhsT=wpl[:, g, :], rhs=pl[:, tg * TPG:(tg + 1) * TPG, :].rearrange("p s t -> p (s t)"),
                                    start=True, stop=True), reads=[T_wd, T_pl[g % 2]], writes=[Tb[pb]])
                                S.op("act", lambda e, tg=tg, pb=pb, g=g: e.activation(out=yBT[:, g, tg * TG:(tg + 1) * TG], in_=banks[pb][:, 0:TG],
                                                                                     func=AF.Copy, scale=pscale[:, g:g + 1]),
                                     reads=[Tb[pb], T_const], writes=[T_yBT])
                        S.barrier()

                    mT = sbuf(pd, "mT", [128, 8, NOWN], BF16)
                    T_mT = [Tok() for _ in range(NTG)]
                    with ExitStack() as pd2:
                        sga = [sbuf(pd2, f"sga{i}", [128, TG], BF16) for i in range(2)]
                        sgp = [sbuf(pd2, f"sgp{i}", [128, TG], BF16) for i in range(2)]
                        t1 = [sbuf(pd2, f"t1_{i}", [128, TG], F32) for i in range(2)]
                        t2 = [sbuf(pd2, f"t2_{i}", [128, TG], F32) for i in range(2)]
                        T_sg = [Tok() for _ in range(2)]
                        T_sp = [Tok() for _ in range(2)]
                        T_t1 = [Tok() for _ in range(2)]
                        T_t2 = [Tok() for _ in range(2)]
                        it = 0
                        for tg in range(NTG):
                            tsl = slice(tg * TG, (tg + 1) * TG)
                            hdeps = T_hTown[tg * TPG:(tg + 1) * TPG]
                            for cc in range(8):
                                ib = it % 2
                                it += 1
                                csl = slice(cc * 128, (cc + 1) * 128)
                                bga, bgp, bya, byp = 0 + ib, 2 + ib, 4 + ib, 6 + ib

                                def mm_ga(e, csl=csl, bga=bga, tsl=tsl):
                                    for kc in range(8):
                                        ins = e.matmul(banks[bga][:, 0:TG], lhsT=wga[:, kc, csl], rhs=hT_own[:, kc, tsl], start=(kc == 0), stop=(kc == 7))
                                    return ins

                                def mm_gp(e, csl=csl, bgp=bgp, tsl=tsl):
                                    for kc in range(8):
                                        ins = e.matmul(banks[bgp][:, 0:TG], lhsT=wgp[:, kc, csl], rhs=hT_own[:, kc, tsl], start=(kc == 0), stop=(kc == 7))
                                    return ins

                                def mm_ya(e, csl=csl, tsl=tsl, bya=bya):
                                    for hh in range(4):
                                        ins = e.matmul(banks[bya][:, 0:TG], lhsT=wba[:, hh, csl], rhs=oT[:, hh, tsl], start=(hh == 0), stop=(hh == 3))
                                    return ins

                                def mm_yp(e, csl=csl, tsl=tsl, byp=byp):
                                    for gg in range(4):
                                        ins = e.matmul(banks[byp][:, 0:TG], lhsT=wbb[:, gg, csl], rhs=yBT[:, gg, tsl], start=(gg == 0), stop=(gg == 3))
                                    return ins
                                S.op("pe", mm_ga, reads=[T_wd2] + hdeps, writes=[Tb[bga]])
                                S.op("act", lambda e, ib=ib, bga=bga: e.activation(out=sga[ib][:], in_=banks[bga][:, 0:TG], func=AF.Sigmoid),
                                     reads=[Tb[bga]], writes=[T_sg[ib]])
                                S.op("pe", mm_gp, reads=[T_wd2] + hdeps, writes=[Tb[bgp]])
                                S.op("act", lambda e, ib=ib, bgp=bgp: e.activation(out=sgp[ib][:], in_=banks[bgp][:, 0:TG], func=AF.Sigmoid),
                                     reads=[Tb[bgp]], writes=[T_sp[ib]])
                                S.op("pe", mm_ya, reads=[T_wd2, T_oT], writes=[Tb[bya]])
                                S.op("dve", lambda e, ib=ib, bya=bya: e.tensor_tensor(out=t1[ib][:], in0=banks[bya][:, 0:TG], in1=sga[ib][:], op=ALU.mult),
                                     reads=[Tb[bya], T_sg[ib]], writes=[T_t1[ib]])
                                S.op("pe", mm_yp, reads=[T_wd2, T_yBT], writes=[Tb[byp]])
                                S.op("dve", lambda e, ib=ib, byp=byp: e.tensor_tensor(out=t2[ib][:], in0=banks[byp][:, 0:TG], in1=sgp[ib][:], op=ALU.mult),
                                     reads=[Tb[byp], T_sp[ib]], writes=[T_t2[ib]])
                                S.op("pool", lambda e, ib=ib, cc=cc, tsl=tsl: e.tensor_tensor(out=mT[:, cc, tsl], in0=t1[ib][:], in1=t2[ib][:], op=ALU.add),
                                     reads=[T_t1[ib], T_t2[ib]], writes=[T_mT[tg]])
                        S.barrier()

                    with ExitStack() as pd3:
                        wo = arena2[:, 0:8192].rearrange("p (c n) -> p c n", c=8)
                        T_wo = Tok()
                        S.dma("pool", wo, w_out.rearrange("(c p) n -> p c n", p=128), "wo", writes=[T_wo])
                        for t in range(NSLOT):
                            S.dma("sp", x1[:, t, :], x_own[t * 128:(t + 1) * 128, :], f"x1_{t % 4}", writes=[T_x1[t]])
                        tres = [sbuf(pd3, f"tres{i}", [128, 512], F32) for i in range(2)]
                        T_tres = [Tok() for _ in range(2)]
                        oi = 0
                        for slot in range(NSLOT):
                            tg = slot // TPG
                            for half in range(2):
                                ob_ = oi % 4
                                rb_ = oi % 2
                                oi += 1

                                def mm_o(e, slot=slot, half=half, ob_=ob_):
                                    for kc in range(8):
                                        ins = e.matmul(banks[ob_][:], lhsT=mT[:, kc, slot * 128:(slot + 1) * 128],
                                                       rhs=wo[:, kc, half * 512:(half + 1) * 512], start=(kc == 0), stop=(kc == 7))
                                    return ins
                                S.op("pe", mm_o, reads=[T_wo, T_mT[tg]], writes=[Tb[ob_]])
                                S.op("dve", lambda e, half=half, ob_=ob_, rb_=rb_: e.tensor_tensor(out=tres[rb_][:], in0=banks[ob_][:],
                                                                                                  in1=gate1_bc[:, half * 512:(half + 1) * 512], op=ALU.mult),
                                     reads=[Tb[ob_], T_mod], writes=[T_tres[rb_]])
                                S.op("pool", lambda e, half=half, rb_=rb_, slot=slot: e.tensor_tensor(
                                    out=x1[:, slot, half * 512:(half + 1) * 512], in0=x1[:, slot, half * 512:(half + 1) * 512], in1=tres[rb_][:], op=ALU.add),
                                    reads=[T_tres[rb_], T_x1[slot]], writes=[T_x1[slot]])
                        S.barrier()

            if dbg:
                d_x1 = nc.dram_tensor("d_x1", [NOWN, D], F32, kind="ExternalOutput").ap()
                for t in range(NSLOT):
                    S.dma("sp", d_x1[t * 128:(t + 1) * 128, :], x1[:, t, :], "dbg", reads=[T_x1[t]])
                S.barrier()

            I32 = mybir.dt.int32
            IOA = bass.IndirectOffsetOnAxis
            XROWS = NE * CAPR
            with ExitStack() as pe_:
                NS = NSLOT
                idx_i = sbuf(pe_, "idx_i", [128, 2, NS], I32)
                wts = sbuf(pe_, "wts", [128, 2, NS], F32)
                cnt_i = sbuf(pe_, "cnt_i", [128, 16], I32)
                zidx_i = sbuf(pe_, "zidx_i", [128, 16], I32)
                T_idx = Tok()
                T_sc = []
                wgt = [sbuf(pe_, "wgt0", [128, 8, DE], BF16)]
                wut = [sbuf(pe_, "wut0", [128, 8, DE], BF16)]
                wdt = [sbuf(pe_, "wdt0", [128, 4, D], BF16)]
                T_we = [Tok() for _ in range(2)]

                def load_expert(ei):
                    b = ei % 2
                    S.dma("pool", wgt[b][:], e_wg[ei].rearrange("(c p) n -> p c n", p=128), f"we{b}", writes=[T_we[b]])
                    S.dma("pool", wut[b][:], e_wu[ei].rearrange("(c p) n -> p c n", p=128), f"we{b}", writes=[T_we[b]])
                    S.dma("pool", wdt[b][:], e_wd[ei].rearrange("(c p) n -> p c n", p=128), f"we{b}", writes=[T_we[b]])
                load_expert(0)
                with ExitStack() as pn:
                    h2tm = sbuf(pn, "h2tm", [128, NS, D], BF16)
                    T_h2tm = [Tok() for _ in range(NS)]
                    logits = sbuf(pn, "logits", [128, NS, 20], F32)
                    T_lg = Tok()
                    xh2 = [sbuf(pn, f"xh2_{i}", [128, D], F32) for i in range(2)]
                    T_xh2 = [Tok() for _ in range(2)]
                    hrow = [sbuf(pn, f"hrow{i}", [128, D], F32) for i in range(2)]
                    T_hrow = [Tok() for _ in range(2)]
                    junk2 = sbuf(pn, "junk2", [128, D], BF16)
                    T_junk2 = Tok()
                    ss2 = [sbuf(pn, f"ss2_{i}", [128, 1], F32) for i in range(2)]
                    T_ss2 = [Tok() for _ in range(2)]
                    h2f = [sbuf(pn, f"h2f{i}", [128, 8, 128], F32) for i in range(2)]
                    T_h2f = [Tok() for _ in range(2)]
                    zt = sbuf(pn, "zt", [128, D], BF16)
                    T_zt = Tok()
                    gs2_bc = sbuf(pn, "gs2_bc", [128, D], F32)
                    sh2_bc = sbuf(pn, "sh2_bc", [128, D], F32)
                    T_bc2 = Tok()
                    S.dma("sp", gs2_bc[:], modd[0:1, :].broadcast_to([128, D]), "bc2", reads=[T_modd], writes=[T_bc2])
                    S.dma("sp", sh2_bc[:], modd[1:2, :].broadcast_to([128, D]), "bc2", reads=[T_modd], writes=[T_bc2])
                    S.op("pool", lambda e: e.memset(zt[:], 0.0), writes=[T_zt])

                    def n2_stage1(t):
                        b2 = t % 2
                        S.op("act", lambda e: e.activation(out=junk2[:], in_=x1[:, t, :], func=AF.Square, accum_out=ss2[b2][:]),
                             reads=[T_x1[t]], writes=[T_junk2, T_ss2[b2]])
                        S.op("act", lambda e: e.activation(out=ss2[b2][:], in_=ss2[b2][:], func=AF.Sqrt, bias=eps_t[:], scale=1.0 / D),
                             reads=[T_ss2[b2], T_const], writes=[T_ss2[b2]])
                        S.op("dve", lambda e: e.reciprocal(out=ss2[b2][:], in_=ss2[b2][:]), reads=[T_ss2[b2]], writes=[T_ss2[b2]])
                        S.op("dve", lambda e: e.tensor_scalar(out=xh2[b2][:], in0=x1[:, t, :], scalar1=ss2[b2][:, 0:1], scalar2=None, op0=ALU.mult),
                             reads=[T_x1[t], T_ss2[b2]], writes=[T_xh2[b2]])

                    def n2_stage2(t):
                        b2 = t % 2
                        pa_, pb_ = (0, 1) if b2 == 0 else (2, 3)

                        def tr2(e):
                            for kc in range(8):
                                bk = pa_ if kc < 4 else pb_
                                ins = e.transpose(out=banks[bk][:, (kc % 4) * 128:(kc % 4 + 1) * 128], in_=xh2[b2][:, kc * 128:(kc + 1) * 128],
                                                  identity=ident_f[:])
                            return ins
                        S.op("pe", tr2, reads=[T_xh2[b2], T_const], writes=[Tb[pa_], Tb[pb_]])
                        S.op("pool", lambda e: e.tensor_tensor(out=hrow[b2][:], in0=xh2[b2][:], in1=gs2_bc[:], op=ALU.mult),
                             reads=[T_xh2[b2], T_bc2], writes=[T_hrow[b2]])
                        S.op("pool", lambda e: e.tensor_tensor(out=h2tm[:, t, :], in0=hrow[b2][:], in1=sh2_bc[:], op=ALU.add),
                             reads=[T_hrow[b2], T_bc2], writes=[T_h2tm[t]])
                        for kc in range(8):
                            bk = pa_ if kc < 4 else pb_
                            S.op("act", lambda e, kc=kc, bk=bk: e.activation(
                                out=h2f[b2][:, kc, :], in_=banks[bk][:, (kc % 4) * 128:(kc % 4 + 1) * 128], func=AF.Identity,
                                bias=modT[:, 16 + kc:17 + kc], scale=gs2[:, kc:kc + 1]), reads=[Tb[bk], T_mod], writes=[T_h2f[b2]])

                        def mmr(e):
                            for kc in range(8):
                                ins = e.matmul(banks[4 + b2][:, 0:20], lhsT=h2f[b2][:, kc, :], rhs=wr_f[:, kc, :], start=(kc == 0), stop=(kc == 7))
                            return ins
                        S.op("pe", mmr, reads=[T_h2f[b2], T_const], writes=[Tb[4 + b2]])
                        S.op("dve", lambda e: e.tensor_tensor(out=logits[:, t, :], in0=banks[4 + b2][:, 0:20], in1=rbias[:], op=ALU.add),
                             reads=[Tb[4 + b2], T_const], writes=[T_lg])

                    n2_stage1(0)
                    for t in range(NS):
                        if t + 1 < NS:
                            n2_stage1(t + 1)
                        n2_stage2(t)

                    r1 = sbuf(pn, "r1", [128, NS, 16], F32)
                    r2 = sbuf(pn, "r2", [128, NS, 16], F32)
                    r3 = sbuf(pn, "r3", [128, NS, 16], F32)
                    oh1 = sbuf(pn, "oh1", [128, NS, 16], F32)
                    oh2 = sbuf(pn, "oh2", [128, NS, 16], F32)
                    Mb = sbuf(pn, "Mb", [128, NS, 16], BF16)
                    tot = sbuf(pn, "tot", [128, NS, 16], F32)
                    off = sbuf(pn, "off", [128, NS, 16], F32)
                    posb = sbuf(pn, "posb", [128, NS, 16], F32)
                    idxf = sbuf(pn, "idxf", [128, 2, NS], F32)
                    cntf = sbuf(pn, "cntf", [128, 16], F32)
                    zf = sbuf(pn, "zf", [128, 16], F32)
                    pen = sbuf(pn, "pen", [128, NS, 4], F32)
                    elc = sbuf(pn, "elc", [128, NS, 16], F32)
                    gmx = sbuf(pn, "gmx", [128, NS], F32)
                    gsm = sbuf(pn, "gsm", [128, NS], F32)
                    m1 = sbuf(pn, "m1", [128, NS], F32)
                    m2 = sbuf(pn, "m2", [128, NS], F32)
                    w1 = sbuf(pn, "w1", [128, NS], F32)
                    w2 = sbuf(pn, "w2", [128, NS], F32)
                    T_r = Tok()
                    X = mybir.AxisListType.X
                    gl = logits[:, :, 0:4]
                    el = logits[:, :, 4:20]

                    def R(fn, eng="dve", extra=()):
                        S.op(eng, fn, reads=[T_r, T_lg, T_const] + list(extra), writes=[T_r] + list(extra))
                    R(lambda e: e.tensor_reduce(out=gmx[:], in_=gl, axis=X, op=ALU.max))
                    R(lambda e: e.tensor_tensor(out=r1[:, :, 0:4], in0=gl, in1=gmx[:].unsqueeze(2).broadcast_to([128, NS, 4]), op=ALU.subtract))
                    R(lambda e: e.activation(out=r2[:, :, 0:4], in_=r1[:, :, 0:4], func=AF.Exp), eng="act")
                    R(lambda e: e.tensor_reduce(out=gsm[:], in_=r2[:, :, 0:4], axis=X, op=ALU.add))
                    R(lambda e: e.reciprocal(out=gsm[:], in_=gsm[:]))
                    R(lambda e: e.tensor_scalar(out=pen[:], in0=r1[:, :, 0:4], scalar1=0.0, scalar2=None, op0=ALU.is_lt))
                    R(lambda e: e.tensor_copy(out=elc[:], in_=el))
                    R(lambda e: e.scalar_tensor_tensor(out=r3[:].rearrange("p s (g k) -> p (s g) k", g=4),
                                                       in0=pen[:].rearrange("p s g -> p (s g)").unsqueeze(2).broadcast_to([128, NS * 4, 4]), scalar=NEG,
                                                       in1=elc[:].rearrange("p s (g k) -> p (s g) k", g=4), op0=ALU.mult, op1=ALU.add))
                    R(lambda e: e.tensor_reduce(out=m1[:], in_=r3[:], axis=X, op=ALU.max))
                    R(lambda e: e.tensor_tensor(out=oh1[:], in0=r3[:], in1=m1[:].unsqueeze(2).broadcast_to([128, NS, 16]), op=ALU.is_ge))
                    R(lambda e: e.scalar_tensor_tensor(out=r1[:], in0=oh1[:], scalar=NEG, in1=r3[:], op0=ALU.mult, op1=ALU.add))
                    R(lambda e: e.tensor_reduce(out=m2[:], in_=r1[:], axis=X, op=ALU.max))
                    R(lambda e: e.tensor_tensor(out=oh2[:], in0=r1[:], in1=m2[:].unsqueeze(2).broadcast_to([128, NS, 16]), op=ALU.is_ge))
                    R(lambda e: e.tensor_tensor(out=m2[:], in0=m2[:], in1=m1[:], op=ALU.subtract))
                    R(lambda e: e.activation(out=w2[:], in_=m2[:], func=AF.Sigmoid), eng="act")
                    R(lambda e: e.tensor_scalar(out=w1[:], in0=w2[:], scalar1=-1.0, scalar2=1.0, op0=ALU.mult, op1=ALU.add))
                    R(lambda e: e.tensor_tensor(out=wts[:, 0, :], in0=w1[:], in1=gsm[:], op=ALU.mult), extra=[T_idx])
                    R(lambda e: e.tensor_tensor(out=wts[:, 1, :], in0=w2[:], in1=gsm[:], op=ALU.mult), extra=[T_idx])
                    R(lambda e: e.tensor_tensor(out=Mb[:], in0=oh1[:], in1=oh2[:], op=ALU.add))
                    Mflat = Mb[:].rearrange("p s e -> p (s e)")
                    S.op("pe", lambda e: e.matmul(banks[6][:, 0:NS * 16], lhsT=ustrict_b[:], rhs=Mflat, start=True, stop=True),
                         reads=[T_r, T_const], writes=[Tb[6]])
                    S.op("pe", lambda e: e.matmul(banks[7][:, 0:NS * 16], lhsT=ones_b[:], rhs=Mflat, start=True, stop=True),
                         reads=[T_r, T_const], writes=[Tb[7]])
                    S.op("dve", lambda e: e.tensor_copy(out=tot[:].rearrange("p s e -> p (s e)"), in_=banks[7][:, 0:NS * 16]),
                         reads=[Tb[7], T_r], writes=[T_r])
                    R(lambda e: e.memset(off[:, 0, :], 0.0))
                    for t in range(1, NS):
                        R(lambda e, t=t: e.tensor_tensor(out=off[:, t, :], in0=off[:, t - 1, :], in1=tot[:, t - 1, :], op=ALU.add))
                    S.op("dve", lambda e: e.tensor_tensor(out=posb[:].rearrange("p s e -> p (s e)"), in0=banks[6][:, 0:NS * 16],
                                                          in1=off[:].rearrange("p s e -> p (s e)"), op=ALU.add),
                         reads=[Tb[6], T_r], writes=[T_r])
                    R(lambda e: e.tensor_tensor(out=posb[:], in0=posb[:], in1=ebase[:, 0:16].unsqueeze(1).broadcast_to([128, NS, 16]), op=ALU.add))
                    R(lambda e: e.tensor_tensor(out=r1[:], in0=oh1[:], in1=posb[:], op=ALU.mult))
                    R(lambda e: e.tensor_reduce(out=idxf[:, 0, :], in_=r1[:], axis=X, op=ALU.add))
                    R(lambda e: e.tensor_tensor(out=r2[:], in0=oh2[:], in1=posb[:], op=ALU.mult))
                    R(lambda e: e.tensor_reduce(out=idxf[:, 1, :], in_=r2[:], axis=X, op=ALU.add))
                    R(lambda e: e.tensor_copy(out=idx_i[:], in_=idxf[:]), extra=[T_idx])
                    R(lambda e: e.tensor_tensor(out=cntf[:], in0=off[:, NS - 1, :], in1=tot[:, NS - 1, :], op=ALU.add))
                    R(lambda e: e.tensor_copy(out=cnt_i[:], in_=cntf[:]), extra=[T_idx])
                    R(lambda e: e.scalar_tensor_tensor(out=zf[:], in0=cntf[:], scalar=ebase[:, 16:17], in1=ebase[:, 0:16], op0=ALU.add, op1=ALU.add))
                    R(lambda e: e.tensor_copy(out=zidx_i[:], in_=zf[:]), extra=[T_idx])
                    for ei in range(NE):
                        tk = Tok()
                        T_sc.append(tk)
                        S.indirect("zf", [T_zt, T_idx], [tk], out=Xs, out_offset=IOA(ap=zidx_i[:, ei:ei + 1], axis=0), in_=zt[:], in_offset=None)
                    for t in range(NS):
                        for k in range(2):
                            tk = Tok()
                            T_sc.append(tk)
                            S.indirect("sc", [T_h2tm[t], T_idx], [tk], out=Xs, out_offset=IOA(ap=idx_i[:, k, t:t + 1], axis=0), in_=h2tm[:, t, :],
                                       in_offset=None)
                    S.barrier()

                if dbg:
                    d_idx = nc.dram_tensor("d_idx", [128, 2 * NS], I32, kind="ExternalOutput").ap()
                    S.dma("sp", d_idx, idx_i[:].rearrange("p k s -> p (k s)"), "dbg", reads=[T_idx])
                    d_cnt = nc.dram_tensor("d_cnt", [128, 16], I32, kind="ExternalOutput").ap()
                    S.dma("sp", d_cnt, cnt_i[:], "dbg", reads=[T_idx])
                    d_w = nc.dram_tensor("d_w", [128, 2 * NS], F32, kind="ExternalOutput").ap()
                    S.dma("sp", d_w, wts[:].rearrange("p k s -> p (k s)"), "dbg", reads=[T_idx])

                wgt.append(sbuf(pe_, "wgt1", [128, 8, DE], BF16))
                wut.append(sbuf(pe_, "wut1", [128, 8, DE], BF16))
                wdt.append(sbuf(pe_, "wdt1", [128, 4, D], BF16))
                load_expert(1)
                xg = [sbuf(pe_, f"xg{i}", [128, D], BF16) for i in range(3)]
                T_xg = [Tok() for _ in range(3)]
                xgT = [sbuf(pe_, f"xgT{i}", [128, 8, 128], BF16) for i in range(2)]
                T_xgT = [Tok() for _ in range(2)]
                sgl = [sbuf(pe_, f"sgl{i}", [128, 512], BF16) for i in range(2)]
                T_sgl = [Tok() for _ in range(2)]
                hidT = [sbuf(pe_, f"hidT{i}", [128, 4, 128], BF16) for i in range(2)]
                T_hid = [Tok() for _ in range(2)]
                hid_tm = [sbuf(pe_, f"hid_tm{i}", [128, 512], BF16) for i in range(2)]
                T_htm = [Tok() for _ in range(2)]
                ysb = [sbuf(pe_, f"ysb{i}", [128, D], F32) for i in range(2)]
                T_ysb = [Tok() for _ in range(2)]
                T_ys = [Tok() for _ in range(2)]
                regsets = [bass.RegisterHandles([S.engs[e].alloc_register(f"necnt{i}_" + e) for e in S.engs]) for i in range(2)]
                tile_ctr = [0]

                def tile_body(ei, ti):
                    n = tile_ctr[0]
                    tile_ctr[0] += 1
                    wb = ei % 2
                    b3, b2 = n % 3, n % 2
                    row0 = ei * CAPR + ti * 128
                    S.dma("sp", xg[b3][:], Xs[row0:row0 + 128, :], f"xg{b3}", reads=T_sc, writes=[T_xg[b3]])
                    pbT = n % 2
                    pbf = bank_bf(pbT)

                    def trx(e):
                        for kc in range(8):
                            ins = e.transpose(out=pbf[:, kc * 128:(kc + 1) * 128], in_=xg[b3][:, kc * 128:(kc + 1) * 128], identity=ident_b[:])
                        return ins
                    S.op("pe", trx, reads=[T_xg[b3], T_const], writes=[Tb[pbT]])
                    S.op("act", lambda e: e.copy(out=xgT[b2][:, 0:4, :].rearrange("p c n -> p (c n)"), in_=pbf[:, 0:512]),
                         reads=[Tb[pbT]], writes=[T_xgT[b2]])
                    S.op("dve", lambda e: e.tensor_copy(out=xgT[b2][:, 4:8, :].rearrange("p c n -> p (c n)"), in_=pbf[:, 512:1024]),
                         reads=[Tb[pbT]], writes=[T_xgT[b2]])
                    pg, pu = 2 + b2, 4 + b2

                    def mm_gate(e):
                        for kc in range(8):
                            ins = e.matmul(banks[pg][:], lhsT=xgT[b2][:, kc, :], rhs=wgt[wb][:, kc, :], start=(kc == 0), stop=(kc == 7))
                        return ins

                    def mm_up(e):
                        for kc in range(8):
                            ins = e.matmul(banks[pu][:], lhsT=xgT[b2][:, kc, :], rhs=wut[wb][:, kc, :], start=(kc == 0), stop=(kc == 7))
                        return ins
                    S.op("pe", mm_gate, reads=[T_we[wb], T_xgT[b2]], writes=[Tb[pg]])
                    S.op("act", lambda e: e.activation(out=sgl[b2][:], in_=banks[pg][:], func=AF.Silu), reads=[Tb[pg]], writes=[T_sgl[b2]])
                    S.op("pe", mm_up, reads=[T_we[wb], T_xgT[b2]], writes=[Tb[pu]])
                    S.op("dve", lambda e: e.tensor_tensor(out=hid_tm[b2][:], in0=banks[pu][:], in1=sgl[b2][:], op=ALU.mult),
                         reads=[Tb[pu], T_sgl[b2]], writes=[T_htm[b2]])
                    pbf2 = bank_bf(pbT)

                    def trh(e):
                        for fc in range(4):
                            ins = e.transpose(out=pbf2[:, fc * 128:(fc + 1) * 128], in_=hid_tm[b2][:, fc * 128:(fc + 1) * 128], identity=ident_b[:])
                        return ins
                    S.op("pe", trh, reads=[T_htm[b2], T_const], writes=[Tb[pbT]])
                    S.op("act", lambda e: e.copy(out=hidT[b2][:].rearrange("p c n -> p (c n)"), in_=pbf2[:, 0:512]),
                         reads=[Tb[pbT]], writes=[T_hid[b2]])
                    for half in range(2):
                        py = 6 + half

                        def mm_y(e, half=half, py=py):
                            for fc in range(4):
                                ins = e.matmul(banks[py][:], lhsT=hidT[b2][:, fc, :], rhs=wdt[wb][:, fc, half * 512:(half + 1) * 512],
                                               start=(fc == 0), stop=(fc == 3))
                            return ins
                        S.op("pe", mm_y, reads=[T_we[wb], T_hid[b2]], writes=[Tb[py]])
                        if half == 0:
                            S.op("act", lambda e, py=py: e.copy(out=ysb[b2][:, 0:512], in_=banks[py][:]), reads=[Tb[py]], writes=[T_ysb[b2]])
                        else:
                            S.op("dve", lambda e, py=py: e.tensor_copy(out=ysb[b2][:, 512:1024], in_=banks[py][:]), reads=[Tb[py]], writes=[T_ysb[b2]])
                    S.dma("pool", Ys[row0:row0 + 128, :], ysb[b2][:], f"ys{b2}", reads=[T_ysb[b2]], writes=[T_ys[b2]])

                for e_ in S.engs:
                    S._deps(e_, [T_idx], [])
                nc.regs_load(regsets[0], cnt_i[0:1, 0:1])
                for ei in range(NE):
                    regs = regsets[ei % 2]
                    if ei + 1 < NE:
                        nc.regs_load(regsets[(ei + 1) % 2], cnt_i[0:1, ei + 1:ei + 2])
                    def nest(ti, ei=ei, regs=regs):
                        tile_body(ei, ti)
                        if ti + 1 < NS:
                            S.cond_region(regs, (ti + 1) * 128, lambda: nest(ti + 1))
                    S.cond_region(regs, 0, lambda: nest(0))
                    if ei + 2 < NE:
                        load_expert(ei + 2)

                NGB = 3
                yA = [sbuf(pe_, f"yA{i}", [128, D], F32) for i in range(NGB)]
                yB = [sbuf(pe_, f"yB{i}", [128, D], F32) for i in range(NGB)]
                T_yA = [Tok() for _ in range(NGB)]
                T_yB = [Tok() for _ in range(NGB)]
                T_out = Tok()

                def gather(t):
                    b = t % NGB
                    S.indirect(f"ga{b}", T_ys + [T_idx], [T_yA[b]], out=yA[b][:], out_offset=None, in_=Ys,
                               in_offset=IOA(ap=idx_i[:, 0, t:t + 1], axis=0))
                    S.indirect(f"gb{b}", T_ys + [T_idx], [T_yB[b]], out=yB[b][:], out_offset=None, in_=Ys,
                               in_offset=IOA(ap=idx_i[:, 1, t:t + 1], axis=0))
                for t in range(min(NGB - 1, NS)):
                    gather(t)
                for t in range(NS):
                    b = t % NGB
                    if t + NGB - 1 < NS:
                        gather(t + NGB - 1)
                    S.op("dve", lambda e, b=b, t=t: e.tensor_scalar(out=yA[b][:], in0=yA[b][:], scalar1=wts[:, 0, t:t + 1], scalar2=None, op0=ALU.mult),
                         reads=[T_yA[b], T_idx], writes=[T_yA[b]])
                    S.op("dve", lambda e, b=b, t=t: e.scalar_tensor_tensor(out=yB[b][:], in0=yB[b][:], scalar=wts[:, 1, t:t + 1], in1=yA[b][:],
                                                                           op0=ALU.mult, op1=ALU.add),
                         reads=[T_yA[b], T_yB[b], T_idx], writes=[T_yB[b]])
                    S.op("dve", lambda e, b=b: e.tensor_tensor(out=yB[b][:], in0=yB[b][:], in1=gate2_bc[:], op=ALU.mult),
                         reads=[T_yB[b], T_mod], writes=[T_yB[b]])
                    S.op("dve", lambda e, b=b, t=t: e.tensor_tensor(out=x1[:, t, :], in0=x1[:, t, :], in1=yB[b][:], op=ALU.add),
                         reads=[T_yB[b], T_x1[t]], writes=[T_x1[t]])
                    S.dma("sp", out_own[t * 128:(t + 1) * 128, :], x1[:, t, :], "out", reads=[T_x1[t]], writes=[T_out])
                S.barrier()

        @blk.sync
        def _(_unused):
            with nc.allow_non_contiguous_dma(reason="small one-time parameter layouts"):
                _body()

    return nc


def _own_blocks(r, npairs):
    blocks = []
    for j in range(npairs):
        blocks += [8 * j + r, 8 * j + 7 - r]
    return blocks


def make_in_maps(inputs, S_LEN):
    f = lambda a: np.ascontiguousarray(np.asarray(a, dtype=np.float32))
    x = f(inputs["x"])
    B = x.shape[0]
    npairs = S_LEN // 1024
    shared = {
        "rel_bias": f(inputs["rel_bias"]),
        "ada_w": f(inputs["ada_w"][0]),
        "ada_b": f(inputs["ada_b"][0]).reshape(1, -1),
        "norm1_g": f(inputs["norm1_g"][0]).reshape(1, -1),
        "w_in": f(inputs["w_in"][0]),
        "q_norm_g": f(inputs["q_norm_g"][0]).reshape(1, -1),
        "k_norm_g": f(inputs["k_norm_g"][0]).reshape(1, -1),
        "lam_in": f(np.concatenate([inputs["lambda_q1"][0], inputs["lambda_k1"][0],
                                    inputs["lambda_q2"][0], inputs["lambda_k2"][0]])).reshape(1, -1),
        "subln_g": f(inputs["subln_g"][0]).reshape(1, -1),
        "w_ba": f(inputs["w_branch_attn"][0]),
        "pool_w": f(inputs["pool_w"][0]),
        "pool_scale": f(inputs["pool_scale"][0]).reshape(1, -1),
        "w_bb": f(inputs["w_branch_pool"][0]),
        "w_out": f(inputs["w_out"][0]),
        "norm2_g": f(inputs["norm2_g"][0]).reshape(1, -1),
        "r_w": f(np.concatenate([inputs["router_group_w"][0], inputs["router_expert_w"][0]], axis=1)),
        "r_b": f(np.concatenate([inputs["router_group_b"][0], inputs["router_expert_b"][0]])).reshape(1, -1),
        "e_wg": f(inputs["expert_w_gate"][0]),
        "e_wu": f(inputs["expert_w_up"][0]),
        "e_wd": f(inputs["expert_w_down"][0]),
        "ident_in": np.eye(128, dtype=np.float32),
        "bones_in": np.kron(np.eye(2, dtype=np.float32), np.ones((64, 64), np.float32)),
        "ustrict_in": np.triu(np.ones((128, 128), np.float32), 1),
        "ebase_in": np.concatenate([np.tile(np.arange(16, dtype=np.float32) * (S_LEN // 4 + 128), (128, 1)),
                                    np.arange(128, dtype=np.float32)[:, None]], axis=1),
    }
    c = f(inputs["c"])
    in_maps = []
    metas = []
    for core in range(4 * B):
        b, r = core // 4, core % 4
        blocks = _own_blocks(r, npairs)
        xb = x[b]
        x_own = np.concatenate([xb[k * 128:(k + 1) * 128] for k in blocks], axis=0)
        halo = []
        for k in blocks:
            if k == 0:
                halo.append(np.zeros((16, D), np.float32))
            else:
                halo.append(xb[k * 128 - 16:k * 128])
        x_halo = np.concatenate(halo, axis=0)
        x_kv = xb.reshape(S_LEN // 128, 128, D)[:, ::-1, :].reshape(S_LEN, D)
        oh, mask = _core_tables(r)
        hv, ic = _pool_tables(blocks)
        m = dict(shared)
        m.update({
            "x_own": np.ascontiguousarray(x_own), "x_halo": np.ascontiguousarray(x_halo),
            "x_kv": np.ascontiguousarray(x_kv), "c_row": c[b:b + 1],
            "oh_in": oh, "mask_in": mask, "hv_in": hv, "ic_in": ic,
        })
        in_maps.append(m)
        metas.append((b, blocks))
    return in_maps, metas


def kernel(**inputs):
    x = np.asarray(inputs["x"])
    B, S_LEN, _ = x.shape
    nc = build_program(S_LEN)
    in_maps, metas = make_in_maps(inputs, S_LEN)
    res = run_bass_kernel_spmd(nc, in_maps, core_ids=list(range(len(in_maps))))
    out = np.empty((B, S_LEN, D), np.float32)
    for (b, blocks), r in zip(metas, res.results):
        o = np.asarray(r["out_own"])
        for si, k in enumerate(blocks):
            out[b, k * 128:(k + 1) * 128] = o[si * 128:(si + 1) * 128]
    return out
```
